# Optimizing a Trainium2 kernel written in Bass

```python
import functools
import jax, jax.numpy as jnp
from jax import lax
import numpy as np

D_MODEL = 2048
BATCH = 2
SEQ = 4096
DEPTH = 1
DEC_BATCH = 8
DEC_SEQ = 4
PAST_LEN = 16384
PAGE_SIZE = 128

N_HEADS = 8
HEAD_DIM = 128
ATTN_WIDTH = N_HEADS * HEAD_DIM
CONV_WIDTH = D_MODEL - ATTN_WIDTH
MIX_WIDTH = ATTN_WIDTH + CONV_WIDTH
IN_COLS = 3 * ATTN_WIDTH + 2 * CONV_WIDTH
CONV_K = 31
CONV_BUF = CONV_K - 1
MOBA_BLOCK = 256
MOBA_TOPK = 3
Q_CHUNK = 32
D_FF = ((8 * D_MODEL + 3 * 256 - 1) // (3 * 256)) * 256
EPS = 1e-6
NEG = -1e30

kernel_name = 'hymba_moba_conformer_conv_adaln_step'


def rmsnorm(x, g):
    xf = x.astype(jnp.float32)
    y = xf * lax.rsqrt(jnp.mean(xf * xf, axis=-1, keepdims=True) + EPS)
    return (y * g.astype(jnp.float32)).astype(x.dtype)


def layernorm(x, g, b):
    xf = x.astype(jnp.float32)
    mu = jnp.mean(xf, axis=-1, keepdims=True)
    var = jnp.mean(jnp.square(xf - mu), axis=-1, keepdims=True)
    y = (xf - mu) * lax.rsqrt(var + EPS)
    return (y * g.astype(jnp.float32) + b.astype(jnp.float32)).astype(x.dtype)


def alibi_slopes():
    return 2.0 ** (-8.0 * (jnp.arange(N_HEADS, dtype=jnp.float32) + 1.0) / N_HEADS)


def select_blocks(q, kmean, t_pos):
    s = jnp.einsum('bhqd,bhnd->bhqn', q.astype(jnp.float32), kmean.astype(jnp.float32))
    nb = kmean.shape[2]
    n_full = t_pos // MOBA_BLOCK
    s = jnp.where(jnp.arange(nb)[None, :] < n_full[:, None], s, NEG)
    if nb < MOBA_TOPK:
        s = jnp.pad(s, ((0, 0), (0, 0), (0, 0), (0, MOBA_TOPK - nb)), constant_values=NEG)
    _, idx = lax.top_k(s, MOBA_TOPK)
    valid = jnp.arange(MOBA_TOPK)[None, :] < n_full[:, None]
    idx = jnp.where(valid, idx, 0)
    return idx, valid


def moba_attend(q, t_pos, k_sel, v_sel, pos_sel, valid_sel, k_own, v_own, pos_own, slopes):
    B, H, Q, J, N, _ = k_sel.shape
    qf = q.astype(jnp.float32) * (HEAD_DIM ** -0.5)
    m = slopes[None, :, None, None]
    dist_sel = (t_pos[:, None, None] - pos_sel).astype(jnp.float32)
    s_sel = jnp.einsum('bhqd,bhqjnd->bhqjn', qf, k_sel.astype(jnp.float32)) - m[..., None] * dist_sel
    s_sel = jnp.where(valid_sel[..., None], s_sel, NEG).reshape(B, H, Q, J * N)
    dist_own = t_pos[:, None] - pos_own[None, :]
    s_own = jnp.einsum('bhqd,bhnd->bhqn', qf, k_own.astype(jnp.float32)) - m * dist_own.astype(jnp.float32)
    s_own = jnp.where(dist_own >= 0, s_own, NEG)
    p = jax.nn.softmax(jnp.concatenate([s_sel, s_own], axis=-1), axis=-1)
    p_sel = p[..., : J * N].reshape(B, H, Q, J, N)
    out = (jnp.einsum('bhqjn,bhqjnd->bhqd', p_sel, v_sel.astype(jnp.float32))
           + jnp.einsum('bhqn,bhnd->bhqd', p[..., J * N:], v_own.astype(jnp.float32)))
    return out.astype(q.dtype)


def moba_prompt(q, k, v, slopes):
    B, H, S, _ = q.shape
    nb = -(-S // MOBA_BLOCK)
    pad = nb * MOBA_BLOCK - S
    kb = jnp.pad(k, ((0, 0), (0, 0), (0, pad), (0, 0))).reshape(B, H, nb, MOBA_BLOCK, HEAD_DIM)
    vb = jnp.pad(v, ((0, 0), (0, 0), (0, pad), (0, 0))).reshape(B, H, nb, MOBA_BLOCK, HEAD_DIM)
    kmean = kb.astype(jnp.float32).mean(axis=3)
    bi = jnp.arange(B)[:, None, None, None]
    hi = jnp.arange(H)[None, :, None, None]

    def chunk(ci):
        q0 = ci * Q_CHUNK
        qc = lax.dynamic_slice_in_dim(q, q0, Q_CHUNK, axis=2)
        t_pos = q0 + jnp.arange(Q_CHUNK)
        idx, valid = select_blocks(qc, kmean, t_pos)
        k_sel = kb[bi, hi, idx]
        v_sel = vb[bi, hi, idx]
        pos_sel = idx[..., None] * MOBA_BLOCK + jnp.arange(MOBA_BLOCK)
        own = q0 // MOBA_BLOCK
        k_own = lax.dynamic_index_in_dim(kb, own, axis=2, keepdims=False)
        v_own = lax.dynamic_index_in_dim(vb, own, axis=2, keepdims=False)
        pos_own = own * MOBA_BLOCK + jnp.arange(MOBA_BLOCK)
        return moba_attend(qc, t_pos, k_sel, v_sel, pos_sel, valid, k_own, v_own, pos_own, slopes)

    out = lax.map(chunk, jnp.arange(S // Q_CHUNK))
    return out.transpose(1, 2, 0, 3, 4).reshape(B, H, S, HEAD_DIM)


def moba_sample(q, k_new, v_new, pool_k, pool_v, page_table, slopes):
    DB, H, DS, _ = q.shape
    ppb = MOBA_BLOCK // PAGE_SIZE
    past = page_table.shape[1] * PAGE_SIZE
    n_full = past // MOBA_BLOCK
    own0 = n_full * MOBA_BLOCK
    n_tail = past - own0
    t_pos = past + jnp.arange(DS)
    kp = pool_k[page_table[:, : n_full * ppb]]
    kmean = kp.astype(jnp.float32).reshape(DB, n_full, ppb, H, PAGE_SIZE, HEAD_DIM).mean(axis=(2, 4))
    kmean = kmean.transpose(0, 2, 1, 3)
    idx, valid = select_blocks(q, kmean, t_pos)
    bi = jnp.arange(DB)[:, None, None, None, None]
    hi = jnp.arange(H)[None, :, None, None, None]
    phys = page_table[bi, idx[..., None] * ppb + jnp.arange(ppb)]
    k_sel = pool_k[phys, hi].reshape(DB, H, DS, MOBA_TOPK, MOBA_BLOCK, HEAD_DIM)
    v_sel = pool_v[phys, hi].reshape(DB, H, DS, MOBA_TOPK, MOBA_BLOCK, HEAD_DIM)
    pos_sel = idx[..., None] * MOBA_BLOCK + jnp.arange(MOBA_BLOCK)
    tail_pages = page_table[:, own0 // PAGE_SIZE: past // PAGE_SIZE]
    k_tail = pool_k[tail_pages].transpose(0, 2, 1, 3, 4).reshape(DB, H, n_tail, HEAD_DIM)
    v_tail = pool_v[tail_pages].transpose(0, 2, 1, 3, 4).reshape(DB, H, n_tail, HEAD_DIM)
    k_own = jnp.concatenate([k_tail.astype(k_new.dtype), k_new], axis=2)
    v_own = jnp.concatenate([v_tail.astype(v_new.dtype), v_new], axis=2)
    pos_own = own0 + jnp.arange(n_tail + DS)
    return moba_attend(q, t_pos, k_sel, v_sel, pos_sel, valid, k_own, v_own, pos_own, slopes)


def conv_module(a, gate, buf, w_dw, b_dw, g_ln, b_ln):
    u = a * jax.nn.sigmoid(gate)
    ext = jnp.concatenate([buf.astype(u.dtype), u], axis=1)
    y = lax.conv_general_dilated(ext, w_dw.astype(ext.dtype), window_strides=(1,), padding='VALID',
                                 dimension_numbers=('NWC', 'WIO', 'NWC'),
                                 feature_group_count=CONV_WIDTH) + b_dw
    y = jax.nn.silu(layernorm(y, g_ln, b_ln))
    return y, ext[:, -CONV_BUF:]


def layer(x, c, conv_buf, attend, w_ada, b_ada, g_pre_mix, w_in, w_dw, b_dw, g_conv_ln, b_conv_ln,
          g_attn_out, g_conv_out, w_out, g_post_mix, g_pre_ffn, w_gate_up, w_down, g_post_ffn):
    B, L, _ = x.shape
    mod = jax.nn.silu(c) @ w_ada + b_ada
    sh_m, sc_m, gt_m, sh_f, sc_f, gt_f = jnp.split(mod[:, None, :], 6, axis=-1)
    h = rmsnorm(x, g_pre_mix) * (1.0 + sc_m) + sh_m
    q, k, v, a, g = jnp.split(h @ w_in, [ATTN_WIDTH, 2 * ATTN_WIDTH, 3 * ATTN_WIDTH,
                                        3 * ATTN_WIDTH + CONV_WIDTH], axis=-1)
    to_heads = lambda t: t.reshape(B, L, N_HEADS, HEAD_DIM).transpose(0, 2, 1, 3)
    qh, kh, vh = to_heads(q), to_heads(k), to_heads(v)
    o_attn = attend(qh, kh, vh).transpose(0, 2, 1, 3).reshape(B, L, ATTN_WIDTH)
    o_conv, new_buf = conv_module(a, g, conv_buf, w_dw, b_dw, g_conv_ln, b_conv_ln)
    merged = jnp.concatenate([rmsnorm(o_attn, g_attn_out), rmsnorm(o_conv, g_conv_out)], axis=-1) @ w_out
    x = x + gt_m * rmsnorm(merged, g_post_mix)
    h = rmsnorm(x, g_pre_ffn) * (1.0 + sc_f) + sh_f
    gate, up = jnp.split(h @ w_gate_up, 2, axis=-1)
    x = x + gt_f * rmsnorm((jax.nn.silu(gate) * up) @ w_down, g_post_ffn)
    return x, kh, vh, new_buf


def setup_inputs(seed: int = 0) -> dict:
    key = jax.random.key(seed)
    ks = jax.random.split(key, 32)
    f32 = jnp.float32
    n_pages = PAST_LEN // PAGE_SIZE
    n_used = DEC_BATCH * n_pages
    n_pool = n_used + max(1, n_used // 4)
    nrm = lambda k, shape, s: s * jax.random.normal(k, shape, f32)
    gain = lambda k, n: 1.0 + 0.02 * jax.random.normal(k, (DEPTH, n), f32)
    page_table = jax.random.permutation(ks[0], n_pool)[:n_used].reshape(DEC_BATCH, n_pages).astype(jnp.int32)
    return {
        'x_prompt': nrm(ks[1], (BATCH, SEQ, D_MODEL), 1.0),
        'x_sample': nrm(ks[2], (DEC_BATCH, DEC_SEQ, D_MODEL), 1.0),
        'cache_k': nrm(ks[3], (DEPTH, n_pool, N_HEADS, PAGE_SIZE, HEAD_DIM), 1.0),
        'cache_v': nrm(ks[4], (DEPTH, n_pool, N_HEADS, PAGE_SIZE, HEAD_DIM), 1.0),
        'state_conv': nrm(ks[5], (DEPTH, DEC_BATCH, CONV_BUF, CONV_WIDTH), 0.5),
        'page_table': page_table,
        'c_prompt': nrm(ks[6], (BATCH, D_MODEL), 1.0),
        'c_sample': nrm(ks[7], (DEC_BATCH, D_MODEL), 1.0),
        'w_ada': nrm(ks[8], (DEPTH, D_MODEL, 6 * D_MODEL), 0.5 * D_MODEL ** -0.5),
        'b_ada': nrm(ks[9], (DEPTH, 6 * D_MODEL), 0.02),
        'g_pre_mix': gain(ks[10], D_MODEL),
        'w_in': nrm(ks[11], (DEPTH, D_MODEL, IN_COLS), D_MODEL ** -0.5),
        'w_dw': nrm(ks[12], (DEPTH, CONV_K, 1, CONV_WIDTH), CONV_K ** -0.5),
        'b_dw': nrm(ks[13], (DEPTH, CONV_WIDTH), 0.02),
        'g_conv_ln': gain(ks[14], CONV_WIDTH),
        'b_conv_ln': nrm(ks[15], (DEPTH, CONV_WIDTH), 0.02),
        'g_attn_out': gain(ks[16], ATTN_WIDTH),
        'g_conv_out': gain(ks[17], CONV_WIDTH),
        'w_out': nrm(ks[18], (DEPTH, MIX_WIDTH, D_MODEL), MIX_WIDTH ** -0.5),
        'g_post_mix': gain(ks[19], D_MODEL),
        'g_pre_ffn': gain(ks[20], D_MODEL),
        'w_gate_up': nrm(ks[21], (DEPTH, D_MODEL, 2 * D_FF), D_MODEL ** -0.5),
        'w_down': nrm(ks[22], (DEPTH, D_FF, D_MODEL), D_FF ** -0.5),
        'g_post_ffn': gain(ks[23], D_MODEL),
    }


def reference(x_prompt, x_sample, cache_k, cache_v, state_conv, page_table, c_prompt, c_sample,
              w_ada, b_ada, g_pre_mix, w_in, w_dw, b_dw, g_conv_ln, b_conv_ln, g_attn_out, g_conv_out,
              w_out, g_post_mix, g_pre_ffn, w_gate_up, w_down, g_post_ffn):
    slopes = alibi_slopes()
    B, S, _ = x_prompt.shape
    y_p, y_s = x_prompt, x_sample
    k_p, v_p, cv_p, k_s, v_s, cv_s = [], [], [], [], [], []
    zero_buf = jnp.zeros((B, CONV_BUF, CONV_WIDTH), x_prompt.dtype)
    to_pages = lambda t: t.reshape(B, N_HEADS, S // PAGE_SIZE, PAGE_SIZE, HEAD_DIM).transpose(0, 2, 1, 3, 4)
    for l in range(DEPTH):
        lw = (w_ada[l], b_ada[l], g_pre_mix[l], w_in[l], w_dw[l], b_dw[l], g_conv_ln[l], b_conv_ln[l],
              g_attn_out[l], g_conv_out[l], w_out[l], g_post_mix[l], g_pre_ffn[l], w_gate_up[l],
              w_down[l], g_post_ffn[l])
        y_p, kh, vh, buf = layer(y_p, c_prompt, zero_buf, functools.partial(moba_prompt, slopes=slopes), *lw)
        k_p.append(to_pages(kh))
        v_p.append(to_pages(vh))
        cv_p.append(buf)
        attend_s = functools.partial(moba_sample, pool_k=cache_k[l], pool_v=cache_v[l],
                                     page_table=page_table, slopes=slopes)
        y_s, kh_s, vh_s, buf_s = layer(y_s, c_sample, state_conv[l], attend_s, *lw)
        k_s.append(kh_s)
        v_s.append(vh_s)
        cv_s.append(buf_s)
    return (y_p, y_s, jnp.stack(k_p), jnp.stack(v_p), jnp.stack(cv_p),
            jnp.stack(k_s), jnp.stack(v_s), jnp.stack(cv_s))
```

```python
import numpy as np
import concourse.bass as bass
import concourse.mybir as mybir

F32 = mybir.dt.float32
BF16 = mybir.dt.bfloat16
I32 = mybir.dt.int32
AF = mybir.ActivationFunctionType
ALU = mybir.AluOpType
AX = mybir.AxisListType

GRAN = 64
NDSEM = 20


def _esize(dt):
    if dt == BF16:
        return 2
    return 4


class Prog:
    def __init__(self, nc):
        self.nc = nc
        self.ins = []
        self.lastw = {}
        self.readers = {}
        self.tbytes = {}
        self.psum = set()
        self.dma_n = {"sp": 0, "pool": 0}
        self.dma_last = {}

    def reg(self, t, nbytes, psum=False):
        self.tbytes[t.name] = nbytes
        if psum:
            self.psum.add(t.name)

    def keys(self, x):
        if isinstance(x, tuple):
            return [x]
        name = x.tensor.name
        pb = self.tbytes.get(name)
        if pb is None or name in self.psum:
            return [(name,)]
        es = _esize(x.dtype)
        off = (x.offset * es) % pb
        span = es
        for (st, cnt) in x.ap[1:]:
            span += abs(st) * (cnt - 1) * es
        g0 = off // GRAN
        g1 = (off + span - 1) // GRAN
        return [(name, g) for g in range(g0, g1 + 1)]

    def add(self, eng, fn, reads=(), writes=(), dma=False):
        idx = len(self.ins)
        deps = set()
        rk = [k for r in reads for k in self.keys(r)]
        wk = [k for w in writes for k in self.keys(w)]
        pk = [k for k in rk if k[0] in self.psum]
        if pk:
            rk = [k for k in rk if k[0] not in self.psum]
            wk = wk + pk
        for k in rk:
            d = self.lastw.get(k)
            if d is not None:
                deps.add(d)
        for k in wk:
            d = self.lastw.get(k)
            if d is not None:
                deps.add(d)
            for r in self.readers.get(k, ()):
                deps.add(r)
        for k in rk:
            self.readers.setdefault(k, []).append(idx)
        for k in wk:
            self.lastw[k] = idx
            self.readers[k] = []
        sem = None
        if dma:
            n = self.dma_n[eng]
            self.dma_n[eng] = n + 1
            sem = (eng, n % NDSEM)
            prev = self.dma_last.get(sem)
            if prev is not None:
                deps.add(prev)
            self.dma_last[sem] = idx
        deps.discard(idx)
        self.ins.append(dict(eng=eng, fn=fn, deps=deps, dma=dma, sig=bool(dma), sem=sem, val=0))
        return idx

    def emit(self, es):
        nc = self.nc
        ins = self.ins
        for i in ins:
            for d in i["deps"]:
                di = ins[d]
                if i["eng"] == "pe" and di["eng"] == "pe" and not di["dma"] and not i["dma"]:
                    continue
                di["sig"] = True
        cnt = {}
        for i in ins:
            if not i["sig"]:
                continue
            if i["dma"]:
                s = i["sem"]
                cnt[s] = cnt.get(s, 0) + 16
            else:
                s = (i["eng"], "c")
                i["sem"] = s
                cnt[s] = cnt.get(s, 0) + 1
            i["val"] = cnt[s]
        semobj = {}
        for s in cnt:
            semobj[s] = es.enter_context(nc.semaphore("s_%s_%s" % s))
        final = dict(cnt)

        def make(engname):
            def body(e):
                waited = {}
                for i in ins:
                    if i["eng"] != engname:
                        continue
                    need = {}
                    for d in sorted(i["deps"]):
                        di = ins[d]
                        if engname == "pe" and di["eng"] == "pe" and not di["dma"] and not i["dma"]:
                            continue
                        s = di["sem"]
                        v = di["val"]
                        if need.get(s, 0) < v:
                            need[s] = v
                    for s, v in need.items():
                        if waited.get(s, 0) < v:
                            e.wait_ge(semobj[s], v)
                            waited[s] = v
                    r = i["fn"](e)
                    if i["sig"]:
                        r.then_inc(semobj[i["sem"]], 16 if i["dma"] else 1)
                if engname == "sp":
                    for s, v in final.items():
                        if s[1] != "c" and waited.get(s, 0) < v:
                            e.wait_ge(semobj[s], v)
            return body

        block = es.enter_context(nc.Block())
        block.tensor(make("pe"))
        block.scalar(make("act"))
        block.vector(make("dve"))
        block.gpsimd(make("pool"))
        block.sync(make("sp"))

    def mm(self, out, lhsT, rhs, start=True, stop=True):
        self.add("pe", lambda e: e.matmul(out, lhsT, rhs, start=start, stop=stop),
                 reads=[lhsT, rhs], writes=[out])

    def tr(self, out, in_, ident):
        self.add("pe", lambda e: e.transpose(out, in_, ident), reads=[in_, ident], writes=[out])

    def act(self, out, in_, func, bias=None, scale=None, accum=None):
        kw = {}
        rd = [in_]
        if bias is not None:
            kw["bias"] = bias
            if not isinstance(bias, (int, float)):
                rd.append(bias)
        if scale is not None:
            kw["scale"] = scale
            if not isinstance(scale, (int, float)):
                rd.append(scale)
        wr = [out]
        if accum is not None:
            kw["accum_out"] = accum
            wr.append(accum)
            self.memset(accum, 0.0)
        self.add("act", lambda e: e.activation(out, in_, func, **kw), reads=rd, writes=wr)

    def tt(self, out, in0, in1, op, eng="dve"):
        self.add(eng, lambda e: e.tensor_tensor(out, in0, in1, op), reads=[in0, in1], writes=[out])

    def ts(self, out, in0, s1, s2=None, op0=ALU.mult, op1=None, eng="dve", accum=None):
        rd = [in0]
        if not isinstance(s1, (int, float)):
            rd.append(s1)
        if s2 is not None and not isinstance(s2, (int, float)):
            rd.append(s2)
        kw = {}
        if op1 is not None:
            kw["op1"] = op1
        wr = [out]
        if accum is not None:
            kw["accum_out"] = accum
            wr.append(accum)
        self.add(eng, lambda e: e.tensor_scalar(out, in0, s1, s2, op0, **kw), reads=rd, writes=wr)

    def stt(self, out, in0, s, in1, op0, op1, eng="dve"):
        rd = [in0, in1]
        if not isinstance(s, (int, float)):
            rd.append(s)
        self.add(eng, lambda e: e.scalar_tensor_tensor(out, in0, s, in1, op0, op1), reads=rd, writes=[out])

    def cp(self, out, in_, eng="dve"):
        if eng == "act":
            self.act(out, in_, AF.Copy)
        else:
            self.add(eng, lambda e: e.tensor_copy(out, in_), reads=[in_], writes=[out])

    def red(self, out, in_, op=ALU.add, eng="dve"):
        self.add(eng, lambda e: e.tensor_reduce(out, in_, AX.X, op), reads=[in_], writes=[out])

    def memset(self, out, v, eng="dve"):
        self.add(eng, lambda e: e.memset(out, v), writes=[out])

    def dma(self, out, in_, q="sp", rk=None, wk=None, **kw):
        rd = [in_] if rk is None else list(rk)
        wr = [out] if wk is None else list(wk)
        self.add(q, lambda e: e.dma_start(out=out, in_=in_, **kw), reads=rd, writes=wr, dma=True)
from contextlib import ExitStack
import ml_dtypes
from concourse.bass_utils import run_bass_kernel_spmd

EPS = 1e-6
SCALE = 128.0 ** -0.5
NEGB = -30000.0


class Cfg:
    D = 2048
    KC = 16
    H = 8
    CC = 8
    FF = 5632
    FC = 44
    T = 256
    DS = 4
    HALO = 32
    MW = 36

    def __init__(self, S=4096, OWN=1024, NPG=128, NPOOL=1280):
        self.S = S
        self.OWN = OWN
        self.NPG = NPG
        self.NPOOL = NPOOL
        self.NKT = S // 128
        self.NBLK = S // 256
        self.NOWN = OWN // 256
        self.NT1 = S // 256
        self.NSB = NPG // 2


PV_GPRE_M, PV_GPRE_F, PV_GATTN, PV_GCONV, PV_GLN, PV_BLN, PV_BDW, PV_WDW = 0, 16, 32, 40, 48, 56, 64, 72
NPV = 72 + 8 * 31


def build(cfg):
    c = cfg
    nc = bass.Bass("TRN2", target_bir_lowering=False)
    P = Prog(nc)
    es = ExitStack()
    T, NKT, S = c.T, c.NKT, c.S

    def din(name, shape, dt=F32):
        return nc.dram_tensor(name, list(shape), dt, kind="ExternalInput")

    def dout(name, shape, dt=F32):
        return nc.dram_tensor(name, list(shape), dt, kind="ExternalOutput")

    ident_f_h = din("ident_f", [128, 128])
    ident_b_h = din("ident_b", [128, 128], BF16)
    cvec_h = din("cvec", [128, 16, 33])
    w_ada_h = din("w_ada", [2048, 12288])
    b_ada_h = din("b_ada33", [33, 12288])
    gpost_h = din("gpost33", [33, 2, 2048])
    pvec_h = din("pvec", [128, NPV])
    w_in_h = din("w_in", [2048, 5120])
    w_out_h = din("w_out", [2048, 2048])
    w_gu_h = din("w_gu", [2048, 11264])
    w_dn_h = din("w_down", [5632, 2048])
    xb_h = din("xb", [S, 2048])
    xown_h = din("xown", [c.OWN, 2048])
    xmini_h = din("xmini", [c.MW, 2048])
    hflag_h = din("haloflag", [128, 1])
    lpast_h = din("lpast", [36, NKT * 128], BF16)
    caus_h = din("caus", [128, 2, 256], BF16)
    rc_h = din("rc", [c.NOWN, 4, 8, 256], BF16)
    vbias_h = din("vbias", [c.OWN, 16])
    v01_h = din("v01", [c.OWN, 16])
    ck_h = din("cache_k", [c.NPOOL * 1024, 128])
    cv_h = din("cache_v", [c.NPOOL * 1024, 128])
    ptab_h = din("ptab", [1, c.NPG], I32)
    ioff_h = din("ioff", [128, 8])
    sconv_h = din("sconv", [30, 1024])
    sbias_h = din("sbias", [128, 8, c.NPG])
    fq_h = din("fq", [128, 8, 4])
    sown_h = din("sown", [4, 8, 4])
    eq_h = din("eqc", [4, 4, 128])

    y_own_h = dout("y_own", [c.OWN, 2048])
    k_own_h = dout("k_own", [c.OWN // 128, 8, 128, 128])
    v_own_h = dout("v_own", [c.OWN // 128, 8, 128, 128])
    convp_h = dout("convp", [30, 1024])
    y_s_h = dout("y_s", [4, 2048])
    k_s_h = dout("k_s", [8, 4, 128])
    v_s_h = dout("v_s", [8, 4, 128])
    conv_s_h = dout("conv_s", [30, 1024])

    kf_h = nc.dram_tensor("kf_scr", [8, 128, S], BF16)
    vt_h = nc.dram_tensor("vt_scr", [S, 1024], BF16)

    def sb(name, shape, dt=F32):
        t = es.enter_context(nc.sbuf_tensor(name, list(shape), dt))
        n = 1
        for s_ in shape[1:]:
            n *= s_
        P.reg(t, n * (2 if dt == BF16 else 4))
        return t

    banks = []
    for i in range(8):
        t = es.enter_context(nc.psum_tensor("pb%d" % i, [128, 512], F32))
        P.reg(t, 2048, psum=True)
        banks.append(t)
    rot = [0]

    def bank():
        b = banks[rot[0] % 4]
        rot[0] += 1
        return b
    BK_O, BK_S, BK_X, BK_Y = banks[4], banks[5], banks[6], banks[7]

    ident_f = sb("identf", [128, 128])
    ident_b = sb("identb", [128, 128], BF16)
    ones_f = sb("onesf", [128, 128])
    ones_b = sb("onesb", [128, 128], BF16)
    epsc = sb("epsc", [128, 1])
    pvec = sb("pvecs", [128, NPV])
    modcol = sb("modcol", [128, 192])
    abv = sb("abv", [128, 2, 2, 16])
    gtrow = sb("gtrow", [33, 2, 2048])
    lpast = sb("lpasts", [36, NKT * 128], BF16)
    caus = sb("causs", [128, 2, 256], BF16)
    rall = sb("rall", [36, 8, 256], BF16)
    kmsum = sb("kmsum", [128, 8, 16])
    hflag = sb("hflag", [128, 1])
    wsl = [sb("wsl%d" % i, [128, 16, 512], BF16) for i in range(2)]
    x1 = sb("x1", [128, 2, 2048])
    xn = sb("xn", [128, 2, 2048], BF16)
    hB = sb("hB", [128, 16, 256], BF16)
    bigA = sb("bigA", [128, 44, 256], BF16)
    f32A = sb("f32A", [128, 8, 288])
    f32B = sb("f32B", [128, 8, 256])
    f32C = sb("f32C", [128, 8, 256])
    scr16 = sb("scr16", [128, 4096])
    ptl = [sb("ptl%d" % i, [128, 256], BF16) for i in range(3)]
    t32 = [sb("t32_%d" % i, [128, 512]) for i in range(4)]
    sml = sb("sml", [128, 64])
    lnm = sb("lnm", [128, 256])
    lnv = sb("lnv", [128, 256])
    sel = sb("sel", [128, 4, 16])
    selb = sb("selb", [128, 16], BF16)
    vb_t = sb("vb_t", [128, 2, 16])
    v01_t = sb("v01_t", [128, 2, 16])
    uhalo = sb("uhalo", [128, 8, 32])
    usamp = sb("usamp", [128, 8, 36])
    cvs = sb("cvs", [128, 16, 33])
    cvb = sb("cvb", [128, 16, 33], BF16)
    qs_b = sb("qs_b", [128, 8, 4], BF16)
    qs_f = sb("qs_f", [128, 8, 4])
    kn_b = sb("kn_b", [128, 8, 4], BF16)
    vn_b = sb("vn_b", [4, 1024], BF16)
    ksum = sb("ksum", [128, 8, c.NPG])
    kms = sb("kms", [128, 8, c.NSB])
    sbias = sb("sbias_s", [128, 8, c.NPG])
    fq = sb("fq_s", [128, 8, 4])
    sown = sb("sown_s", [4, 8, 4])
    eqc = sb("eqc_s", [4, 4, 128])
    ptb_i = sb("ptb_i", [128, c.NPG], I32)
    ptb_f = sb("ptb_f", [128, c.NPG])
    ioff = sb("ioff_s", [128, 8])
    idx_i = sb("idx_i", [128, c.NPG, 8], I32)
    ktb = [sb("ktb%d" % i, [128, 128], BF16) for i in range(2)]
    sexp = [sb("sexp%d" % i, [128, 4], BF16) for i in range(2)]
    oat_s = sb("oat_s", [128, 8, 4])
    ue_s = sb("ue_s", [128, 8, 34])
    y_sm = sb("y_sm", [128, 8, 4])

    mrow = t32[0][0:33, :]
    brow = t32[1][0:33, :]
    grow = t32[2][0:33, :]
    idx_f = f32C[:, :, :].rearrange("p a b -> p (a b)")[:, 0:c.NPG * 8].rearrange("p (j h) -> p j h", h=8)
    cst = scr16[0:30, 0:1024]
    tctr = [0]

    def tmp32():
        t = t32[tctr[0] % 4]
        tctr[0] += 1
        return t
    pctr = [0]

    def ptile():
        t = ptl[pctr[0] % 3]
        pctr[0] += 1
        return t

    P.dma(ident_f[:, :], ident_f_h.ap())
    P.dma(ident_b[:, :], ident_b_h.ap())
    P.memset(ones_f[:, :], 1.0)
    P.memset(ones_b[:, :], 1.0)
    P.memset(epsc[:, :], EPS)
    P.memset(rall[:, :, :], 0.0)
    P.memset(kmsum[:, :, :], 0.0)
    P.dma(pvec[:, :], pvec_h.ap())
    P.dma(lpast[:, :], lpast_h.ap())
    P.dma(caus[:, :, :], caus_h.ap())
    P.dma(hflag[:, :], hflag_h.ap())
    P.dma(cvs[:, :, :], cvec_h.ap())
    P.dma(sbias[:, :, :], sbias_h.ap())
    P.dma(fq[:, :, :], fq_h.ap())
    P.dma(sown[:, :, :], sown_h.ap())
    P.dma(eqc[:, :, :], eq_h.ap())
    P.dma(ioff[:, :], ioff_h.ap())
    P.dma(ptb_i[:, :], ptab_h.ap().partition_broadcast(128))

    wctr = [0]

    def stream(src, kch, cols):
        slot = wsl[wctr[0] % 2]
        wctr[0] += 1
        v = src.rearrange("(kc p) c -> p kc c", p=128)
        for k0 in range(0, kch, 4):
            k1 = min(kch, k0 + 4)
            P.dma(slot[:, k0:k1, 0:cols], v[:, k0:k1, :], q="pool")
        return slot

    import os as _os
    KSUB = int(_os.environ.get("KSUB", "99"))
    if KSUB < 1:
        P.emit(es); es.close(); return nc
    P.act(cvb[:, :, :], cvs[:, :, :], AF.Silu)
    pmc = BK_X
    for t in range(24 if KSUB >= 3 else 1):
        wt = stream(w_ada_h[:, t * 512:(t + 1) * 512], 16, 512)
        pb = bank()
        for kc in range(16):
            P.mm(pb[0:33, :], cvb[:, kc, :], wt[:, kc, :], start=(kc == 0), stop=(kc == 15))
        P.dma(brow[:, :], b_ada_h[:, t * 512:(t + 1) * 512])
        P.tt(mrow[:, :], pb[0:33, :], brow[:, :], ALU.add)
        for ri, r in enumerate((0, 32) if KSUB >= 2 else ()):
            for j in range(4):
                col = ri * 96 + t * 4 + j
                P.mm(pmc[:, col:col + 1], mrow[r:r + 1, j * 128:(j + 1) * 128], ones_f[r:r + 1, 0:1])
        which = {2: 0, 5: 1}.get(t // 4)
        if which is not None:
            cs = slice((t % 4) * 512, (t % 4 + 1) * 512)
            P.dma(grow[:, :], gpost_h[:, which, cs])
            P.tt(gtrow[:, which, cs], mrow[:, :], grow[:, :], ALU.mult)
    P.cp(modcol[:, :], pmc[:, 0:192])
    for ri in range(2):
        o = ri * 96
        P.stt(abv[:, ri, 0, :], modcol[:, o + 16:o + 32], 1.0, pvec[:, PV_GPRE_M:PV_GPRE_M + 16], ALU.add, ALU.mult)
        P.stt(abv[:, ri, 1, :], modcol[:, o + 64:o + 80], 1.0, pvec[:, PV_GPRE_F:PV_GPRE_F + 16], ALU.add, ALU.mult)

    def ab(ri, f, kc):
        o = ri * 96 + (48 if f else 0)
        return abv[:, ri, f, kc:kc + 1], modcol[:, o + kc:o + kc + 1]

    def rstd_from(out, in_, inv_n, M):
        P.act(out, in_, AF.Sqrt, bias=epsc[0:M, 0:1], scale=inv_n)
        P.add("dve", lambda e: e.reciprocal(out, out), reads=[out], writes=[out])

    def prologue(subs, f, colparams):
        for (si, M, coff) in subs:
            ssq = sml[0:M, 0:1]
            P.act(xn[0:M, si, :], x1[0:M, si, :], AF.Square, accum=ssq)
            rs = sml[0:M, 1:2]
            rstd_from(rs, ssq, 1.0 / 2048, M)
            P.ts(xn[0:M, si, :], x1[0:M, si, :], rs, None, op0=ALU.mult)
            for kc in range(16):
                pb = bank()
                pv = pb[:, :].bitcast(BF16)
                P.tr(pv[:, 0:M], xn[0:M, si, kc * 128:(kc + 1) * 128], ident_b[0:M, 0:M])
                for (c0, c1, ri) in colparams:
                    a, b = ab(ri, f, kc)
                    if kc % 2 == 0:
                        P.act(hB[:, kc, coff + c0:coff + c1], pv[:, c0:c1], AF.Identity, bias=b, scale=a)
                    else:
                        P.ts(hB[:, kc, coff + c0:coff + c1], pv[:, c0:c1], a, b, op0=ALU.mult, op1=ALU.add)

    def linear_fm(wt, kch, rhs_kc, N, mch, cb):
        for m in range(mch):
            pb = bank()
            for kc in range(kch):
                P.mm(pb[:, 0:N], wt[:, kc, m * 128:(m + 1) * 128], rhs_kc(kc), start=(kc == 0), stop=(kc == kch - 1))
            cb(m, pb[:, 0:N])

    def fm_sumsq(chunks, N, pacc):
        n = len(chunks)
        for i, ch in enumerate(chunks):
            t = tmp32()
            P.act(t[:, 0:N], ch, AF.Square)
            P.mm(pacc[:, 0:N], ones_f[:, :], t[:, 0:N], start=(i == 0), stop=(i == n - 1))

    def fm_rmsnorm_to(chunks, N, gcol0, dst_of):
        fm_sumsq(chunks, N, BK_X)
        rt = tmp32()
        rstd_from(rt[:, 0:N], BK_X[:, 0:N], 1.0 / (128 * len(chunks)), 128)
        for i, ch in enumerate(chunks):
            P.stt(dst_of(i), ch, pvec[:, gcol0 + i:gcol0 + i + 1], rt[:, 0:N], ALU.mult, ALU.mult)

    def conv_post(ych, N):
        for i in range(8):
            P.mm(BK_X[:, 0:N], ones_f[:, :], ych(i), start=(i == 0), stop=(i == 7))
        fm_sumsq([ych(i) for i in range(8)], N, BK_Y)
        mean = lnm
        P.ts(mean[:, 0:N], BK_X[:, 0:N], 1.0 / 1024, None, op0=ALU.mult)
        msq = tmp32()
        P.tt(msq[:, 0:N], mean[:, 0:N], mean[:, 0:N], ALU.mult)
        var = lnv
        P.stt(var[:, 0:N], BK_Y[:, 0:N], 1.0 / 1024, msq[:, 0:N], ALU.mult, ALU.subtract)
        rstd_from(var[:, 0:N], var[:, 0:N], 1.0, 128)
        for i in range(8):
            y = ych(i)
            P.tt(y, y, mean[:, 0:N], ALU.subtract)
            P.tt(y, y, var[:, 0:N], ALU.mult)
            P.act(y, y, AF.Identity, bias=pvec[:, PV_BLN + i:PV_BLN + i + 1], scale=pvec[:, PV_GLN + i:PV_GLN + i + 1])
            sg = tmp32()
            P.act(sg[:, 0:N], y, AF.Sigmoid)
            P.tt(y, y, sg[:, 0:N], ALU.mult)

    def proj_post(w_h, kch_total, kgrp, lhs_kc, subs, ri, which, dst_cb):
        stages = [f32B[:, :, :].rearrange("p a b -> p (a b)"), f32C[:, :, :].rearrange("p a b -> p (a b)")]
        ngr = (kch_total + kgrp - 1) // kgrp
        r = 32 * ri
        for ct in range(4):
            pbs = [bank() for _ in subs]
            for g in range(ngr):
                k0 = g * kgrp
                kn = min(kgrp, kch_total - k0)
                wt = stream(w_h[k0 * 128:(k0 + kn) * 128, ct * 512:(ct + 1) * 512], kn, 512)
                for (si, M), pb in zip(subs, pbs):
                    for kk in range(kn):
                        kc = k0 + kk
                        P.mm(pb[0:M, :], lhs_kc(kc, si, M), wt[:, kk, :], start=(kc == 0), stop=(kc == kch_total - 1))
            for (si, M), pb in zip(subs, pbs):
                st = stages[si]
                P.cp(st[0:M, ct * 512:(ct + 1) * 512], pb[0:M, :])
                j = tmp32()
                P.act(j[0:M, :], pb[0:M, :], AF.Square, accum=sml[0:M, 8 + si * 4 + ct:9 + si * 4 + ct])
        for (si, M) in subs:
            st = stages[si]
            ss = sml[0:M, 16 + si:17 + si]
            P.red(ss, sml[0:M, 8 + si * 4:12 + si * 4])
            rstd_from(ss, ss, 1.0 / 2048, M)
            for ct in range(4):
                cs = slice(ct * 512, (ct + 1) * 512)
                pg = bank()
                P.mm(pg[0:M, :], ones_f[r:r + 1, 0:M], gtrow[r:r + 1, which, cs])
                tw = tmp32()
                P.stt(tw[0:M, :], st[0:M, cs], ss, pg[0:M, :], ALU.mult, ALU.mult)
                P.tt(st[0:M, cs], x1[0:M, si, cs], tw[0:M, :], ALU.add)
            dst_cb(si, M, st)

    def ffn(subs, colparams, N, ri, out_cb):
        prologue([(si, M, si * 128) for (si, M) in subs], 1, colparams)
        for g in range(11):
            wg = stream(w_gu_h[:, g * 512:(g + 1) * 512], 16, 512)
            wu = stream(w_gu_h[:, 5632 + g * 512:5632 + (g + 1) * 512], 16, 512)
            for m in range(4):
                pg = bank()
                for kc in range(16):
                    P.mm(pg[:, 0:N], wg[:, kc, m * 128:(m + 1) * 128], hB[:, kc, 0:N], start=(kc == 0), stop=(kc == 15))
                P.act(f32A[:, m, 0:N], pg[:, 0:N], AF.Silu)
            for m in range(4):
                pu = bank()
                for kc in range(16):
                    P.mm(pu[:, 0:N], wu[:, kc, m * 128:(m + 1) * 128], hB[:, kc, 0:N], start=(kc == 0), stop=(kc == 15))
                P.tt(bigA[:, g * 4 + m, 0:N], f32A[:, m, 0:N], pu[:, 0:N], ALU.mult)
        proj_post(w_dn_h, 44, 11, lambda kc, si, M: bigA[:, kc, si * 128:si * 128 + M], subs, ri, 1, out_cb)

    def mix_out(subs, N, ri):
        def dst(si, M, st):
            P.cp(x1[0:M, si, :], st[0:M, :])
        proj_post(w_out_h, 16, 16, lambda kc, si, M: bigA[:, 16 + kc, si * 128:si * 128 + M], subs, ri, 0, dst)

    def transpose_out(src_cc, ncols, dst_h):
        for cc in range(8):
            pb = bank()
            P.tr(pb[0:ncols, 0:128], src_cc(cc), ident_f[:, :])
            P.cp(cst[0:ncols, cc * 128:(cc + 1) * 128], pb[0:ncols, 0:128])
        P.dma(dst_h.ap(), cst[0:ncols, :])

    import os as _os
    STAGE = int(_os.environ.get("KSTAGE", "99"))
    kst = bigA[:, 0:8, :]
    vst = bigA[:, 8:16, :].rearrange("p (s a) b -> p s (a b)", s=2)
    KP1 = int(_os.environ.get("KP1", "99"))
    KP1SUB = int(_os.environ.get("KP1SUB", "99"))
    for ti in range(min(KP1, c.NT1) if STAGE >= 1 else 0):
        for si in range(2):
            P.dma(x1[:, si, :], xb_h[ti * 256 + si * 128:ti * 256 + (si + 1) * 128, :])
        prologue([(0, 128, 0), (1, 128, 128)], 0, [(0, 128, 0)])
        if KP1SUB < 1:
            continue
        for half in range(2):
            wt = stream(w_in_h[:, 1024 + half * 512:1024 + (half + 1) * 512], 16, 512)

            KD = int(_os.environ.get("KDBG", "7"))

            def cbk(m, pb, half=half):
                hh = half * 4 + m
                if KD & 2:
                    P.cp(kst[:, hh, :], pb, eng="act")
                if KD & 4:
                    P.red(kmsum[:, hh, ti:ti + 1], pb)
            if KD & 1:
                linear_fm(wt, 16, lambda kc: hB[:, kc, :], 256, 4, cbk)
        if KP1SUB < 2:
            continue
        P.dma(kf_h.ap()[:, :, ti * 256:(ti + 1) * 256].rearrange("h p t -> p h t"), kst, wk=[("kf", ti)])
        for half in range(2):
            wt = stream(w_in_h[:, 2048 + half * 512:2048 + (half + 1) * 512], 16, 512)
            for si in range(2):
                pb = bank()
                for kc in range(16):
                    P.mm(pb[:, :], hB[:, kc, si * 128:(si + 1) * 128], wt[:, kc, :], start=(kc == 0), stop=(kc == 15))
                P.cp(vst[:, si, half * 512:(half + 1) * 512], pb[:, :], eng=("act" if si else "dve"))
        P.dma(vt_h.ap()[ti * 256:(ti + 1) * 256, :].rearrange("(s p) c -> p s c", p=128), vst, wk=[("vt", ti)])
    kmT = sb("kmT", [128, 8, 16])
    P.ts(kmT[:, :, :], kmsum[:, :, :], 1.0 / 256, None, op0=ALU.mult)
    kf_keys = [("kf", i) for i in range(c.NT1)]
    vt_keys = [("vt", i) for i in range(c.NT1)]

    qf = bigA[:, 0:8, :]
    kfm = bigA[:, 8:16, :]
    cat = bigA[:, 16:32, :]
    vown = bigA[:, 32:40, :].rearrange("p (s a) b -> p s (a b)", s=2)
    kvst = [f32B[:, :, :].rearrange("p a b -> p (a b)"), f32C[:, :, :].rearrange("p a b -> p (a b)")]

    def inproj(N, subs, mini, tile_i):
        rhs = lambda kc: hB[:, kc, 0:N]
        for ct in range(4):
            wt = stream(w_in_h[:, 1024 + ct * 512:1024 + (ct + 1) * 512], 16, 512)
            for (si, M) in subs:
                pb = bank()
                for kc in range(16):
                    P.mm(pb[0:M, :], hB[:, kc, si * 128:si * 128 + M], wt[:, kc, :], start=(kc == 0), stop=(kc == 15))
                P.cp(kvst[si][0:M, ct * 512:(ct + 1) * 512], pb[0:M, :], eng=("act" if ct % 2 else "dve"))
        for (si, M) in subs:
            if mini:
                P.dma(k_s_h.ap().rearrange("h t d -> t h d"), kvst[0][0:4, 0:1024].rearrange("t (h d) -> t h d", h=8))
                P.dma(v_s_h.ap().rearrange("h t d -> t h d"), kvst[0][0:4, 1024:2048].rearrange("t (h d) -> t h d", h=8))
                P.cp(vn_b[0:4, :], kvst[0][0:4, 1024:2048])
            else:
                pg = tile_i * 2 + si
                P.dma(k_own_h.ap()[pg].rearrange("h t d -> t h d"), kvst[si][:, 0:1024].rearrange("t (h d) -> t h d", h=8))
                P.dma(v_own_h.ap()[pg].rearrange("h t d -> t h d"), kvst[si][:, 1024:2048].rearrange("t (h d) -> t h d", h=8))
                P.cp(vown[:, si, :], kvst[si][:, 1024:2048])
        for half in range(2):
            wt = stream(w_in_h[:, half * 512:(half + 1) * 512], 16, 512)

            def cbq(m, pb, half=half):
                hh = half * 4 + m
                P.act(qf[:, hh, 0:N], pb, AF.Copy, scale=SCALE)
                if mini:
                    P.ts(qs_f[:, hh, :], pb[:, 0:4], SCALE, None, op0=ALU.mult)
                    P.cp(qs_b[:, hh, :], qf[:, hh, 0:4])
                else:
                    q32 = tmp32()
                    P.ts(q32[:, 0:N], pb, SCALE, None, op0=ALU.mult)
                    select_main(hh, q32)
            linear_fm(wt, 16, rhs, N, 4, cbq)
        for half in range(2):
            wt = stream(w_in_h[:, 1024 + half * 512:1024 + (half + 1) * 512], 16, 512)

            def cbk(m, pb, half=half):
                hh = half * 4 + m
                P.cp(kfm[:, hh, 0:N], pb, eng="act")
                if mini:
                    P.cp(kn_b[:, hh, :], kfm[:, hh, 0:4])
            linear_fm(wt, 16, rhs, N, 4, cbk)
        for half in range(2):
            wt = stream(w_in_h[:, 3072 + half * 512:3072 + (half + 1) * 512], 16, 512)

            def cba(m, pb, half=half):
                P.cp(f32B[:, half * 4 + m, 0:N], pb, eng="act")
            linear_fm(wt, 16, rhs, N, 4, cba)
        for half in range(2):
            wt = stream(w_in_h[:, 4096 + half * 512:4096 + (half + 1) * 512], 16, 512)

            def cbg(m, pb, half=half):
                cc = half * 4 + m
                sg = tmp32()
                P.act(sg[:, 0:N], pb, AF.Sigmoid)
                if mini:
                    P.tt(usamp[:, cc, 0:N], f32B[:, cc, 0:N], sg[:, 0:N], ALU.mult)
                else:
                    P.tt(f32A[:, cc, 32:32 + N], f32B[:, cc, 0:N], sg[:, 0:N], ALU.mult)
            linear_fm(wt, 16, rhs, N, 4, cbg)

    def select_main(hh, q32):
        for si in range(2):
            pb = bank()
            P.mm(pb[:, 0:16], q32[:, si * 128:(si + 1) * 128], kmT[:, hh, :])
            s0 = sel[:, 0, :]
            P.tt(s0, pb[:, 0:16], vb_t[:, si, :], ALU.add)
            P.add("dve", lambda e: e.max(sel[:, 1, 0:8], sel[:, 0, :]), reads=[s0], writes=[sel[:, 1, 0:8]])
            P.ts(sel[:, 2, :], s0, sel[:, 1, 2:3], None, op0=ALU.is_ge)
            P.tt(sel[:, 2, :], sel[:, 2, :], v01_t[:, si, :], ALU.mult)
            P.ts(selb[:, :], sel[:, 2, :], -1.0, -NEGB, op0=ALU.add, op1=ALU.mult)
            pt = bank()
            pv = pt[:, :].bitcast(BF16)
            P.tr(pv[0:16, 0:128], selb[:, :], ident_b[:, :])
            P.cp(rall[0:16, hh, si * 128:(si + 1) * 128], pv[0:16, 0:128], eng="act")

    kA = scr16[:, 0:2048].bitcast(BF16)
    vA = scr16[:, 2048:4096].bitcast(BF16).rearrange("p (k d) -> p k d", d=128)

    def attention_main():
        for hh in range(8):
            P.dma(kA[:, 0:S], kf_h.ap()[hh], rk=kf_keys)
            P.dma(vA[:, 0:NKT, :], vt_h.ap()[:, hh * 128:(hh + 1) * 128].rearrange("(k p) d -> p k d", p=128), rk=vt_keys)
            tot = NKT + 2
            for i in range(tot):
                pS = bank()
                if i < NKT:
                    P.mm(pS[:, 0:256], kA[:, i * 128:(i + 1) * 128], qf[:, hh, :], start=True, stop=False)
                    P.mm(pS[:, 0:256], lpast[0:36, i * 128:(i + 1) * 128], rall[0:36, hh, :], start=False, stop=True)
                    vt_ = vA[:, i, :]
                else:
                    jt = i - NKT
                    P.mm(pS[:, 0:256], kfm[:, hh, jt * 128:(jt + 1) * 128], qf[:, hh, :], start=True, stop=False)
                    P.mm(pS[:, 0:256], lpast[32:35, jt * 128:(jt + 1) * 128], rall[32:35, hh, :], start=False, stop=False)
                    P.mm(pS[:, 0:256], ident_b[:, :], caus[:, jt, :], start=False, stop=True)
                    vt_ = vown[:, jt, hh * 128:(hh + 1) * 128]
                pt = ptile()
                P.act(pt[:, :], pS[:, 0:256], AF.Exp)
                P.mm(BK_O[:, 0:256], vt_, pt[:, :], start=(i == 0), stop=(i == tot - 1))
                P.mm(BK_S[:, 0:256], ones_b[:, :], pt[:, :], start=(i == 0), stop=(i == tot - 1))
            rs = tmp32()
            P.add("dve", lambda e, rs=rs: e.reciprocal(rs[:, 0:256], BK_S[:, 0:256]), reads=[BK_S[:, 0:256]], writes=[rs[:, 0:256]])
            P.tt(f32C[:, hh, :], BK_O[:, 0:256], rs[:, 0:256], ALU.mult)

    O_s = scr16[:, 0:2048].rearrange("p (h n q) -> p h n q", h=8, q=4)
    L_s = scr16[:, 2048:4096].rearrange("p (h n q) -> p h n q", h=8, q=4)
    kpg = [f32B[:, :, :].rearrange("p a b -> p (a b)")[:, 0:1024].rearrange("p (h d) -> p h d", h=8),
           f32B[:, :, :].rearrange("p a b -> p (a b)")[:, 1024:2048].rearrange("p (h d) -> p h d", h=8)]
    vpg = [f32C[:, :, :].rearrange("p a b -> p (a b)")[:, 0:1024].rearrange("p (h d) -> p h d", h=8),
           f32C[:, :, :].rearrange("p a b -> p (a b)")[:, 1024:2048].rearrange("p (h d) -> p h d", h=8)]
    vpb = f32A[:, :, :].rearrange("p a b -> p (a b)")[:, 0:1024].bitcast(BF16).rearrange("p (e h d) -> p e h d", e=2, h=8)

    def attention_sample():
        P.cp(ptb_f[:, :], ptb_i[:, :])
        for hh in range(8):
            P.ts(idx_f[:, :, hh], ptb_f[:, :], 1024.0, ioff[:, hh:hh + 1], op0=ALU.mult, op1=ALU.add)
        P.cp(idx_i[:, :, :], idx_f[:, :, :])
        P.memset(ksum[:, :, :], 0.0)
        for n in range(c.NSB):
            for e_ in range(2):
                j = 2 * n + e_
                for hh in range(8):
                    ix = idx_i[:, j, hh:hh + 1]
                    P.add("pool", lambda e, o=kpg[e_][:, hh, :], ix=ix: e.indirect_dma_start(
                        out=o, out_offset=None, in_=ck_h.ap(), in_offset=bass.IndirectOffsetOnAxis(ap=ix, axis=0)),
                        reads=[ix], writes=[kpg[e_][:, hh, :]], dma=True)
                    P.add("pool", lambda e, o=vpg[e_][:, hh, :], ix=ix: e.indirect_dma_start(
                        out=o, out_offset=None, in_=cv_h.ap(), in_offset=bass.IndirectOffsetOnAxis(ap=ix, axis=0)),
                        reads=[ix], writes=[vpg[e_][:, hh, :]], dma=True)
                P.cp(vpb[:, e_, :, :], vpg[e_][:, :, :])
            for hh in range(8):
                pts = []
                for e_ in range(2):
                    j = 2 * n + e_
                    pT = bank()
                    P.tr(pT[:, 0:128], kpg[e_][:, hh, :], ident_f[:, :])
                    kt_ = ktb[(hh * 2 + e_) % 2]
                    P.act(kt_[:, :], pT[:, 0:128], AF.Copy, accum=ksum[:, hh, j:j + 1])
                    pS = bank()
                    P.mm(pS[:, 0:4], kt_[:, :], qs_b[:, hh, :])
                    se = sexp[e_]
                    P.act(se[:, :], pS[:, 0:4], AF.Exp, bias=sbias[:, hh, j:j + 1])
                    pts.append(se)
                for e_ in range(2):
                    P.mm(BK_O[:, hh * 4:hh * 4 + 4], vpb[:, e_, hh, :], pts[e_][:, :], start=(e_ == 0), stop=(e_ == 1))
                for e_ in range(2):
                    P.mm(BK_S[:, hh * 4:hh * 4 + 4], ones_b[:, :], pts[e_][:, :], start=(e_ == 0), stop=(e_ == 1))
            P.cp(O_s[:, :, n, :], BK_O[:, 0:32].rearrange("p (h q) -> p h q", q=4))
            P.cp(L_s[:, :, n, :], BK_S[:, 0:32].rearrange("p (h q) -> p h q", q=4), eng="act")
        NSB = c.NSB
        P.red(kms[:, :, :], ksum[:, :, :].rearrange("p h (n e) -> p h n e", e=2))
        P.ts(kms[:, :, :], kms[:, :, :], 1.0 / 256, None, op0=ALU.mult)
        ncol = max(NSB, 8)
        for hh in range(8):
            pb = bank()
            P.mm(pb[0:4, 0:NSB], qs_f[:, hh, :], kms[:, hh, :])
            P.memset(sel[0:4, :, :], -1e9)
            sv = sel[0:4, :, :].rearrange("p a b -> p (a b)")
            P.cp(sv[:, 0:NSB], pb[0:4, 0:NSB])
            P.add("dve", lambda e, sv=sv: e.max(sml[0:4, 32:40], sv[:, 0:64]), reads=[sv[:, 0:64]], writes=[sml[0:4, 32:40]])
            m01 = tmp32()
            P.ts(m01[0:4, 0:NSB], sv[:, 0:NSB], sml[0:4, 34:35], None, op0=ALU.is_ge)
            pB = bank()
            for q in range(4):
                P.mm(pB[:, q * NSB:(q + 1) * NSB], eqc[0:4, q, :], m01[0:4, 0:NSB])
            pBv = pB[:, 0:4 * NSB].rearrange("p (q n) -> p q n", q=4)
            tm = tmp32()
            tmv = tm[:, 0:4 * NSB].rearrange("p (q n) -> p q n", q=4)
            P.tt(tmv, O_s[:, hh, 0:NSB, :].rearrange("p n q -> p q n"), pBv, ALU.mult)
            P.red(sml[:, 40:44], tmv)
            tm2 = tmp32()
            tmv2 = tm2[:, 0:4 * NSB].rearrange("p (q n) -> p q n", q=4)
            P.tt(tmv2, L_s[:, hh, 0:NSB, :].rearrange("p n q -> p q n"), pBv, ALU.mult)
            P.red(sml[:, 44:48], tmv2)
            pS = bank()
            P.mm(pS[0:4, 0:4], kn_b[:, hh, :], qs_b[:, hh, :])
            so = tmp32()
            P.tt(so[0:4, 0:4], pS[0:4, 0:4], sown[:, hh, :], ALU.add)
            P.act(sexp[0][0:4, 0:4], so[0:4, 0:4], AF.Exp)
            P.mm(BK_O[:, 0:4], vn_b[0:4, hh * 128:(hh + 1) * 128], sexp[0][0:4, 0:4])
            P.mm(BK_S[:, 0:4], ones_b[0:4, :], sexp[0][0:4, 0:4])
            P.tt(sml[:, 40:44], sml[:, 40:44], fq[:, hh, :], ALU.mult)
            P.tt(sml[:, 44:48], sml[:, 44:48], fq[:, hh, :], ALU.mult)
            P.tt(sml[:, 40:44], sml[:, 40:44], BK_O[:, 0:4], ALU.add)
            P.tt(sml[:, 44:48], sml[:, 44:48], BK_S[:, 0:4], ALU.add)
            P.add("dve", lambda e: e.reciprocal(sml[:, 44:48], sml[:, 44:48]), reads=[sml[:, 44:48]], writes=[sml[:, 44:48]])
            P.tt(oat_s[:, hh, :], sml[:, 40:44], sml[:, 44:48], ALU.mult)

    def finish():
        P.emit(es)
        es.close()
        return nc
    if STAGE < 2:
        return finish()
    P.dma(x1[0:c.MW, 0, :], xmini_h.ap())
    prologue([(0, c.MW, 0)], 0, [(0, 4, 1), (4, c.MW, 0)])
    inproj(c.MW, [(0, 4)], True, 0)
    for cc in range(8):
        P.ts(uhalo[:, cc, :], usamp[:, cc, 4:36], hflag[:, 0:1], None, op0=ALU.mult)
    if STAGE < 3:
        return finish()
    P.dma(cst[0:30, :], sconv_h.ap())
    for cc in range(8):
        pb = bank()
        P.tr(pb[:, 0:30], cst[0:30, cc * 128:(cc + 1) * 128], ident_f[0:30, 0:30])
        P.cp(ue_s[:, cc, 0:30], pb[:, 0:30])
        P.cp(ue_s[:, cc, 30:34], usamp[:, cc, 0:4])
    for cc in range(8):
        for t_ in range(4):
            tw = tmp32()
            P.tt(tw[:, 0:31], pvec[:, PV_WDW + cc * 31:PV_WDW + (cc + 1) * 31], ue_s[:, cc, t_:t_ + 31], ALU.mult)
            P.red(y_sm[:, cc, t_:t_ + 1], tw[:, 0:31])
        P.ts(y_sm[:, cc, :], y_sm[:, cc, :], pvec[:, PV_BDW + cc:PV_BDW + cc + 1], None, op0=ALU.add)
    transpose_out(lambda cc: ue_s[:, cc, 4:34], 30, conv_s_h)
    if STAGE < 4:
        return finish()
    attention_sample()
    if STAGE < 5:
        return finish()
    fm_rmsnorm_to([oat_s[:, hh, :] for hh in range(8)], 4, PV_GATTN, lambda i: cat[:, i, 0:4])
    conv_post(lambda cc: y_sm[:, cc, :], 4)
    fm_rmsnorm_to([y_sm[:, cc, :] for cc in range(8)], 4, PV_GCONV, lambda i: cat[:, 8 + i, 0:4])
    mix_out([(0, 4)], 4, 1)

    def out_s(si, M, st):
        P.dma(y_s_h.ap(), st[0:4, :])
    ffn([(0, 4)], [(0, 4, 1)], 4, 1, out_s)

    if STAGE < 6:
        return finish()
    for it in range(c.NOWN):
        for si in range(2):
            r0 = it * 256 + si * 128
            P.dma(x1[:, si, :], xown_h[r0:r0 + 128, :])
            P.dma(vb_t[:, si, :], vbias_h[r0:r0 + 128, :])
            P.dma(v01_t[:, si, :], v01_h[r0:r0 + 128, :])
        P.dma(rall[32:36, :, :], rc_h[it])
        subs = [(0, 128), (1, 128)]
        prologue([(0, 128, 0), (1, 128, 128)], 0, [(0, 128, 0)])
        inproj(256, subs, False, it)
        attention_main()
        fm_rmsnorm_to([f32C[:, hh, :] for hh in range(8)], 256, PV_GATTN, lambda i: cat[:, i, :])
        for cc in range(8):
            P.cp(f32A[:, cc, 0:32], uhalo[:, cc, :], eng="act")
        for cc in range(8):
            eng = "dve"
            y = f32B[:, cc, :]
            w0 = PV_WDW + cc * 31
            P.ts(y, f32A[:, cc, 2:258], pvec[:, w0:w0 + 1], pvec[:, PV_BDW + cc:PV_BDW + cc + 1], op0=ALU.mult, op1=ALU.add, eng=eng)
            for j in range(1, 31):
                P.stt(y, f32A[:, cc, 2 + j:258 + j], pvec[:, w0 + j:w0 + j + 1], y, ALU.mult, ALU.add, eng=eng)
        if it == c.NOWN - 1:
            transpose_out(lambda cc: f32A[:, cc, 258:288], 30, convp_h)
        for cc in range(8):
            P.cp(uhalo[:, cc, :], f32A[:, cc, 256:288], eng="act")
        conv_post(lambda cc: f32B[:, cc, :], 256)
        fm_rmsnorm_to([f32B[:, cc, :] for cc in range(8)], 256, PV_GCONV, lambda i: cat[:, 8 + i, :])
        mix_out(subs, 256, 0)

        def out_m(si, M, st, it=it):
            r0 = it * 256 + si * 128
            P.dma(y_own_h.ap()[r0:r0 + 128, :], st[:, :])
        ffn(subs, [(0, 128, 0)], 256, 0, out_m)

    P.emit(es)
    es.close()
    return nc


def _bf(a):
    return np.asarray(a, dtype=np.float32).astype(ml_dtypes.bfloat16)


def host_consts(cfg, qstart):
    c = cfg
    m = (2.0 ** (-8.0 * (np.arange(8) + 1.0) / 8)).astype(np.float64)
    NKT = c.NKT
    lp = np.zeros((36, NKT * 128), np.float32)
    for kt in range(NKT):
        sl = slice(kt * 128, (kt + 1) * 128)
        if kt // 2 < 16:
            lp[kt // 2, sl] = 1.0
        lp[32, sl] = 1.0
        lp[33, sl] = np.arange(128)
        lp[34, sl] = 128.0 * kt
        lp[35, sl] = 1.0
    j = np.arange(256)
    caus = np.zeros((128, 2, 256), np.float32)
    for jt in range(2):
        kidx = 128 * jt + np.arange(128)
        caus[:, jt, :] = np.where(kidx[:, None] <= j[None, :], 0.0, NEGB)
    rc = np.zeros((c.NOWN, 4, 8, 256), np.float32)
    for it in range(c.NOWN):
        q0 = qstart + it * 256
        for h in range(8):
            rc[it, 0, h] = -m[h] * j
            rc[it, 1, h] = m[h]
            rc[it, 2, h] = m[h]
            rc[it, 3, h] = -m[h] * q0
    tq = qstart + np.arange(c.OWN)
    nfull = tq // 256
    v01 = (np.arange(16)[None, :] < nfull[:, None]).astype(np.float32)
    vbias = np.where(v01 > 0, 0.0, -1e9).astype(np.float32)
    past = c.NPG * 128
    sbias = np.zeros((128, 8, c.NPG), np.float32)
    for h in range(8):
        for jp in range(c.NPG):
            sbias[:, h, jp] = m[h] * (128 * jp + np.arange(128) - past)
    fq = np.zeros((128, 8, 4), np.float32)
    sown = np.zeros((4, 8, 4), np.float32)
    for h in range(8):
        for q in range(4):
            fq[:, h, q] = np.exp(-m[h] * q)
            for k in range(4):
                sown[k, h, q] = -m[h] * (q - k) if k <= q else NEGB
    eqc = np.zeros((4, 4, 128), np.float32)
    for q in range(4):
        eqc[q, q, :] = 1.0
    ioff = (np.arange(128)[:, None] + 128.0 * np.arange(8)[None, :]).astype(np.float32)
    return dict(lpast=_bf(lp), caus=_bf(caus), rc=_bf(rc), vbias=vbias, v01=v01, sbias=sbias, fq=fq,
                sown=sown, eqc=eqc, ioff=ioff, ident_f=np.eye(128, dtype=np.float32), ident_b=_bf(np.eye(128)))


def core_inputs(cfg, inp, b, qstart, sb_):
    c = cfg
    f = np.float32
    d = host_consts(c, qstart)
    col = lambda v: np.ascontiguousarray(np.asarray(v, f).reshape(-1, 128).T)
    pv = np.zeros((128, NPV), f)
    pv[:, PV_GPRE_M:PV_GPRE_M + 16] = col(inp["g_pre_mix"][0])
    pv[:, PV_GPRE_F:PV_GPRE_F + 16] = col(inp["g_pre_ffn"][0])
    pv[:, PV_GATTN:PV_GATTN + 8] = col(inp["g_attn_out"][0])
    pv[:, PV_GCONV:PV_GCONV + 8] = col(inp["g_conv_out"][0])
    pv[:, PV_GLN:PV_GLN + 8] = col(inp["g_conv_ln"][0])
    pv[:, PV_BLN:PV_BLN + 8] = col(inp["b_conv_ln"][0])
    pv[:, PV_BDW:PV_BDW + 8] = col(inp["b_dw"][0])
    wdw = np.asarray(inp["w_dw"], f)[0, :, 0, :]
    pv[:, PV_WDW:] = wdw.reshape(31, 8, 128).transpose(2, 1, 0).reshape(128, 8 * 31)
    d["pvec"] = pv
    cv = np.zeros((128, 16, 33), f)
    cv[:, :, 0] = col(inp["c_prompt"][b])
    cv[:, :, 32] = col(inp["c_sample"][sb_])
    d["cvec"] = cv
    d["w_ada"] = np.asarray(inp["w_ada"], f)[0]
    ba = np.zeros((33, 12288), f)
    ba[0] = inp["b_ada"][0]
    ba[32] = inp["b_ada"][0]
    d["b_ada33"] = ba
    gp = np.zeros((33, 2, 2048), f)
    for r in (0, 32):
        gp[r, 0] = inp["g_post_mix"][0]
        gp[r, 1] = inp["g_post_ffn"][0]
    d["gpost33"] = gp
    d["w_in"] = np.asarray(inp["w_in"], f)[0]
    d["w_out"] = np.asarray(inp["w_out"], f)[0]
    d["w_gu"] = np.asarray(inp["w_gate_up"], f)[0]
    d["w_down"] = np.asarray(inp["w_down"], f)[0]
    xp = np.asarray(inp["x_prompt"], f)
    d["xb"] = xp[b]
    d["xown"] = xp[b, qstart:qstart + c.OWN]
    xm = np.zeros((c.MW, 2048), f)
    xm[0:4] = np.asarray(inp["x_sample"], f)[sb_]
    if qstart > 0:
        xm[4:36] = xp[b, qstart - 32:qstart]
    d["xmini"] = xm
    d["haloflag"] = np.full((128, 1), 1.0 if qstart > 0 else 0.0, f)
    d["cache_k"] = np.asarray(inp["cache_k"], f)[0].reshape(-1, 128)
    d["cache_v"] = np.asarray(inp["cache_v"], f)[0].reshape(-1, 128)
    d["ptab"] = np.asarray(inp["page_table"], np.int32)[sb_:sb_ + 1]
    d["sconv"] = np.asarray(inp["state_conv"], f)[0, sb_]
    return d


_NC_CACHE = {}


def run_cores(cfg, inp, assign):
    key = (cfg.S, cfg.OWN, cfg.NPG, cfg.NPOOL)
    if key not in _NC_CACHE:
        _NC_CACHE[key] = build(cfg)
    nc = _NC_CACHE[key]
    maps = [core_inputs(cfg, inp, b, q, s) for (b, q, s) in assign]
    res = run_bass_kernel_spmd(nc, maps, core_ids=list(range(len(assign))))
    return res.results


def kernel(**inp):
    cfg = Cfg()
    assign = [(cid // 4, (cid % 4) * 1024, cid) for cid in range(8)]
    r = run_cores(cfg, inp, assign)
    f = np.float32
    y_p = np.zeros((2, 4096, 2048), f)
    k_p = np.zeros((1, 2, 32, 8, 128, 128), f)
    v_p = np.zeros((1, 2, 32, 8, 128, 128), f)
    cv_p = np.zeros((1, 2, 30, 1024), f)
    y_s = np.zeros((8, 4, 2048), f)
    k_s = np.zeros((1, 8, 8, 4, 128), f)
    v_s = np.zeros((1, 8, 8, 4, 128), f)
    cv_s = np.zeros((1, 8, 30, 1024), f)
    for cid, (b, q, s) in enumerate(assign):
        o = r[cid]
        y_p[b, q:q + 1024] = o["y_own"]
        k_p[0, b, q // 128:q // 128 + 8] = o["k_own"]
        v_p[0, b, q // 128:q // 128 + 8] = o["v_own"]
        if q == 3072:
            cv_p[0, b] = o["convp"]
        y_s[s] = o["y_s"]
        k_s[0, s] = o["k_s"]
        v_s[0, s] = o["v_s"]
        cv_s[0, s] = o["conv_s"]
    return (y_p, y_s, k_p, v_p, cv_p, k_s, v_s, cv_s)
```

```python
import numpy as np
import concourse.bass as bass
import concourse.mybir as mybir

F32 = mybir.dt.float32
BF16 = mybir.dt.bfloat16
I32 = mybir.dt.int32
AF = mybir.ActivationFunctionType
ALU = mybir.AluOpType
AX = mybir.AxisListType

GRAN = 64
NDSEM = 20


def _esize(dt):
    if dt == BF16:
        return 2
    return 4


class Prog:
    def __init__(self, nc):
        self.nc = nc
        self.ins = []
        self.lastw = {}
        self.readers = {}
        self.tbytes = {}
        self.psum = set()
        self.dma_n = {"sp": 0, "pool": 0}
        self.dma_last = {}

    def reg(self, t, nbytes, psum=False):
        self.tbytes[t.name] = nbytes
        if psum:
            self.psum.add(t.name)

    def keys(self, x):
        if isinstance(x, tuple):
            return [x]
        name = x.tensor.name
        pb = self.tbytes.get(name)
        if pb is None or name in self.psum:
            return [(name,)]
        es = _esize(x.dtype)
        off = (x.offset * es) % pb
        span = es
        for (st, cnt) in x.ap[1:]:
            span += abs(st) * (cnt - 1) * es
        g0 = off // GRAN
        g1 = (off + span - 1) // GRAN
        return [(name, g) for g in range(g0, g1 + 1)]

    def add(self, eng, fn, reads=(), writes=(), dma=False):
        idx = len(self.ins)
        deps = set()
        rk = [k for r in reads for k in self.keys(r)]
        wk = [k for w in writes for k in self.keys(w)]
        pk = [k for k in rk if k[0] in self.psum]
        if pk:
            rk = [k for k in rk if k[0] not in self.psum]
            wk = wk + pk
        for k in rk:
            d = self.lastw.get(k)
            if d is not None:
                deps.add(d)
        for k in wk:
            d = self.lastw.get(k)
            if d is not None:
                deps.add(d)
            for r in self.readers.get(k, ()):
                deps.add(r)
        for k in rk:
            self.readers.setdefault(k, []).append(idx)
        for k in wk:
            self.lastw[k] = idx
            self.readers[k] = []
        sem = None
        if dma:
            n = self.dma_n[eng]
            self.dma_n[eng] = n + 1
            sem = (eng, n % NDSEM)
            prev = self.dma_last.get(sem)
            if prev is not None:
                deps.add(prev)
            self.dma_last[sem] = idx
        deps.discard(idx)
        self.ins.append(dict(eng=eng, fn=fn, deps=deps, dma=dma, sig=bool(dma), sem=sem, val=0))
        return idx

    def emit(self, es):
        nc = self.nc
        ins = self.ins
        for i in ins:
            for d in i["deps"]:
                di = ins[d]
                if i["eng"] == "pe" and di["eng"] == "pe" and not di["dma"] and not i["dma"]:
                    continue
                di["sig"] = True
        cnt = {}
        for i in ins:
            if not i["sig"]:
                continue
            if i["dma"]:
                s = i["sem"]
                cnt[s] = cnt.get(s, 0) + 16
            else:
                s = (i["eng"], "c")
                i["sem"] = s
                cnt[s] = cnt.get(s, 0) + 1
            i["val"] = cnt[s]
        semobj = {}
        for s in cnt:
            semobj[s] = es.enter_context(nc.semaphore("s_%s_%s" % s))
        final = dict(cnt)

        def make(engname):
            def body(e):
                waited = {}
                for i in ins:
                    if i["eng"] != engname:
                        continue
                    need = {}
                    for d in sorted(i["deps"]):
                        di = ins[d]
                        if engname == "pe" and di["eng"] == "pe" and not di["dma"] and not i["dma"]:
                            continue
                        s = di["sem"]
                        v = di["val"]
                        if need.get(s, 0) < v:
                            need[s] = v
                    for s, v in need.items():
                        if waited.get(s, 0) < v:
                            e.wait_ge(semobj[s], v)
                            waited[s] = v
                    r = i["fn"](e)
                    if i["sig"]:
                        r.then_inc(semobj[i["sem"]], 16 if i["dma"] else 1)
                if engname == "sp":
                    for s, v in final.items():
                        if s[1] != "c" and waited.get(s, 0) < v:
                            e.wait_ge(semobj[s], v)
            return body

        block = es.enter_context(nc.Block())
        block.tensor(make("pe"))
        block.scalar(make("act"))
        block.vector(make("dve"))
        block.gpsimd(make("pool"))
        block.sync(make("sp"))

    def mm(self, out, lhsT, rhs, start=True, stop=True):
        self.add("pe", lambda e: e.matmul(out, lhsT, rhs, start=start, stop=stop),
                 reads=[lhsT, rhs], writes=[out])

    def tr(self, out, in_, ident):
        self.add("pe", lambda e: e.transpose(out, in_, ident), reads=[in_, ident], writes=[out])

    def act(self, out, in_, func, bias=None, scale=None, accum=None):
        kw = {}
        rd = [in_]
        if bias is not None:
            kw["bias"] = bias
            if not isinstance(bias, (int, float)):
                rd.append(bias)
        if scale is not None:
            kw["scale"] = scale
            if not isinstance(scale, (int, float)):
                rd.append(scale)
        wr = [out]
        if accum is not None:
            kw["accum_out"] = accum
            wr.append(accum)
            self.memset(accum, 0.0)
        self.add("act", lambda e: e.activation(out, in_, func, **kw), reads=rd, writes=wr)

    def tt(self, out, in0, in1, op, eng="dve"):
        self.add(eng, lambda e: e.tensor_tensor(out, in0, in1, op), reads=[in0, in1], writes=[out])

    def ts(self, out, in0, s1, s2=None, op0=ALU.mult, op1=None, eng="dve", accum=None):
        rd = [in0]
        if not isinstance(s1, (int, float)):
            rd.append(s1)
        if s2 is not None and not isinstance(s2, (int, float)):
            rd.append(s2)
        kw = {}
        if op1 is not None:
            kw["op1"] = op1
        wr = [out]
        if accum is not None:
            kw["accum_out"] = accum
            wr.append(accum)
        self.add(eng, lambda e: e.tensor_scalar(out, in0, s1, s2, op0, **kw), reads=rd, writes=wr)

    def stt(self, out, in0, s, in1, op0, op1, eng="dve"):
        rd = [in0, in1]
        if not isinstance(s, (int, float)):
            rd.append(s)
        self.add(eng, lambda e: e.scalar_tensor_tensor(out, in0, s, in1, op0, op1), reads=rd, writes=[out])

    def cp(self, out, in_, eng="dve"):
        if eng == "act":
            self.act(out, in_, AF.Copy)
        else:
            self.add(eng, lambda e: e.tensor_copy(out, in_), reads=[in_], writes=[out])

    def red(self, out, in_, op=ALU.add, eng="dve", axis=AX.X):
        self.add(eng, lambda e: e.tensor_reduce(out, in_, axis, op), reads=[in_], writes=[out])

    def memset(self, out, v, eng="dve"):
        self.add(eng, lambda e: e.memset(out, v), writes=[out])

    def dma(self, out, in_, q="sp", rk=None, wk=None, **kw):
        rd = [in_] if rk is None else list(rk)
        wr = [out] if wk is None else list(wk)
        self.add(q, lambda e: e.dma_start(out=out, in_=in_, **kw), reads=rd, writes=wr, dma=True)
from contextlib import ExitStack
import ml_dtypes
from concourse.bass_utils import run_bass_kernel_spmd

EPS = 1e-6
SCALE = 128.0 ** -0.5
NEGB = -30000.0


class Cfg:
    D = 2048
    KC = 16
    H = 8
    CC = 8
    FF = 5632
    FC = 44
    T = 256
    DS = 4
    HALO = 32
    MW = 36

    def __init__(self, S=4096, OWN=1024, NPG=128, NPOOL=1280):
        self.S = S
        self.OWN = OWN
        self.NPG = NPG
        self.NPOOL = NPOOL
        self.NKT = S // 128
        self.NBLK = S // 256
        self.NOWN = OWN // 256
        self.NT1 = S // 256
        self.NSB = NPG // 2


PV_GPRE_M, PV_GPRE_F, PV_GATTN, PV_GCONV, PV_GLN, PV_BLN, PV_BDW, PV_WDW = 0, 16, 32, 40, 48, 56, 64, 72
NPV = 72 + 8 * 31


def build(cfg):
    c = cfg
    nc = bass.Bass("TRN2", target_bir_lowering=False)
    P = Prog(nc)
    es = ExitStack()
    T, NKT, S = c.T, c.NKT, c.S

    def din(name, shape, dt=F32):
        return nc.dram_tensor(name, list(shape), dt, kind="ExternalInput")

    def dout(name, shape, dt=F32):
        return nc.dram_tensor(name, list(shape), dt, kind="ExternalOutput")

    ident_f_h = din("ident_f", [128, 128])
    ident_b_h = din("ident_b", [128, 128], BF16)
    cvec_h = din("cvec", [128, 16, 33])
    w_ada_h = din("w_ada", [2048, 12288])
    b_ada_h = din("b_ada33", [33, 12288])
    gpost_h = din("gpost33", [33, 2, 2048])
    pvec_h = din("pvec", [128, NPV])
    w_in_h = din("w_in", [2048, 5120])
    w_out_h = din("w_out", [2048, 2048])
    w_gu_h = din("w_gu", [2048, 11264])
    w_dn_h = din("w_down", [5632, 2048])
    xb_h = din("xb", [S, 2048])
    xown_h = din("xown", [c.OWN, 2048])
    xmini_h = din("xmini", [c.MW, 2048])
    hflag_h = din("haloflag", [128, 1])
    lpast_h = din("lpast", [36, NKT * 128], BF16)
    caus_h = din("caus", [128, 2, 256], BF16)
    rc_h = din("rc", [c.NOWN, 4, 8, 256], BF16)
    vbias_h = din("vbias", [c.OWN, 16])
    v01_h = din("v01", [c.OWN, 16])
    ck_h = din("cache_k", [c.NPOOL * 128, 1024])
    cv_h = din("cache_v", [c.NPOOL * 128, 1024])
    ptab_h = din("ptab", [1, c.NPG], I32)
    ioff_h = din("ioff", [128, 8])
    sconv_h = din("sconv", [30, 1024])
    sbias_h = din("sbias", [128, c.NPG, 8])
    bmask_h = din("bmask", [128, 32])
    fq_h = din("fq", [128, 8, 4])
    sown_h = din("sown", [4, 8, 4])
    eq_h = din("eqc", [4, 4, 128])

    y_own_h = dout("y_own", [c.OWN, 2048])
    k_own_h = dout("k_own", [c.OWN // 128, 8, 128, 128])
    v_own_h = dout("v_own", [c.OWN // 128, 8, 128, 128])
    convp_h = dout("convp", [30, 1024])
    y_s_h = dout("y_s", [4, 2048])
    k_s_h = dout("k_s", [8, 4, 128])
    v_s_h = dout("v_s", [8, 4, 128])
    conv_s_h = dout("conv_s", [30, 1024])

    kf_h = nc.dram_tensor("kf_scr", [8, 128, S], BF16)
    vt_h = nc.dram_tensor("vt_scr", [S, 1024], BF16)

    def sb(name, shape, dt=F32):
        t = es.enter_context(nc.sbuf_tensor(name, list(shape), dt))
        n = 1
        for s_ in shape[1:]:
            n *= s_
        P.reg(t, n * (2 if dt == BF16 else 4))
        return t

    banks = []
    for i in range(8):
        t = es.enter_context(nc.psum_tensor("pb%d" % i, [128, 512], F32))
        P.reg(t, 2048, psum=True)
        banks.append(t)
    rot = [0]

    def bank():
        b = banks[rot[0] % 4]
        rot[0] += 1
        return b
    BK_O, BK_S, BK_X, BK_Y = banks[4], banks[5], banks[6], banks[7]

    ident_f = sb("identf", [128, 128])
    ident_b = sb("identb", [128, 128], BF16)
    ones_f = sb("onesf", [128, 128])
    ones_b = sb("onesb", [128, 128], BF16)
    epsc = sb("epsc", [128, 1])
    pvec = sb("pvecs", [128, NPV])
    modcol = sb("modcol", [128, 192])
    abv = sb("abv", [128, 2, 2, 16])
    gtrow = sb("gtrow", [33, 2, 2048])
    lpast = sb("lpasts", [36, NKT * 128], BF16)
    caus = sb("causs", [128, 2, 256], BF16)
    rall = sb("rall", [36, 8, 256], BF16)
    kmsum = sb("kmsum", [128, 8, 16])
    hflag = sb("hflag", [128, 1])
    wsl = [sb("wsl%d" % i, [128, 16, 512], BF16) for i in range(2)]
    x1 = sb("x1", [128, 2, 2048])
    xn = sb("xn", [128, 2, 2048], BF16)
    hB = sb("hB", [128, 16, 256], BF16)
    bigA = sb("bigA", [128, 44, 256], BF16)
    f32A = sb("f32A", [128, 8, 288])
    f32B = sb("f32B", [128, 8, 256])
    f32C = sb("f32C", [128, 8, 256])
    scr16 = sb("scr16", [128, 4096])
    ptl = [sb("ptl%d" % i, [128, 256], BF16) for i in range(3)]
    t32 = [sb("t32_%d" % i, [128, 512]) for i in range(4)]
    sml = sb("sml", [128, 64])
    lnm = sb("lnm", [128, 256])
    lnv = sb("lnv", [128, 256])
    sel = sb("sel", [128, 4, 16])
    selb = sb("selb", [128, 16], BF16)
    vb_t = sb("vb_t", [128, 2, 16])
    v01_t = sb("v01_t", [128, 2, 16])
    uhalo = sb("uhalo", [128, 8, 32])
    usamp = sb("usamp", [128, 8, 36])
    cvs = sb("cvs", [128, 16, 33])
    cvb = sb("cvb", [128, 16, 33], BF16)
    qs_b = sb("qs_b", [128, 8, 4], BF16)
    qs_f = sb("qs_f", [128, 8, 4])
    kn_b = sb("kn_b", [128, 8, 4], BF16)
    vn_b = sb("vn_b", [4, 1024], BF16)
    ksum = sb("ksum", [128, 8, c.NPG])
    kms = sb("kms", [128, 8, c.NSB])
    sbias = sb("sbias_s", [128, c.NPG, 8])
    bmask = sb("bmask_s", [128, 32])
    KT = [sb("KT%d" % i, [128, 8, 128], BF16) for i in range(2)]
    Pb = [sb("Pb%d" % i, [128, 8, 32], BF16) for i in range(2)]
    fq = sb("fq_s", [128, 8, 4])
    sown = sb("sown_s", [4, 8, 4])
    eqc = sb("eqc_s", [4, 4, 128])
    ptb_i = sb("ptb_i", [128, c.NPG], I32)
    ptb_f = sb("ptb_f", [128, c.NPG])
    ioff = sb("ioff_s", [128, 8])
    idx_i = sb("idx_i", [128, c.NPG], I32)
    sexp = [sb("sexp%d" % i, [128, 4], BF16) for i in range(2)]
    oat_s = sb("oat_s", [128, 8, 4])
    ue_s = sb("ue_s", [128, 8, 34])
    y_sm = sb("y_sm", [128, 8, 4])

    mrow = t32[0][0:33, :]
    brow = t32[1][0:33, :]
    grow = t32[2][0:33, :]
    idx_f = f32C[:, :, :].rearrange("p a b -> p (a b)")[:, 0:c.NPG]
    cst = scr16[0:30, 0:1024]
    tctr = [0]

    def tmp32():
        t = t32[tctr[0] % 4]
        tctr[0] += 1
        return t
    pctr = [0]

    def ptile():
        t = ptl[pctr[0] % 3]
        pctr[0] += 1
        return t

    P.dma(ident_f[:, :], ident_f_h.ap())
    P.dma(ident_b[:, :], ident_b_h.ap())
    P.memset(ones_f[:, :], 1.0)
    P.memset(ones_b[:, :], 1.0)
    P.memset(epsc[:, :], EPS)
    P.memset(rall[:, :, :], 0.0)
    P.memset(kmsum[:, :, :], 0.0)
    P.dma(pvec[:, :], pvec_h.ap())
    P.dma(lpast[:, :], lpast_h.ap())
    P.dma(caus[:, :, :], caus_h.ap())
    P.dma(hflag[:, :], hflag_h.ap())
    P.dma(cvs[:, :, :], cvec_h.ap())
    P.dma(sbias[:, :, :], sbias_h.ap())
    P.dma(bmask[:, :], bmask_h.ap())
    P.dma(fq[:, :, :], fq_h.ap())
    P.dma(sown[:, :, :], sown_h.ap())
    P.dma(eqc[:, :, :], eq_h.ap())
    P.dma(ioff[:, :], ioff_h.ap())
    P.dma(ptb_i[:, :], ptab_h.ap().partition_broadcast(128))

    wctr = [0]

    def stream(src, kch, cols):
        slot = wsl[wctr[0] % 2]
        wctr[0] += 1
        v = src.rearrange("(kc p) c -> p kc c", p=128)
        for k0 in range(0, kch, 4):
            k1 = min(kch, k0 + 4)
            P.dma(slot[:, k0:k1, 0:cols], v[:, k0:k1, :], q="pool")
        return slot

    import os as _os
    KSUB = int(_os.environ.get("KSUB", "99"))
    if KSUB < 1:
        P.emit(es); es.close(); return nc
    P.act(cvb[:, :, :], cvs[:, :, :], AF.Silu)
    pmc = BK_X
    for t in range(24 if KSUB >= 3 else 1):
        wt = stream(w_ada_h[:, t * 512:(t + 1) * 512], 16, 512)
        pb = bank()
        for kc in range(16):
            P.mm(pb[0:33, :], cvb[:, kc, :], wt[:, kc, :], start=(kc == 0), stop=(kc == 15))
        P.dma(brow[:, :], b_ada_h[:, t * 512:(t + 1) * 512])
        P.tt(mrow[:, :], pb[0:33, :], brow[:, :], ALU.add)
        for ri, r in enumerate((0, 32) if KSUB >= 2 else ()):
            for j in range(4):
                col = ri * 96 + t * 4 + j
                P.mm(pmc[:, col:col + 1], mrow[r:r + 1, j * 128:(j + 1) * 128], ones_f[r:r + 1, 0:1])
        which = {2: 0, 5: 1}.get(t // 4)
        if which is not None:
            cs = slice((t % 4) * 512, (t % 4 + 1) * 512)
            P.dma(grow[:, :], gpost_h[:, which, cs])
            P.tt(gtrow[:, which, cs], mrow[:, :], grow[:, :], ALU.mult)
    P.cp(modcol[:, :], pmc[:, 0:192])
    for ri in range(2):
        o = ri * 96
        P.stt(abv[:, ri, 0, :], modcol[:, o + 16:o + 32], 1.0, pvec[:, PV_GPRE_M:PV_GPRE_M + 16], ALU.add, ALU.mult)
        P.stt(abv[:, ri, 1, :], modcol[:, o + 64:o + 80], 1.0, pvec[:, PV_GPRE_F:PV_GPRE_F + 16], ALU.add, ALU.mult)

    def ab(ri, f, kc):
        o = ri * 96 + (48 if f else 0)
        return abv[:, ri, f, kc:kc + 1], modcol[:, o + kc:o + kc + 1]

    def rstd_from(out, in_, inv_n, M):
        P.act(out, in_, AF.Sqrt, bias=epsc[0:M, 0:1], scale=inv_n)
        P.add("dve", lambda e: e.reciprocal(out, out), reads=[out], writes=[out])

    def prologue(subs, f, colparams):
        for (si, M, coff) in subs:
            ssq = sml[0:M, 0:1]
            P.act(xn[0:M, si, :], x1[0:M, si, :], AF.Square, accum=ssq)
            rs = sml[0:M, 1:2]
            rstd_from(rs, ssq, 1.0 / 2048, M)
            P.ts(xn[0:M, si, :], x1[0:M, si, :], rs, None, op0=ALU.mult)
            for kc in range(16):
                pb = bank()
                pv = pb[:, :].bitcast(BF16)
                P.tr(pv[:, 0:M], xn[0:M, si, kc * 128:(kc + 1) * 128], ident_b[0:M, 0:M])
                for (c0, c1, ri) in colparams:
                    a, b = ab(ri, f, kc)
                    if kc % 2 == 0:
                        P.act(hB[:, kc, coff + c0:coff + c1], pv[:, c0:c1], AF.Identity, bias=b, scale=a)
                    else:
                        P.ts(hB[:, kc, coff + c0:coff + c1], pv[:, c0:c1], a, b, op0=ALU.mult, op1=ALU.add)

    def linear_fm(wt, kch, rhs_kc, N, mch, cb):
        for m in range(mch):
            pb = bank()
            for kc in range(kch):
                P.mm(pb[:, 0:N], wt[:, kc, m * 128:(m + 1) * 128], rhs_kc(kc), start=(kc == 0), stop=(kc == kch - 1))
            cb(m, pb[:, 0:N])

    def fm_sumsq(chunks, N, pacc):
        n = len(chunks)
        for i, ch in enumerate(chunks):
            t = tmp32()
            P.act(t[:, 0:N], ch, AF.Square)
            P.mm(pacc[:, 0:N], ones_f[:, :], t[:, 0:N], start=(i == 0), stop=(i == n - 1))

    def fm_rmsnorm_to(chunks, N, gcol0, dst_of):
        fm_sumsq(chunks, N, BK_X)
        rt = tmp32()
        rstd_from(rt[:, 0:N], BK_X[:, 0:N], 1.0 / (128 * len(chunks)), 128)
        for i, ch in enumerate(chunks):
            P.stt(dst_of(i), ch, pvec[:, gcol0 + i:gcol0 + i + 1], rt[:, 0:N], ALU.mult, ALU.mult)

    def conv_post(ych, N):
        for i in range(8):
            P.mm(BK_X[:, 0:N], ones_f[:, :], ych(i), start=(i == 0), stop=(i == 7))
        fm_sumsq([ych(i) for i in range(8)], N, BK_Y)
        mean = lnm
        P.ts(mean[:, 0:N], BK_X[:, 0:N], 1.0 / 1024, None, op0=ALU.mult)
        msq = tmp32()
        P.tt(msq[:, 0:N], mean[:, 0:N], mean[:, 0:N], ALU.mult)
        var = lnv
        P.stt(var[:, 0:N], BK_Y[:, 0:N], 1.0 / 1024, msq[:, 0:N], ALU.mult, ALU.subtract)
        rstd_from(var[:, 0:N], var[:, 0:N], 1.0, 128)
        for i in range(8):
            y = ych(i)
            P.tt(y, y, mean[:, 0:N], ALU.subtract)
            P.tt(y, y, var[:, 0:N], ALU.mult)
            P.act(y, y, AF.Identity, bias=pvec[:, PV_BLN + i:PV_BLN + i + 1], scale=pvec[:, PV_GLN + i:PV_GLN + i + 1])
            sg = tmp32()
            P.act(sg[:, 0:N], y, AF.Sigmoid)
            P.tt(y, y, sg[:, 0:N], ALU.mult)

    def proj_post(w_h, kch_total, kgrp, lhs_kc, subs, ri, which, dst_cb):
        stages = [f32B[:, :, :].rearrange("p a b -> p (a b)"), f32C[:, :, :].rearrange("p a b -> p (a b)")]
        ngr = (kch_total + kgrp - 1) // kgrp
        r = 32 * ri
        for ct in range(4):
            pbs = [bank() for _ in subs]
            for g in range(ngr):
                k0 = g * kgrp
                kn = min(kgrp, kch_total - k0)
                wt = stream(w_h[k0 * 128:(k0 + kn) * 128, ct * 512:(ct + 1) * 512], kn, 512)
                for (si, M), pb in zip(subs, pbs):
                    for kk in range(kn):
                        kc = k0 + kk
                        P.mm(pb[0:M, :], lhs_kc(kc, si, M), wt[:, kk, :], start=(kc == 0), stop=(kc == kch_total - 1))
            for (si, M), pb in zip(subs, pbs):
                st = stages[si]
                P.cp(st[0:M, ct * 512:(ct + 1) * 512], pb[0:M, :])
                j = tmp32()
                P.act(j[0:M, :], pb[0:M, :], AF.Square, accum=sml[0:M, 8 + si * 4 + ct:9 + si * 4 + ct])
        for (si, M) in subs:
            st = stages[si]
            ss = sml[0:M, 16 + si:17 + si]
            P.red(ss, sml[0:M, 8 + si * 4:12 + si * 4])
            rstd_from(ss, ss, 1.0 / 2048, M)
            for ct in range(4):
                cs = slice(ct * 512, (ct + 1) * 512)
                pg = bank()
                P.mm(pg[0:M, :], ones_f[r:r + 1, 0:M], gtrow[r:r + 1, which, cs])
                tw = tmp32()
                P.stt(tw[0:M, :], st[0:M, cs], ss, pg[0:M, :], ALU.mult, ALU.mult)
                P.tt(st[0:M, cs], x1[0:M, si, cs], tw[0:M, :], ALU.add)
            dst_cb(si, M, st)

    def ffn(subs, colparams, N, ri, out_cb):
        prologue([(si, M, si * 128) for (si, M) in subs], 1, colparams)
        for g in range(11):
            wg = stream(w_gu_h[:, g * 512:(g + 1) * 512], 16, 512)
            wu = stream(w_gu_h[:, 5632 + g * 512:5632 + (g + 1) * 512], 16, 512)
            for m in range(4):
                pg = bank()
                for kc in range(16):
                    P.mm(pg[:, 0:N], wg[:, kc, m * 128:(m + 1) * 128], hB[:, kc, 0:N], start=(kc == 0), stop=(kc == 15))
                P.act(f32A[:, m, 0:N], pg[:, 0:N], AF.Silu)
            for m in range(4):
                pu = bank()
                for kc in range(16):
                    P.mm(pu[:, 0:N], wu[:, kc, m * 128:(m + 1) * 128], hB[:, kc, 0:N], start=(kc == 0), stop=(kc == 15))
                P.tt(bigA[:, g * 4 + m, 0:N], f32A[:, m, 0:N], pu[:, 0:N], ALU.mult)
        proj_post(w_dn_h, 44, 11, lambda kc, si, M: bigA[:, kc, si * 128:si * 128 + M], subs, ri, 1, out_cb)

    def mix_out(subs, N, ri):
        def dst(si, M, st):
            P.cp(x1[0:M, si, :], st[0:M, :])
        proj_post(w_out_h, 16, 16, lambda kc, si, M: bigA[:, 16 + kc, si * 128:si * 128 + M], subs, ri, 0, dst)

    def transpose_out(src_cc, ncols, dst_h):
        for cc in range(8):
            pb = bank()
            P.tr(pb[0:ncols, 0:128], src_cc(cc), ident_f[:, :])
            P.cp(cst[0:ncols, cc * 128:(cc + 1) * 128], pb[0:ncols, 0:128])
        P.dma(dst_h.ap(), cst[0:ncols, :])

    import os as _os
    STAGE = int(_os.environ.get("KSTAGE", "99"))
    kst = bigA[:, 0:8, :]
    vst = bigA[:, 8:16, :].rearrange("p (s a) b -> p s (a b)", s=2)
    KP1 = int(_os.environ.get("KP1", "99"))
    KP1SUB = int(_os.environ.get("KP1SUB", "99"))
    for ti in range(min(KP1, c.NT1) if STAGE >= 1 else 0):
        for si in range(2):
            P.dma(x1[:, si, :], xb_h[ti * 256 + si * 128:ti * 256 + (si + 1) * 128, :])
        prologue([(0, 128, 0), (1, 128, 128)], 0, [(0, 128, 0)])
        if KP1SUB < 1:
            continue
        for half in range(2):
            wt = stream(w_in_h[:, 1024 + half * 512:1024 + (half + 1) * 512], 16, 512)

            KD = int(_os.environ.get("KDBG", "7"))

            def cbk(m, pb, half=half):
                hh = half * 4 + m
                if KD & 2:
                    P.cp(kst[:, hh, :], pb, eng="act")
                if KD & 4:
                    P.red(kmsum[:, hh, ti:ti + 1], pb)
            if KD & 1:
                linear_fm(wt, 16, lambda kc: hB[:, kc, :], 256, 4, cbk)
        if KP1SUB < 2:
            continue
        P.dma(kf_h.ap()[:, :, ti * 256:(ti + 1) * 256].rearrange("h p t -> p h t"), kst, wk=[("kf", ti)])
        for half in range(2):
            wt = stream(w_in_h[:, 2048 + half * 512:2048 + (half + 1) * 512], 16, 512)
            for si in range(2):
                pb = bank()
                for kc in range(16):
                    P.mm(pb[:, :], hB[:, kc, si * 128:(si + 1) * 128], wt[:, kc, :], start=(kc == 0), stop=(kc == 15))
                P.cp(vst[:, si, half * 512:(half + 1) * 512], pb[:, :], eng=("act" if si else "dve"))
        P.dma(vt_h.ap()[ti * 256:(ti + 1) * 256, :].rearrange("(s p) c -> p s c", p=128), vst, wk=[("vt", ti)])
    kmT = sb("kmT", [128, 8, 16])
    P.ts(kmT[:, :, :], kmsum[:, :, :], 1.0 / 256, None, op0=ALU.mult)
    kf_keys = [("kf", i) for i in range(c.NT1)]
    vt_keys = [("vt", i) for i in range(c.NT1)]

    qf = bigA[:, 0:8, :]
    kfm = bigA[:, 8:16, :]
    cat = bigA[:, 16:32, :]
    vown = bigA[:, 32:40, :].rearrange("p (s a) b -> p s (a b)", s=2)
    kvst = [f32B[:, :, :].rearrange("p a b -> p (a b)"), f32C[:, :, :].rearrange("p a b -> p (a b)")]

    def inproj(N, subs, mini, tile_i):
        rhs = lambda kc: hB[:, kc, 0:N]
        for ct in range(4):
            wt = stream(w_in_h[:, 1024 + ct * 512:1024 + (ct + 1) * 512], 16, 512)
            for (si, M) in subs:
                pb = bank()
                for kc in range(16):
                    P.mm(pb[0:M, :], hB[:, kc, si * 128:si * 128 + M], wt[:, kc, :], start=(kc == 0), stop=(kc == 15))
                P.cp(kvst[si][0:M, ct * 512:(ct + 1) * 512], pb[0:M, :], eng=("act" if ct % 2 else "dve"))
        for (si, M) in subs:
            if mini:
                P.dma(k_s_h.ap().rearrange("h t d -> t h d"), kvst[0][0:4, 0:1024].rearrange("t (h d) -> t h d", h=8))
                P.dma(v_s_h.ap().rearrange("h t d -> t h d"), kvst[0][0:4, 1024:2048].rearrange("t (h d) -> t h d", h=8))
                P.cp(vn_b[0:4, :], kvst[0][0:4, 1024:2048])
            else:
                pg = tile_i * 2 + si
                P.dma(k_own_h.ap()[pg].rearrange("h t d -> t h d"), kvst[si][:, 0:1024].rearrange("t (h d) -> t h d", h=8))
                P.dma(v_own_h.ap()[pg].rearrange("h t d -> t h d"), kvst[si][:, 1024:2048].rearrange("t (h d) -> t h d", h=8))
                P.cp(vown[:, si, :], kvst[si][:, 1024:2048])
        for half in range(2):
            wt = stream(w_in_h[:, half * 512:(half + 1) * 512], 16, 512)

            def cbq(m, pb, half=half):
                hh = half * 4 + m
                P.act(qf[:, hh, 0:N], pb, AF.Copy, scale=SCALE)
                if mini:
                    P.ts(qs_f[:, hh, :], pb[:, 0:4], SCALE, None, op0=ALU.mult)
                    P.cp(qs_b[:, hh, :], qf[:, hh, 0:4])
                else:
                    q32 = tmp32()
                    P.ts(q32[:, 0:N], pb, SCALE, None, op0=ALU.mult)
                    select_main(hh, q32)
            linear_fm(wt, 16, rhs, N, 4, cbq)
        for half in range(2):
            wt = stream(w_in_h[:, 1024 + half * 512:1024 + (half + 1) * 512], 16, 512)

            def cbk(m, pb, half=half):
                hh = half * 4 + m
                P.cp(kfm[:, hh, 0:N], pb, eng="act")
                if mini:
                    P.cp(kn_b[:, hh, :], kfm[:, hh, 0:4])
            linear_fm(wt, 16, rhs, N, 4, cbk)
        for half in range(2):
            wt = stream(w_in_h[:, 3072 + half * 512:3072 + (half + 1) * 512], 16, 512)

            def cba(m, pb, half=half):
                P.cp(f32B[:, half * 4 + m, 0:N], pb, eng="act")
            linear_fm(wt, 16, rhs, N, 4, cba)
        for half in range(2):
            wt = stream(w_in_h[:, 4096 + half * 512:4096 + (half + 1) * 512], 16, 512)

            def cbg(m, pb, half=half):
                cc = half * 4 + m
                sg = tmp32()
                P.act(sg[:, 0:N], pb, AF.Sigmoid)
                if mini:
                    P.tt(usamp[:, cc, 0:N], f32B[:, cc, 0:N], sg[:, 0:N], ALU.mult)
                else:
                    P.tt(f32A[:, cc, 32:32 + N], f32B[:, cc, 0:N], sg[:, 0:N], ALU.mult)
            linear_fm(wt, 16, rhs, N, 4, cbg)

    def select_main(hh, q32):
        for si in range(2):
            pb = bank()
            P.mm(pb[:, 0:16], q32[:, si * 128:(si + 1) * 128], kmT[:, hh, :])
            s0 = sel[:, 0, :]
            P.tt(s0, pb[:, 0:16], vb_t[:, si, :], ALU.add)
            P.add("dve", lambda e: e.max(sel[:, 1, 0:8], sel[:, 0, :]), reads=[s0], writes=[sel[:, 1, 0:8]])
            P.ts(sel[:, 2, :], s0, sel[:, 1, 2:3], None, op0=ALU.is_ge)
            P.tt(sel[:, 2, :], sel[:, 2, :], v01_t[:, si, :], ALU.mult)
            P.ts(selb[:, :], sel[:, 2, :], -1.0, -NEGB, op0=ALU.add, op1=ALU.mult)
            pt = bank()
            pv = pt[:, :].bitcast(BF16)
            P.tr(pv[0:16, 0:128], selb[:, :], ident_b[:, :])
            P.cp(rall[0:16, hh, si * 128:(si + 1) * 128], pv[0:16, 0:128], eng="act")

    kA = scr16[:, 0:2048].bitcast(BF16)
    vA = scr16[:, 2048:4096].bitcast(BF16).rearrange("p (k d) -> p k d", d=128)

    def attention_main():
        for hh in range(8):
            P.dma(kA[:, 0:S], kf_h.ap()[hh], rk=kf_keys)
            P.dma(vA[:, 0:NKT, :], vt_h.ap()[:, hh * 128:(hh + 1) * 128].rearrange("(k p) d -> p k d", p=128), rk=vt_keys)
            tot = NKT + 2
            for i in range(tot):
                pS = bank()
                if i < NKT:
                    P.mm(pS[:, 0:256], kA[:, i * 128:(i + 1) * 128], qf[:, hh, :], start=True, stop=False)
                    P.mm(pS[:, 0:256], lpast[0:36, i * 128:(i + 1) * 128], rall[0:36, hh, :], start=False, stop=True)
                    vt_ = vA[:, i, :]
                else:
                    jt = i - NKT
                    P.mm(pS[:, 0:256], kfm[:, hh, jt * 128:(jt + 1) * 128], qf[:, hh, :], start=True, stop=False)
                    P.mm(pS[:, 0:256], lpast[32:35, jt * 128:(jt + 1) * 128], rall[32:35, hh, :], start=False, stop=False)
                    P.mm(pS[:, 0:256], ident_b[:, :], caus[:, jt, :], start=False, stop=True)
                    vt_ = vown[:, jt, hh * 128:(hh + 1) * 128]
                pt = ptile()
                P.act(pt[:, :], pS[:, 0:256], AF.Exp)
                P.mm(BK_O[:, 0:256], vt_, pt[:, :], start=(i == 0), stop=(i == tot - 1))
                P.mm(BK_S[:, 0:256], ones_b[:, :], pt[:, :], start=(i == 0), stop=(i == tot - 1))
            rs = tmp32()
            P.add("dve", lambda e, rs=rs: e.reciprocal(rs[:, 0:256], BK_S[:, 0:256]), reads=[BK_S[:, 0:256]], writes=[rs[:, 0:256]])
            P.tt(f32C[:, hh, :], BK_O[:, 0:256], rs[:, 0:256], ALU.mult)

    O_s = scr16[:, 0:2048].rearrange("p (h n q) -> p h n q", h=8, q=4)
    L_s = scr16[:, 2048:4096].rearrange("p (h n q) -> p h n q", h=8, q=4)
    kpg = [f32B[:, :, :].rearrange("p a b -> p (a b)")[:, 0:1024].rearrange("p (h d) -> p h d", h=8),
           f32B[:, :, :].rearrange("p a b -> p (a b)")[:, 1024:2048].rearrange("p (h d) -> p h d", h=8)]
    vpg = [f32C[:, :, :].rearrange("p a b -> p (a b)")[:, 0:1024].rearrange("p (h d) -> p h d", h=8),
           f32C[:, :, :].rearrange("p a b -> p (a b)")[:, 1024:2048].rearrange("p (h d) -> p h d", h=8)]
    vpb = f32A[:, :, :].rearrange("p a b -> p (a b)")[:, 0:1024].bitcast(BF16).rearrange("p (e h d) -> p e h d", e=2, h=8)

    def attention_sample():
        P.cp(ptb_f[:, :], ptb_i[:, :])
        P.ts(idx_f, ptb_f[:, :], 128.0, ioff[:, 0:1], op0=ALU.mult, op1=ALU.add)
        P.cp(idx_i[:, :], idx_f)
        qall = qs_b[:, :, :].rearrange("p h q -> p (h q)")
        kflat = f32B[:, :, :].rearrange("p a b -> p (a b)")
        vflat = f32C[:, :, :].rearrange("p a b -> p (a b)")
        vpb4 = f32A[:, :, :].rearrange("p a b -> p (a b)")[:, 0:1024].bitcast(BF16).rearrange("p (e r d) -> p e r d", e=2, r=8)
        for n in range(c.NSB):
            for e_ in range(2):
                j = 2 * n + e_
                ix = idx_i[:, j:j + 1]
                ko = kflat[:, e_ * 1024:(e_ + 1) * 1024]
                vo = vflat[:, e_ * 1024:(e_ + 1) * 1024]
                P.add("pool", lambda e, o=ko, ix=ix: e.indirect_dma_start(
                    out=o, out_offset=None, in_=ck_h.ap(), in_offset=bass.IndirectOffsetOnAxis(ap=ix, axis=0)),
                    reads=[ix], writes=[ko], dma=True)
                P.add("pool", lambda e, o=vo, ix=ix: e.indirect_dma_start(
                    out=o, out_offset=None, in_=cv_h.ap(), in_offset=bass.IndirectOffsetOnAxis(ap=ix, axis=0)),
                    reads=[ix], writes=[vo], dma=True)
                P.cp(vpb4[:, e_, :, :], vo.rearrange("p (r d) -> p r d", r=8))
            for e_ in range(2):
                j = 2 * n + e_
                ko = kflat[:, e_ * 1024:(e_ + 1) * 1024]
                kt = KT[e_]
                for half in range(2):
                    pT = bank()
                    for rr in range(4):
                        r = half * 4 + rr
                        P.tr(pT[:, rr * 128:(rr + 1) * 128], ko[:, r * 128:(r + 1) * 128], ident_f[:, :])
                    P.cp(kt[:, half * 4:(half + 1) * 4, :], pT[:, :].rearrange("p (r k) -> p r k", r=4), eng="act")
                P.red(ksum[:, :, j], kt[:, :, :].rearrange("p r (h g) -> p h r g", h=8), axis=AX.XY)
                pS = bank()
                for r in range(8):
                    P.mm(pS[:, r * 32:(r + 1) * 32], kt[:, r, :], qall)
                t1 = tmp32()
                t3 = t1[:, 0:256].rearrange("p (r k) -> p r k", r=8)
                P.tt(t3, pS[:, 0:256].rearrange("p (r k) -> p r k", r=8),
                     sbias[:, j, :].unsqueeze(2).to_broadcast([128, 8, 32]), ALU.add)
                P.act(t1[:, 0:256], t1[:, 0:256], AF.Exp)
                P.tt(Pb[e_][:, :, :], t3, bmask[:, :].unsqueeze(1).to_broadcast([128, 8, 32]), ALU.mult)
            cnt = 0
            for e_ in range(2):
                for r in range(8):
                    P.mm(BK_O[:, 0:32], vpb4[:, e_, r, :], Pb[e_][:, r, :], start=(cnt == 0), stop=(cnt == 15))
                    cnt += 1
            cnt = 0
            for e_ in range(2):
                for r in range(8):
                    P.mm(BK_S[:, 0:32], ones_b[:, :], Pb[e_][:, r, :], start=(cnt == 0), stop=(cnt == 15))
                    cnt += 1
            P.cp(O_s[:, :, n, :], BK_O[:, 0:32].rearrange("p (h q) -> p h q", q=4))
            P.cp(L_s[:, :, n, :], BK_S[:, 0:32].rearrange("p (h q) -> p h q", q=4), eng="act")
        NSB = c.NSB
        P.red(kms[:, :, :], ksum[:, :, :].rearrange("p h (n e) -> p h n e", e=2))
        P.ts(kms[:, :, :], kms[:, :, :], 1.0 / 256, None, op0=ALU.mult)
        ncol = max(NSB, 8)
        for hh in range(8):
            pb = bank()
            P.mm(pb[0:4, 0:NSB], qs_f[:, hh, :], kms[:, hh, :])
            P.memset(sel[0:4, :, :], -1e9)
            sv = sel[0:4, :, :].rearrange("p a b -> p (a b)")
            P.cp(sv[:, 0:NSB], pb[0:4, 0:NSB])
            P.add("dve", lambda e, sv=sv: e.max(sml[0:4, 32:40], sv[:, 0:64]), reads=[sv[:, 0:64]], writes=[sml[0:4, 32:40]])
            m01 = tmp32()
            P.ts(m01[0:4, 0:NSB], sv[:, 0:NSB], sml[0:4, 34:35], None, op0=ALU.is_ge)
            pB = bank()
            for q in range(4):
                P.mm(pB[:, q * NSB:(q + 1) * NSB], eqc[0:4, q, :], m01[0:4, 0:NSB])
            pBv = pB[:, 0:4 * NSB].rearrange("p (q n) -> p q n", q=4)
            tm = tmp32()
            tmv = tm[:, 0:4 * NSB].rearrange("p (q n) -> p q n", q=4)
            P.tt(tmv, O_s[:, hh, 0:NSB, :].rearrange("p n q -> p q n"), pBv, ALU.mult)
            P.red(sml[:, 40:44], tmv)
            tm2 = tmp32()
            tmv2 = tm2[:, 0:4 * NSB].rearrange("p (q n) -> p q n", q=4)
            P.tt(tmv2, L_s[:, hh, 0:NSB, :].rearrange("p n q -> p q n"), pBv, ALU.mult)
            P.red(sml[:, 44:48], tmv2)
            pS = bank()
            P.mm(pS[0:4, 0:4], kn_b[:, hh, :], qs_b[:, hh, :])
            so = tmp32()
            P.tt(so[0:4, 0:4], pS[0:4, 0:4], sown[:, hh, :], ALU.add)
            P.act(sexp[0][0:4, 0:4], so[0:4, 0:4], AF.Exp)
            P.mm(BK_O[:, 0:4], vn_b[0:4, hh * 128:(hh + 1) * 128], sexp[0][0:4, 0:4])
            P.mm(BK_S[:, 0:4], ones_b[0:4, :], sexp[0][0:4, 0:4])
            P.tt(sml[:, 40:44], sml[:, 40:44], fq[:, hh, :], ALU.mult)
            P.tt(sml[:, 44:48], sml[:, 44:48], fq[:, hh, :], ALU.mult)
            P.tt(sml[:, 40:44], sml[:, 40:44], BK_O[:, 0:4], ALU.add)
            P.tt(sml[:, 44:48], sml[:, 44:48], BK_S[:, 0:4], ALU.add)
            P.add("dve", lambda e: e.reciprocal(sml[:, 44:48], sml[:, 44:48]), reads=[sml[:, 44:48]], writes=[sml[:, 44:48]])
            P.tt(oat_s[:, hh, :], sml[:, 40:44], sml[:, 44:48], ALU.mult)

    def finish():
        P.emit(es)
        es.close()
        return nc
    if STAGE < 2:
        return finish()
    P.dma(x1[0:c.MW, 0, :], xmini_h.ap())
    prologue([(0, c.MW, 0)], 0, [(0, 4, 1), (4, c.MW, 0)])
    inproj(c.MW, [(0, 4)], True, 0)
    for cc in range(8):
        P.ts(uhalo[:, cc, :], usamp[:, cc, 4:36], hflag[:, 0:1], None, op0=ALU.mult)
    if STAGE < 3:
        return finish()
    P.dma(cst[0:30, :], sconv_h.ap())
    for cc in range(8):
        pb = bank()
        P.tr(pb[:, 0:30], cst[0:30, cc * 128:(cc + 1) * 128], ident_f[0:30, 0:30])
        P.cp(ue_s[:, cc, 0:30], pb[:, 0:30])
        P.cp(ue_s[:, cc, 30:34], usamp[:, cc, 0:4])
    for cc in range(8):
        for t_ in range(4):
            tw = tmp32()
            P.tt(tw[:, 0:31], pvec[:, PV_WDW + cc * 31:PV_WDW + (cc + 1) * 31], ue_s[:, cc, t_:t_ + 31], ALU.mult)
            P.red(y_sm[:, cc, t_:t_ + 1], tw[:, 0:31])
        P.ts(y_sm[:, cc, :], y_sm[:, cc, :], pvec[:, PV_BDW + cc:PV_BDW + cc + 1], None, op0=ALU.add)
    transpose_out(lambda cc: ue_s[:, cc, 4:34], 30, conv_s_h)
    if STAGE < 4:
        return finish()
    attention_sample()
    if STAGE < 5:
        return finish()
    fm_rmsnorm_to([oat_s[:, hh, :] for hh in range(8)], 4, PV_GATTN, lambda i: cat[:, i, 0:4])
    conv_post(lambda cc: y_sm[:, cc, :], 4)
    fm_rmsnorm_to([y_sm[:, cc, :] for cc in range(8)], 4, PV_GCONV, lambda i: cat[:, 8 + i, 0:4])
    mix_out([(0, 4)], 4, 1)

    def out_s(si, M, st):
        P.dma(y_s_h.ap(), st[0:4, :])
    ffn([(0, 4)], [(0, 4, 1)], 4, 1, out_s)

    if STAGE < 6:
        return finish()
    for it in range(c.NOWN):
        for si in range(2):
            r0 = it * 256 + si * 128
            P.dma(x1[:, si, :], xown_h[r0:r0 + 128, :])
            P.dma(vb_t[:, si, :], vbias_h[r0:r0 + 128, :])
            P.dma(v01_t[:, si, :], v01_h[r0:r0 + 128, :])
        P.dma(rall[32:36, :, :], rc_h[it])
        subs = [(0, 128), (1, 128)]
        prologue([(0, 128, 0), (1, 128, 128)], 0, [(0, 128, 0)])
        inproj(256, subs, False, it)
        attention_main()
        fm_rmsnorm_to([f32C[:, hh, :] for hh in range(8)], 256, PV_GATTN, lambda i: cat[:, i, :])
        for cc in range(8):
            P.cp(f32A[:, cc, 0:32], uhalo[:, cc, :], eng="act")
        for cc in range(8):
            eng = "dve"
            y = f32B[:, cc, :]
            w0 = PV_WDW + cc * 31
            P.ts(y, f32A[:, cc, 2:258], pvec[:, w0:w0 + 1], pvec[:, PV_BDW + cc:PV_BDW + cc + 1], op0=ALU.mult, op1=ALU.add, eng=eng)
            for j in range(1, 31):
                P.stt(y, f32A[:, cc, 2 + j:258 + j], pvec[:, w0 + j:w0 + j + 1], y, ALU.mult, ALU.add, eng=eng)
        if it == c.NOWN - 1:
            transpose_out(lambda cc: f32A[:, cc, 258:288], 30, convp_h)
        for cc in range(8):
            P.cp(uhalo[:, cc, :], f32A[:, cc, 256:288], eng="act")
        conv_post(lambda cc: f32B[:, cc, :], 256)
        fm_rmsnorm_to([f32B[:, cc, :] for cc in range(8)], 256, PV_GCONV, lambda i: cat[:, 8 + i, :])
        mix_out(subs, 256, 0)

        def out_m(si, M, st, it=it):
            r0 = it * 256 + si * 128
            P.dma(y_own_h.ap()[r0:r0 + 128, :], st[:, :])
        ffn(subs, [(0, 128, 0)], 256, 0, out_m)

    P.emit(es)
    es.close()
    return nc


def _bf(a):
    return np.asarray(a, dtype=np.float32).astype(ml_dtypes.bfloat16)


def host_consts(cfg, qstart):
    c = cfg
    m = (2.0 ** (-8.0 * (np.arange(8) + 1.0) / 8)).astype(np.float64)
    NKT = c.NKT
    lp = np.zeros((36, NKT * 128), np.float32)
    for kt in range(NKT):
        sl = slice(kt * 128, (kt + 1) * 128)
        if kt // 2 < 16:
            lp[kt // 2, sl] = 1.0
        lp[32, sl] = 1.0
        lp[33, sl] = np.arange(128)
        lp[34, sl] = 128.0 * kt
        lp[35, sl] = 1.0
    j = np.arange(256)
    caus = np.zeros((128, 2, 256), np.float32)
    for jt in range(2):
        kidx = 128 * jt + np.arange(128)
        caus[:, jt, :] = np.where(kidx[:, None] <= j[None, :], 0.0, NEGB)
    rc = np.zeros((c.NOWN, 4, 8, 256), np.float32)
    for it in range(c.NOWN):
        q0 = qstart + it * 256
        for h in range(8):
            rc[it, 0, h] = -m[h] * j
            rc[it, 1, h] = m[h]
            rc[it, 2, h] = m[h]
            rc[it, 3, h] = -m[h] * q0
    tq = qstart + np.arange(c.OWN)
    nfull = tq // 256
    v01 = (np.arange(16)[None, :] < nfull[:, None]).astype(np.float32)
    vbias = np.where(v01 > 0, 0.0, -1e9).astype(np.float32)
    past = c.NPG * 128
    sbias = np.zeros((128, c.NPG, 8), np.float32)
    bmask = np.zeros((128, 32), np.float32)
    for h in range(8):
        bmask[h * 16:(h + 1) * 16, h * 4:(h + 1) * 4] = 1.0
        for g in range(16):
            for jp in range(c.NPG):
                sbias[h * 16 + g, jp, :] = m[h] * (128 * jp + 8 * g + np.arange(8) - past)
    fq = np.zeros((128, 8, 4), np.float32)
    sown = np.zeros((4, 8, 4), np.float32)
    for h in range(8):
        for q in range(4):
            fq[:, h, q] = np.exp(-m[h] * q)
            for k in range(4):
                sown[k, h, q] = -m[h] * (q - k) if k <= q else NEGB
    eqc = np.zeros((4, 4, 128), np.float32)
    for q in range(4):
        eqc[q, q, :] = 1.0
    ioff = (np.arange(128)[:, None] + 128.0 * np.arange(8)[None, :]).astype(np.float32)
    return dict(lpast=_bf(lp), caus=_bf(caus), rc=_bf(rc), vbias=vbias, v01=v01, sbias=sbias, bmask=bmask, fq=fq,
                sown=sown, eqc=eqc, ioff=ioff, ident_f=np.eye(128, dtype=np.float32), ident_b=_bf(np.eye(128)))


def core_inputs(cfg, inp, b, qstart, sb_):
    c = cfg
    f = np.float32
    d = host_consts(c, qstart)
    col = lambda v: np.ascontiguousarray(np.asarray(v, f).reshape(-1, 128).T)
    pv = np.zeros((128, NPV), f)
    pv[:, PV_GPRE_M:PV_GPRE_M + 16] = col(inp["g_pre_mix"][0])
    pv[:, PV_GPRE_F:PV_GPRE_F + 16] = col(inp["g_pre_ffn"][0])
    pv[:, PV_GATTN:PV_GATTN + 8] = col(inp["g_attn_out"][0])
    pv[:, PV_GCONV:PV_GCONV + 8] = col(inp["g_conv_out"][0])
    pv[:, PV_GLN:PV_GLN + 8] = col(inp["g_conv_ln"][0])
    pv[:, PV_BLN:PV_BLN + 8] = col(inp["b_conv_ln"][0])
    pv[:, PV_BDW:PV_BDW + 8] = col(inp["b_dw"][0])
    wdw = np.asarray(inp["w_dw"], f)[0, :, 0, :]
    pv[:, PV_WDW:] = wdw.reshape(31, 8, 128).transpose(2, 1, 0).reshape(128, 8 * 31)
    d["pvec"] = pv
    cv = np.zeros((128, 16, 33), f)
    cv[:, :, 0] = col(inp["c_prompt"][b])
    cv[:, :, 32] = col(inp["c_sample"][sb_])
    d["cvec"] = cv
    d["w_ada"] = np.asarray(inp["w_ada"], f)[0]
    ba = np.zeros((33, 12288), f)
    ba[0] = inp["b_ada"][0]
    ba[32] = inp["b_ada"][0]
    d["b_ada33"] = ba
    gp = np.zeros((33, 2, 2048), f)
    for r in (0, 32):
        gp[r, 0] = inp["g_post_mix"][0]
        gp[r, 1] = inp["g_post_ffn"][0]
    d["gpost33"] = gp
    d["w_in"] = np.asarray(inp["w_in"], f)[0]
    d["w_out"] = np.asarray(inp["w_out"], f)[0]
    d["w_gu"] = np.asarray(inp["w_gate_up"], f)[0]
    d["w_down"] = np.asarray(inp["w_down"], f)[0]
    xp = np.asarray(inp["x_prompt"], f)
    d["xb"] = xp[b]
    d["xown"] = xp[b, qstart:qstart + c.OWN]
    xm = np.zeros((c.MW, 2048), f)
    xm[0:4] = np.asarray(inp["x_sample"], f)[sb_]
    if qstart > 0:
        xm[4:36] = xp[b, qstart - 32:qstart]
    d["xmini"] = xm
    d["haloflag"] = np.full((128, 1), 1.0 if qstart > 0 else 0.0, f)
    d["cache_k"] = np.asarray(inp["cache_k"], f)[0].reshape(-1, 1024)
    d["cache_v"] = np.asarray(inp["cache_v"], f)[0].reshape(-1, 1024)
    d["ptab"] = np.asarray(inp["page_table"], np.int32)[sb_:sb_ + 1]
    d["sconv"] = np.asarray(inp["state_conv"], f)[0, sb_]
    return d


_NC_CACHE = {}


def run_cores(cfg, inp, assign):
    key = (cfg.S, cfg.OWN, cfg.NPG, cfg.NPOOL)
    if key not in _NC_CACHE:
        _NC_CACHE[key] = build(cfg)
    nc = _NC_CACHE[key]
    maps = [core_inputs(cfg, inp, b, q, s) for (b, q, s) in assign]
    res = run_bass_kernel_spmd(nc, maps, core_ids=list(range(len(assign))))
    return res.results


def kernel(**inp):
    cfg = Cfg()
    assign = [(cid // 4, (cid % 4) * 1024, cid) for cid in range(8)]
    r = run_cores(cfg, inp, assign)
    f = np.float32
    y_p = np.zeros((2, 4096, 2048), f)
    k_p = np.zeros((1, 2, 32, 8, 128, 128), f)
    v_p = np.zeros((1, 2, 32, 8, 128, 128), f)
    cv_p = np.zeros((1, 2, 30, 1024), f)
    y_s = np.zeros((8, 4, 2048), f)
    k_s = np.zeros((1, 8, 8, 4, 128), f)
    v_s = np.zeros((1, 8, 8, 4, 128), f)
    cv_s = np.zeros((1, 8, 30, 1024), f)
    for cid, (b, q, s) in enumerate(assign):
        o = r[cid]
        y_p[b, q:q + 1024] = o["y_own"]
        k_p[0, b, q // 128:q // 128 + 8] = o["k_own"]
        v_p[0, b, q // 128:q // 128 + 8] = o["v_own"]
        if q == 3072:
            cv_p[0, b] = o["convp"]
        y_s[s] = o["y_s"]
        k_s[0, s] = o["k_s"]
        v_s[0, s] = o["v_s"]
        cv_s[0, s] = o["conv_s"]
    return (y_p, y_s, k_p, v_p, cv_p, k_s, v_s, cv_s)
```

```python
import numpy as np
import concourse.bass as bass
import concourse.mybir as mybir

F32 = mybir.dt.float32
BF16 = mybir.dt.bfloat16
I32 = mybir.dt.int32
AF = mybir.ActivationFunctionType
ALU = mybir.AluOpType
AX = mybir.AxisListType

GRAN = 64
NDSEM = 20


def _esize(dt):
    if dt == BF16:
        return 2
    return 4


class Prog:
    def __init__(self, nc):
        self.nc = nc
        self.ins = []
        self.lastw = {}
        self.readers = {}
        self.tbytes = {}
        self.psum = set()
        self.dma_n = {"sp": 0, "pool": 0}
        self.dma_last = {}

    def reg(self, t, nbytes, psum=False):
        self.tbytes[t.name] = nbytes
        if psum:
            self.psum.add(t.name)

    def keys(self, x):
        if isinstance(x, tuple):
            return [x]
        name = x.tensor.name
        pb = self.tbytes.get(name)
        if pb is None or name in self.psum:
            return [(name,)]
        es = _esize(x.dtype)
        off = (x.offset * es) % pb
        span = es
        for (st, cnt) in x.ap[1:]:
            span += abs(st) * (cnt - 1) * es
        g0 = off // GRAN
        g1 = (off + span - 1) // GRAN
        return [(name, g) for g in range(g0, g1 + 1)]

    def add(self, eng, fn, reads=(), writes=(), dma=False):
        idx = len(self.ins)
        deps = set()
        rk = [k for r in reads for k in self.keys(r)]
        wk = [k for w in writes for k in self.keys(w)]
        pk = [k for k in rk if k[0] in self.psum]
        if pk:
            rk = [k for k in rk if k[0] not in self.psum]
            wk = wk + pk
        for k in rk:
            d = self.lastw.get(k)
            if d is not None:
                deps.add(d)
        for k in wk:
            d = self.lastw.get(k)
            if d is not None:
                deps.add(d)
            for r in self.readers.get(k, ()):
                deps.add(r)
        for k in rk:
            self.readers.setdefault(k, []).append(idx)
        for k in wk:
            self.lastw[k] = idx
            self.readers[k] = []
        sem = None
        if dma:
            n = self.dma_n[eng]
            self.dma_n[eng] = n + 1
            sem = (eng, n % NDSEM)
            prev = self.dma_last.get(sem)
            if prev is not None:
                deps.add(prev)
            self.dma_last[sem] = idx
        deps.discard(idx)
        self.ins.append(dict(eng=eng, fn=fn, deps=deps, dma=dma, sig=bool(dma), sem=sem, val=0))
        return idx

    def emit(self, es):
        nc = self.nc
        ins = self.ins
        for i in ins:
            for d in i["deps"]:
                di = ins[d]
                if i["eng"] == "pe" and di["eng"] == "pe" and not di["dma"] and not i["dma"]:
                    continue
                di["sig"] = True
        cnt = {}
        for i in ins:
            if not i["sig"]:
                continue
            if i["dma"]:
                s = i["sem"]
                cnt[s] = cnt.get(s, 0) + 16
            else:
                s = (i["eng"], "c")
                i["sem"] = s
                cnt[s] = cnt.get(s, 0) + 1
            i["val"] = cnt[s]
        semobj = {}
        for s in cnt:
            semobj[s] = es.enter_context(nc.semaphore("s_%s_%s" % s))
        final = dict(cnt)

        def make(engname):
            def body(e):
                waited = {}
                for i in ins:
                    if i["eng"] != engname:
                        continue
                    need = {}
                    for d in sorted(i["deps"]):
                        di = ins[d]
                        if engname == "pe" and di["eng"] == "pe" and not di["dma"] and not i["dma"]:
                            continue
                        s = di["sem"]
                        v = di["val"]
                        if need.get(s, 0) < v:
                            need[s] = v
                    for s, v in need.items():
                        if waited.get(s, 0) < v:
                            e.wait_ge(semobj[s], v)
                            waited[s] = v
                    r = i["fn"](e)
                    if i["sig"]:
                        r.then_inc(semobj[i["sem"]], 16 if i["dma"] else 1)
                if engname == "sp":
                    for s, v in final.items():
                        if s[1] != "c" and waited.get(s, 0) < v:
                            e.wait_ge(semobj[s], v)
            return body

        block = es.enter_context(nc.Block())
        block.tensor(make("pe"))
        block.scalar(make("act"))
        block.vector(make("dve"))
        block.gpsimd(make("pool"))
        block.sync(make("sp"))

    def mm(self, out, lhsT, rhs, start=True, stop=True):
        self.add("pe", lambda e: e.matmul(out, lhsT, rhs, start=start, stop=stop),
                 reads=[lhsT, rhs], writes=[out])

    def tr(self, out, in_, ident):
        self.add("pe", lambda e: e.transpose(out, in_, ident), reads=[in_, ident], writes=[out])

    def act(self, out, in_, func, bias=None, scale=None, accum=None):
        kw = {}
        rd = [in_]
        if bias is not None:
            kw["bias"] = bias
            if not isinstance(bias, (int, float)):
                rd.append(bias)
        if scale is not None:
            kw["scale"] = scale
            if not isinstance(scale, (int, float)):
                rd.append(scale)
        wr = [out]
        if accum is not None:
            kw["accum_out"] = accum
            wr.append(accum)
            self.memset(accum, 0.0)
        self.add("act", lambda e: e.activation(out, in_, func, **kw), reads=rd, writes=wr)

    def tt(self, out, in0, in1, op, eng="dve"):
        self.add(eng, lambda e: e.tensor_tensor(out, in0, in1, op), reads=[in0, in1], writes=[out])

    def ts(self, out, in0, s1, s2=None, op0=ALU.mult, op1=None, eng="dve", accum=None):
        rd = [in0]
        if not isinstance(s1, (int, float)):
            rd.append(s1)
        if s2 is not None and not isinstance(s2, (int, float)):
            rd.append(s2)
        kw = {}
        if op1 is not None:
            kw["op1"] = op1
        wr = [out]
        if accum is not None:
            kw["accum_out"] = accum
            wr.append(accum)
        self.add(eng, lambda e: e.tensor_scalar(out, in0, s1, s2, op0, **kw), reads=rd, writes=wr)

    def stt(self, out, in0, s, in1, op0, op1, eng="dve"):
        rd = [in0, in1]
        if not isinstance(s, (int, float)):
            rd.append(s)
        self.add(eng, lambda e: e.scalar_tensor_tensor(out, in0, s, in1, op0, op1), reads=rd, writes=[out])

    def cp(self, out, in_, eng="dve"):
        if eng == "act":
            self.act(out, in_, AF.Copy)
        else:
            self.add(eng, lambda e: e.tensor_copy(out, in_), reads=[in_], writes=[out])

    def red(self, out, in_, op=ALU.add, eng="dve", axis=AX.X):
        self.add(eng, lambda e: e.tensor_reduce(out, in_, axis, op), reads=[in_], writes=[out])

    def memset(self, out, v, eng="dve"):
        self.add(eng, lambda e: e.memset(out, v), writes=[out])

    def dma(self, out, in_, q="sp", rk=None, wk=None, **kw):
        rd = [in_] if rk is None else list(rk)
        wr = [out] if wk is None else list(wk)
        self.add(q, lambda e: e.dma_start(out=out, in_=in_, **kw), reads=rd, writes=wr, dma=True)
from contextlib import ExitStack
import ml_dtypes
from concourse.bass_utils import run_bass_kernel_spmd

EPS = 1e-6
SCALE = 128.0 ** -0.5
NEGB = -30000.0


class Cfg:
    D = 2048
    KC = 16
    H = 8
    CC = 8
    FF = 5632
    FC = 44
    T = 256
    DS = 4
    HALO = 32
    MW = 36

    def __init__(self, S=4096, OWN=1024, NPG=128, NPOOL=1280):
        self.S = S
        self.OWN = OWN
        self.NPG = NPG
        self.NPOOL = NPOOL
        self.NKT = S // 128
        self.NBLK = S // 256
        self.NOWN = OWN // 256
        self.NT1 = S // 256
        self.NSB = NPG // 2


PV_GPRE_M, PV_GPRE_F, PV_GATTN, PV_GCONV, PV_GLN, PV_BLN, PV_BDW, PV_WDW = 0, 16, 32, 40, 48, 56, 64, 72
NPV = 72 + 8 * 31


def build(cfg):
    c = cfg
    nc = bass.Bass("TRN2", target_bir_lowering=False)
    P = Prog(nc)
    es = ExitStack()
    T, NKT, S = c.T, c.NKT, c.S

    def din(name, shape, dt=F32):
        return nc.dram_tensor(name, list(shape), dt, kind="ExternalInput")

    def dout(name, shape, dt=F32):
        return nc.dram_tensor(name, list(shape), dt, kind="ExternalOutput")

    ident_f_h = din("ident_f", [128, 128])
    ident_b_h = din("ident_b", [128, 128], BF16)
    cvec_h = din("cvec", [128, 16, 33])
    w_ada_h = din("w_ada", [2048, 12288])
    b_ada_h = din("b_ada33", [33, 12288])
    gpost_h = din("gpost33", [33, 2, 2048])
    pvec_h = din("pvec", [128, NPV])
    w_in_h = din("w_in", [2048, 5120])
    w_out_h = din("w_out", [2048, 2048])
    w_gu_h = din("w_gu", [2048, 11264])
    w_dn_h = din("w_down", [5632, 2048])
    xb_h = din("xb", [S, 2048])
    xown_h = din("xown", [c.OWN, 2048])
    xmini_h = din("xmini", [c.MW, 2048])
    hflag_h = din("haloflag", [128, 1])
    lpast_h = din("lpast", [36, NKT * 128], BF16)
    caus_h = din("caus", [128, 2, 256], BF16)
    rc_h = din("rc", [c.NOWN, 4, 8, 256], BF16)
    vbias_h = din("vbias", [c.OWN, 16])
    v01_h = din("v01", [c.OWN, 16])
    ck_h = din("cache_k", [c.NPOOL * 128, 1024])
    cv_h = din("cache_v", [c.NPOOL * 128, 1024])
    ptab_h = din("ptab", [1, c.NPG], I32)
    ioff_h = din("ioff", [128, 8])
    sconv_h = din("sconv", [30, 1024])
    sbias_h = din("sbias", [128, c.NPG, 8])
    bmask_h = din("bmask", [128, 32])
    fq_h = din("fq", [128, 8, 4])
    sown_h = din("sown", [4, 8, 4])
    eq_h = din("eqc", [4, 4, 128])

    y_own_h = dout("y_own", [c.OWN, 2048])
    k_own_h = dout("k_own", [c.OWN // 128, 8, 128, 128])
    v_own_h = dout("v_own", [c.OWN // 128, 8, 128, 128])
    convp_h = dout("convp", [30, 1024])
    y_s_h = dout("y_s", [4, 2048])
    k_s_h = dout("k_s", [8, 4, 128])
    v_s_h = dout("v_s", [8, 4, 128])
    conv_s_h = dout("conv_s", [30, 1024])

    kf_h = nc.dram_tensor("kf_scr", [8, 128, S], BF16)
    vt_h = nc.dram_tensor("vt_scr", [S, 1024], BF16)

    def sb(name, shape, dt=F32):
        t = es.enter_context(nc.sbuf_tensor(name, list(shape), dt))
        n = 1
        for s_ in shape[1:]:
            n *= s_
        P.reg(t, n * (2 if dt == BF16 else 4))
        return t

    banks = []
    for i in range(8):
        t = es.enter_context(nc.psum_tensor("pb%d" % i, [128, 512], F32))
        P.reg(t, 2048, psum=True)
        banks.append(t)
    rot = [0]

    def bank():
        b = banks[rot[0] % 4]
        rot[0] += 1
        return b
    BK_O, BK_S, BK_X, BK_Y = banks[4], banks[5], banks[6], banks[7]

    ident_f = sb("identf", [128, 128])
    ident_b = sb("identb", [128, 128], BF16)
    ones_f = sb("onesf", [128, 128])
    ones_b = sb("onesb", [128, 128], BF16)
    epsc = sb("epsc", [128, 1])
    pvec = sb("pvecs", [128, NPV])
    modcol = sb("modcol", [128, 192])
    abv = sb("abv", [128, 2, 2, 16])
    gtrow = sb("gtrow", [33, 2, 2048])
    lpast = sb("lpasts", [36, NKT * 128], BF16)
    caus = sb("causs", [128, 2, 256], BF16)
    rall = sb("rall", [36, 8, 256], BF16)
    kmsum = sb("kmsum", [128, 8, 16])
    hflag = sb("hflag", [128, 1])
    wsl = [sb("wsl%d" % i, [128, 16, 512], BF16) for i in range(3)]
    x1 = sb("x1", [128, 2, 2048])
    xn = sb("xn", [128, 1, 2048], BF16)
    hB = sb("hB", [128, 16, 256], BF16)
    bigA = sb("bigA", [128, 44, 256], BF16)
    f32A = sb("f32A", [128, 8, 288])
    f32B = sb("f32B", [128, 8, 256])
    f32C = sb("f32C", [128, 8, 256])
    scr16 = sb("scr16", [128, 4096])
    ptl = [sb("ptl%d" % i, [128, 256], BF16) for i in range(3)]
    t32 = [sb("t32_%d" % i, [128, 512]) for i in range(4)]
    sml = sb("sml", [128, 64])
    lnm = sb("lnm", [128, 256])
    lnv = sb("lnv", [128, 256])
    sel = sb("sel", [128, 4, 16])
    selb = sb("selb", [128, 16], BF16)
    vb_t = sb("vb_t", [128, 2, 16])
    v01_t = sb("v01_t", [128, 2, 16])
    uhalo = sb("uhalo", [128, 8, 32])
    usamp = sb("usamp", [128, 8, 36])
    qs_b = sb("qs_b", [128, 8, 4], BF16)
    qs_f = sb("qs_f", [128, 8, 4])
    kn_b = sb("kn_b", [128, 8, 4], BF16)
    vn_b = sb("vn_b", [4, 1024], BF16)
    kms = sb("kms", [128, 8, c.NSB])
    sbias = sb("sbias_s", [128, c.NPG, 8])
    bmask = sb("bmask_s", [128, 32])
    KT = [sb("KT%d" % i, [128, 8, 128], BF16) for i in range(2)]
    Pb = [sb("Pb%d" % i, [128, 8, 32], BF16) for i in range(2)]
    fq = sb("fq_s", [128, 8, 4])
    sown = sb("sown_s", [4, 8, 4])
    eqc = sb("eqc_s", [4, 4, 128])
    ptb_i = sb("ptb_i", [128, c.NPG], I32)
    ptb_f = sb("ptb_f", [128, c.NPG])
    ioff = sb("ioff_s", [128, 8])
    idx_i = sb("idx_i", [128, c.NPG], I32)
    sexp = [sb("sexp%d" % i, [128, 4], BF16) for i in range(2)]
    oat_s = sb("oat_s", [128, 8, 4])
    ue_s = sb("ue_s", [128, 8, 34])
    y_sm = sb("y_sm", [128, 8, 4])

    mrow = t32[0][0:33, :]
    brow = t32[1][0:33, :]
    grow = t32[2][0:33, :]
    idx_f = f32C[:, :, :].rearrange("p a b -> p (a b)")[:, 0:c.NPG]
    cst = scr16[0:30, 0:1024]
    _fa = f32A[:, :, :].rearrange("p a b -> p (a b)")
    cvs = _fa[:, 0:528].rearrange("p (k c) -> p k c", c=33)
    cvb = _fa[:, 1024:1288].bitcast(BF16).rearrange("p (k c) -> p k c", c=33)
    ksum = x1[:, 1, 0:8 * c.NPG].rearrange("p (h j) -> p h j", h=8)
    tctr = [0]

    def tmp32():
        t = t32[tctr[0] % 4]
        tctr[0] += 1
        return t
    pctr = [0]

    def ptile():
        t = ptl[pctr[0] % 3]
        pctr[0] += 1
        return t

    P.dma(ident_f[:, :], ident_f_h.ap())
    P.dma(ident_b[:, :], ident_b_h.ap())
    P.memset(ones_f[:, :], 1.0)
    P.memset(ones_b[:, :], 1.0)
    P.memset(epsc[:, :], EPS)
    P.memset(rall[:, :, :], 0.0)
    P.memset(kmsum[:, :, :], 0.0)
    P.dma(pvec[:, :], pvec_h.ap())
    P.dma(lpast[:, :], lpast_h.ap())
    P.dma(caus[:, :, :], caus_h.ap())
    P.dma(hflag[:, :], hflag_h.ap())
    P.dma(cvs[:, :, :], cvec_h.ap())
    P.dma(sbias[:, :, :], sbias_h.ap())
    P.dma(bmask[:, :], bmask_h.ap())
    P.dma(fq[:, :, :], fq_h.ap())
    P.dma(sown[:, :, :], sown_h.ap())
    P.dma(eqc[:, :, :], eq_h.ap())
    P.dma(ioff[:, :], ioff_h.ap())
    P.dma(ptb_i[:, :], ptab_h.ap().partition_broadcast(128))

    wctr = [0]

    def stream(src, kch, cols):
        slot = wsl[wctr[0] % 3]
        wctr[0] += 1
        v = src.rearrange("(kc p) c -> p kc c", p=128)
        for k0 in range(0, kch, 4):
            k1 = min(kch, k0 + 4)
            P.dma(slot[:, k0:k1, 0:cols], v[:, k0:k1, :], q="pool")
        return slot

    import os as _os
    KSUB = int(_os.environ.get("KSUB", "99"))
    if KSUB < 1:
        P.emit(es); es.close(); return nc
    P.act(cvb[:, :, :], cvs[:, :, :], AF.Silu)
    pmc = BK_X
    for t in range(24 if KSUB >= 3 else 1):
        wt = stream(w_ada_h[:, t * 512:(t + 1) * 512], 16, 512)
        pb = bank()
        for kc in range(16):
            P.mm(pb[0:33, :], cvb[:, kc, :], wt[:, kc, :], start=(kc == 0), stop=(kc == 15))
        P.dma(brow[:, :], b_ada_h[:, t * 512:(t + 1) * 512])
        P.tt(mrow[:, :], pb[0:33, :], brow[:, :], ALU.add)
        for ri, r in enumerate((0, 32) if KSUB >= 2 else ()):
            for j in range(4):
                col = ri * 96 + t * 4 + j
                P.mm(pmc[:, col:col + 1], mrow[r:r + 1, j * 128:(j + 1) * 128], ones_f[r:r + 1, 0:1])
        which = {2: 0, 5: 1}.get(t // 4)
        if which is not None:
            cs = slice((t % 4) * 512, (t % 4 + 1) * 512)
            P.dma(grow[:, :], gpost_h[:, which, cs])
            P.tt(gtrow[:, which, cs], mrow[:, :], grow[:, :], ALU.mult)
    P.cp(modcol[:, :], pmc[:, 0:192])
    for ri in range(2):
        o = ri * 96
        P.stt(abv[:, ri, 0, :], modcol[:, o + 16:o + 32], 1.0, pvec[:, PV_GPRE_M:PV_GPRE_M + 16], ALU.add, ALU.mult)
        P.stt(abv[:, ri, 1, :], modcol[:, o + 64:o + 80], 1.0, pvec[:, PV_GPRE_F:PV_GPRE_F + 16], ALU.add, ALU.mult)

    def ab(ri, f, kc):
        o = ri * 96 + (48 if f else 0)
        return abv[:, ri, f, kc:kc + 1], modcol[:, o + kc:o + kc + 1]

    def rstd_from(out, in_, inv_n, M):
        P.act(out, in_, AF.Sqrt, bias=epsc[0:M, 0:1], scale=inv_n)
        P.add("dve", lambda e: e.reciprocal(out, out), reads=[out], writes=[out])

    def prologue(subs, f, colparams):
        for (si, M, coff) in subs:
            ssq = sml[0:M, 0:1]
            P.act(xn[0:M, 0, :], x1[0:M, si, :], AF.Square, accum=ssq)
            rs = sml[0:M, 1:2]
            rstd_from(rs, ssq, 1.0 / 2048, M)
            P.ts(xn[0:M, 0, :], x1[0:M, si, :], rs, None, op0=ALU.mult)
            for kc in range(16):
                pb = bank()
                pv = pb[:, :].bitcast(BF16)
                P.tr(pv[:, 0:M], xn[0:M, 0, kc * 128:(kc + 1) * 128], ident_b[0:M, 0:M])
                for (c0, c1, ri) in colparams:
                    a, b = ab(ri, f, kc)
                    if kc % 2 == 0:
                        P.act(hB[:, kc, coff + c0:coff + c1], pv[:, c0:c1], AF.Identity, bias=b, scale=a)
                    else:
                        P.ts(hB[:, kc, coff + c0:coff + c1], pv[:, c0:c1], a, b, op0=ALU.mult, op1=ALU.add)

    def linear_fm(wt, kch, rhs_kc, N, mch, cb):
        for m in range(mch):
            pb = bank()
            for kc in range(kch):
                P.mm(pb[:, 0:N], wt[:, kc, m * 128:(m + 1) * 128], rhs_kc(kc), start=(kc == 0), stop=(kc == kch - 1))
            cb(m, pb[:, 0:N])

    def fm_sumsq(chunks, N, pacc):
        n = len(chunks)
        for i, ch in enumerate(chunks):
            t = tmp32()
            P.act(t[:, 0:N], ch, AF.Square)
            P.mm(pacc[:, 0:N], ones_f[:, :], t[:, 0:N], start=(i == 0), stop=(i == n - 1))

    def fm_rmsnorm_to(chunks, N, gcol0, dst_of):
        fm_sumsq(chunks, N, BK_X)
        rt = tmp32()
        rstd_from(rt[:, 0:N], BK_X[:, 0:N], 1.0 / (128 * len(chunks)), 128)
        for i, ch in enumerate(chunks):
            P.stt(dst_of(i), ch, pvec[:, gcol0 + i:gcol0 + i + 1], rt[:, 0:N], ALU.mult, ALU.mult)

    def conv_post(ych, N):
        for i in range(8):
            P.mm(BK_X[:, 0:N], ones_f[:, :], ych(i), start=(i == 0), stop=(i == 7))
        fm_sumsq([ych(i) for i in range(8)], N, BK_Y)
        mean = lnm
        P.ts(mean[:, 0:N], BK_X[:, 0:N], 1.0 / 1024, None, op0=ALU.mult)
        msq = tmp32()
        P.tt(msq[:, 0:N], mean[:, 0:N], mean[:, 0:N], ALU.mult)
        var = lnv
        P.stt(var[:, 0:N], BK_Y[:, 0:N], 1.0 / 1024, msq[:, 0:N], ALU.mult, ALU.subtract)
        rstd_from(var[:, 0:N], var[:, 0:N], 1.0, 128)
        for i in range(8):
            y = ych(i)
            P.tt(y, y, mean[:, 0:N], ALU.subtract)
            P.tt(y, y, var[:, 0:N], ALU.mult)
            P.act(y, y, AF.Identity, bias=pvec[:, PV_BLN + i:PV_BLN + i + 1], scale=pvec[:, PV_GLN + i:PV_GLN + i + 1])
            sg = tmp32()
            P.act(sg[:, 0:N], y, AF.Sigmoid)
            P.tt(y, y, sg[:, 0:N], ALU.mult)

    def proj_post(w_h, kch_total, kgrp, lhs_kc, subs, ri, which, dst_cb):
        stages = [f32B[:, :, :].rearrange("p a b -> p (a b)"), f32C[:, :, :].rearrange("p a b -> p (a b)")]
        ngr = (kch_total + kgrp - 1) // kgrp
        r = 32 * ri
        for ct in range(4):
            pbs = [bank() for _ in subs]
            for g in range(ngr):
                k0 = g * kgrp
                kn = min(kgrp, kch_total - k0)
                wt = stream(w_h[k0 * 128:(k0 + kn) * 128, ct * 512:(ct + 1) * 512], kn, 512)
                for (si, M), pb in zip(subs, pbs):
                    for kk in range(kn):
                        kc = k0 + kk
                        P.mm(pb[0:M, :], lhs_kc(kc, si, M), wt[:, kk, :], start=(kc == 0), stop=(kc == kch_total - 1))
            for (si, M), pb in zip(subs, pbs):
                st = stages[si]
                P.cp(st[0:M, ct * 512:(ct + 1) * 512], pb[0:M, :])
                j = tmp32()
                P.act(j[0:M, :], pb[0:M, :], AF.Square, accum=sml[0:M, 8 + si * 4 + ct:9 + si * 4 + ct])
        for (si, M) in subs:
            st = stages[si]
            ss = sml[0:M, 16 + si:17 + si]
            P.red(ss, sml[0:M, 8 + si * 4:12 + si * 4])
            rstd_from(ss, ss, 1.0 / 2048, M)
            for ct in range(4):
                cs = slice(ct * 512, (ct + 1) * 512)
                pg = bank()
                P.mm(pg[0:M, :], ones_f[r:r + 1, 0:M], gtrow[r:r + 1, which, cs])
                tw = tmp32()
                P.stt(tw[0:M, :], st[0:M, cs], ss, pg[0:M, :], ALU.mult, ALU.mult)
                P.tt(st[0:M, cs], x1[0:M, si, cs], tw[0:M, :], ALU.add)
            dst_cb(si, M, st)

    def ffn(subs, colparams, N, ri, out_cb):
        prologue([(si, M, si * 128) for (si, M) in subs], 1, colparams)
        for g in range(11):
            wg = stream(w_gu_h[:, g * 512:(g + 1) * 512], 16, 512)
            wu = stream(w_gu_h[:, 5632 + g * 512:5632 + (g + 1) * 512], 16, 512)
            for m in range(4):
                pg = bank()
                for kc in range(16):
                    P.mm(pg[:, 0:N], wg[:, kc, m * 128:(m + 1) * 128], hB[:, kc, 0:N], start=(kc == 0), stop=(kc == 15))
                P.act(f32A[:, m, 0:N], pg[:, 0:N], AF.Silu)
            for m in range(4):
                pu = bank()
                for kc in range(16):
                    P.mm(pu[:, 0:N], wu[:, kc, m * 128:(m + 1) * 128], hB[:, kc, 0:N], start=(kc == 0), stop=(kc == 15))
                P.tt(bigA[:, g * 4 + m, 0:N], f32A[:, m, 0:N], pu[:, 0:N], ALU.mult)
        proj_post(w_dn_h, 44, 11, lambda kc, si, M: bigA[:, kc, si * 128:si * 128 + M], subs, ri, 1, out_cb)

    def mix_out(subs, N, ri):
        def dst(si, M, st):
            P.cp(x1[0:M, si, :], st[0:M, :])
        proj_post(w_out_h, 16, 16, lambda kc, si, M: bigA[:, 16 + kc, si * 128:si * 128 + M], subs, ri, 0, dst)

    def transpose_out(src_cc, ncols, dst_h):
        for cc in range(8):
            pb = bank()
            P.tr(pb[0:ncols, 0:128], src_cc(cc), ident_f[:, :])
            P.cp(cst[0:ncols, cc * 128:(cc + 1) * 128], pb[0:ncols, 0:128])
        P.dma(dst_h.ap(), cst[0:ncols, :])

    import os as _os
    STAGE = int(_os.environ.get("KSTAGE", "99"))
    kst = bigA[:, 0:8, :]
    vst = bigA[:, 8:16, :].rearrange("p (s a) b -> p s (a b)", s=2)
    KP1 = int(_os.environ.get("KP1", "99"))
    KP1SUB = int(_os.environ.get("KP1SUB", "99"))
    for ti in range(min(KP1, c.NT1) if STAGE >= 1 else 0):
        for si in range(2):
            P.dma(x1[:, si, :], xb_h[ti * 256 + si * 128:ti * 256 + (si + 1) * 128, :])
        prologue([(0, 128, 0), (1, 128, 128)], 0, [(0, 128, 0)])
        if KP1SUB < 1:
            continue
        for half in range(2):
            wt = stream(w_in_h[:, 1024 + half * 512:1024 + (half + 1) * 512], 16, 512)

            KD = int(_os.environ.get("KDBG", "7"))

            def cbk(m, pb, half=half):
                hh = half * 4 + m
                if KD & 2:
                    P.cp(kst[:, hh, :], pb, eng="act")
                if KD & 4:
                    P.red(kmsum[:, hh, ti:ti + 1], pb)
            if KD & 1:
                linear_fm(wt, 16, lambda kc: hB[:, kc, :], 256, 4, cbk)
        if KP1SUB < 2:
            continue
        P.dma(kf_h.ap()[:, :, ti * 256:(ti + 1) * 256].rearrange("h p t -> p h t"), kst, wk=[("kf", ti)])
        for half in range(2):
            wt = stream(w_in_h[:, 2048 + half * 512:2048 + (half + 1) * 512], 16, 512)
            for si in range(2):
                pb = bank()
                for kc in range(16):
                    P.mm(pb[:, :], hB[:, kc, si * 128:(si + 1) * 128], wt[:, kc, :], start=(kc == 0), stop=(kc == 15))
                P.cp(vst[:, si, half * 512:(half + 1) * 512], pb[:, :], eng=("act" if si else "dve"))
        P.dma(vt_h.ap()[ti * 256:(ti + 1) * 256, :].rearrange("(s p) c -> p s c", p=128), vst, wk=[("vt", ti)])
    kmT = sb("kmT", [128, 8, 16])
    P.ts(kmT[:, :, :], kmsum[:, :, :], 1.0 / 256, None, op0=ALU.mult)
    kf_keys = [("kf", i) for i in range(c.NT1)]
    vt_keys = [("vt", i) for i in range(c.NT1)]

    qf = bigA[:, 0:8, :]
    kfm = bigA[:, 8:16, :]
    cat = bigA[:, 16:32, :]
    vown = bigA[:, 32:40, :].rearrange("p (s a) b -> p s (a b)", s=2)
    kvst = [f32B[:, :, :].rearrange("p a b -> p (a b)"), f32C[:, :, :].rearrange("p a b -> p (a b)")]

    def inproj(N, subs, mini, tile_i):
        rhs = lambda kc: hB[:, kc, 0:N]
        for ct in range(4):
            wt = stream(w_in_h[:, 1024 + ct * 512:1024 + (ct + 1) * 512], 16, 512)
            for (si, M) in subs:
                pb = bank()
                for kc in range(16):
                    P.mm(pb[0:M, :], hB[:, kc, si * 128:si * 128 + M], wt[:, kc, :], start=(kc == 0), stop=(kc == 15))
                P.cp(kvst[si][0:M, ct * 512:(ct + 1) * 512], pb[0:M, :], eng=("act" if ct % 2 else "dve"))
        for (si, M) in subs:
            if mini:
                P.dma(k_s_h.ap().rearrange("h t d -> t h d"), kvst[0][0:4, 0:1024].rearrange("t (h d) -> t h d", h=8))
                P.dma(v_s_h.ap().rearrange("h t d -> t h d"), kvst[0][0:4, 1024:2048].rearrange("t (h d) -> t h d", h=8))
                P.cp(vn_b[0:4, :], kvst[0][0:4, 1024:2048])
            else:
                pg = tile_i * 2 + si
                P.dma(k_own_h.ap()[pg].rearrange("h t d -> t h d"), kvst[si][:, 0:1024].rearrange("t (h d) -> t h d", h=8))
                P.dma(v_own_h.ap()[pg].rearrange("h t d -> t h d"), kvst[si][:, 1024:2048].rearrange("t (h d) -> t h d", h=8))
                P.cp(vown[:, si, :], kvst[si][:, 1024:2048])
        for half in range(2):
            wt = stream(w_in_h[:, half * 512:(half + 1) * 512], 16, 512)

            def cbq(m, pb, half=half):
                hh = half * 4 + m
                P.act(qf[:, hh, 0:N], pb, AF.Copy, scale=SCALE)
                if mini:
                    P.ts(qs_f[:, hh, :], pb[:, 0:4], SCALE, None, op0=ALU.mult)
                    P.cp(qs_b[:, hh, :], qf[:, hh, 0:4])
                else:
                    q32 = tmp32()
                    P.ts(q32[:, 0:N], pb, SCALE, None, op0=ALU.mult)
                    select_main(hh, q32)
            linear_fm(wt, 16, rhs, N, 4, cbq)
        for half in range(2):
            wt = stream(w_in_h[:, 1024 + half * 512:1024 + (half + 1) * 512], 16, 512)

            def cbk(m, pb, half=half):
                hh = half * 4 + m
                P.cp(kfm[:, hh, 0:N], pb, eng="act")
                if mini:
                    P.cp(kn_b[:, hh, :], kfm[:, hh, 0:4])
            linear_fm(wt, 16, rhs, N, 4, cbk)
        for half in range(2):
            wt = stream(w_in_h[:, 3072 + half * 512:3072 + (half + 1) * 512], 16, 512)

            def cba(m, pb, half=half):
                P.cp(f32B[:, half * 4 + m, 0:N], pb, eng="act")
            linear_fm(wt, 16, rhs, N, 4, cba)
        for half in range(2):
            wt = stream(w_in_h[:, 4096 + half * 512:4096 + (half + 1) * 512], 16, 512)

            def cbg(m, pb, half=half):
                cc = half * 4 + m
                sg = tmp32()
                P.act(sg[:, 0:N], pb, AF.Sigmoid)
                if mini:
                    P.tt(usamp[:, cc, 0:N], f32B[:, cc, 0:N], sg[:, 0:N], ALU.mult)
                else:
                    P.tt(f32A[:, cc, 32:32 + N], f32B[:, cc, 0:N], sg[:, 0:N], ALU.mult)
            linear_fm(wt, 16, rhs, N, 4, cbg)

    def select_main(hh, q32):
        for si in range(2):
            pb = bank()
            P.mm(pb[:, 0:16], q32[:, si * 128:(si + 1) * 128], kmT[:, hh, :])
            s0 = sel[:, 0, :]
            P.tt(s0, pb[:, 0:16], vb_t[:, si, :], ALU.add)
            P.add("dve", lambda e: e.max(sel[:, 1, 0:8], sel[:, 0, :]), reads=[s0], writes=[sel[:, 1, 0:8]])
            P.ts(sel[:, 2, :], s0, sel[:, 1, 2:3], None, op0=ALU.is_ge)
            P.tt(sel[:, 2, :], sel[:, 2, :], v01_t[:, si, :], ALU.mult)
            P.ts(selb[:, :], sel[:, 2, :], -1.0, -NEGB, op0=ALU.add, op1=ALU.mult)
            pt = bank()
            pv = pt[:, :].bitcast(BF16)
            P.tr(pv[0:16, 0:128], selb[:, :], ident_b[:, :])
            P.cp(rall[0:16, hh, si * 128:(si + 1) * 128], pv[0:16, 0:128], eng="act")

    kA = scr16[:, 0:2048].bitcast(BF16)
    vA = scr16[:, 2048:4096].bitcast(BF16).rearrange("p (k d) -> p k d", d=128)

    def attention_main(per_head=None):
        for hh in range(8):
            P.dma(kA[:, 0:S], kf_h.ap()[hh], rk=kf_keys)
            P.dma(vA[:, 0:NKT, :], vt_h.ap()[:, hh * 128:(hh + 1) * 128].rearrange("(k p) d -> p k d", p=128), rk=vt_keys)
            tot = NKT + 2

            def emit_s(i):
                pS = bank()
                if i < NKT:
                    P.mm(pS[:, 0:256], kA[:, i * 128:(i + 1) * 128], qf[:, hh, :], start=True, stop=False)
                    P.mm(pS[:, 0:256], lpast[0:36, i * 128:(i + 1) * 128], rall[0:36, hh, :], start=False, stop=True)
                else:
                    jt = i - NKT
                    P.mm(pS[:, 0:256], kfm[:, hh, jt * 128:(jt + 1) * 128], qf[:, hh, :], start=True, stop=False)
                    P.mm(pS[:, 0:256], lpast[32:35, jt * 128:(jt + 1) * 128], rall[32:35, hh, :], start=False, stop=False)
                    P.mm(pS[:, 0:256], ident_b[:, :], caus[:, jt, :], start=False, stop=True)
                return pS
            LOOK = 2
            sb_ = {}
            for i in range(min(LOOK, tot)):
                sb_[i] = emit_s(i)
            for i in range(tot):
                pS = sb_.pop(i)
                if i < NKT:
                    vt_ = vA[:, i, :]
                else:
                    jt = i - NKT
                    vt_ = vown[:, jt, hh * 128:(hh + 1) * 128]
                pt = ptile()
                P.act(pt[:, :], pS[:, 0:256], AF.Exp)
                P.mm(BK_O[:, 0:256], vt_, pt[:, :], start=(i == 0), stop=(i == tot - 1))
                P.mm(BK_S[:, 0:256], ones_b[:, :], pt[:, :], start=(i == 0), stop=(i == tot - 1))
                if i + LOOK < tot:
                    sb_[i + LOOK] = emit_s(i + LOOK)
            if per_head is not None:
                per_head(hh)
            rs = tmp32()
            P.add("dve", lambda e, rs=rs: e.reciprocal(rs[:, 0:256], BK_S[:, 0:256]), reads=[BK_S[:, 0:256]], writes=[rs[:, 0:256]])
            P.tt(f32C[:, hh, :], BK_O[:, 0:256], rs[:, 0:256], ALU.mult)

    O_s = scr16[:, 0:2048].rearrange("p (h n q) -> p h n q", h=8, q=4)
    L_s = scr16[:, 2048:4096].rearrange("p (h n q) -> p h n q", h=8, q=4)
    kpg = [f32B[:, :, :].rearrange("p a b -> p (a b)")[:, 0:1024].rearrange("p (h d) -> p h d", h=8),
           f32B[:, :, :].rearrange("p a b -> p (a b)")[:, 1024:2048].rearrange("p (h d) -> p h d", h=8)]
    vpg = [f32C[:, :, :].rearrange("p a b -> p (a b)")[:, 0:1024].rearrange("p (h d) -> p h d", h=8),
           f32C[:, :, :].rearrange("p a b -> p (a b)")[:, 1024:2048].rearrange("p (h d) -> p h d", h=8)]
    vpb = f32A[:, :, :].rearrange("p a b -> p (a b)")[:, 0:1024].bitcast(BF16).rearrange("p (e h d) -> p e h d", e=2, h=8)

    def attention_sample():
        P.cp(ptb_f[:, :], ptb_i[:, :])
        P.ts(idx_f, ptb_f[:, :], 128.0, ioff[:, 0:1], op0=ALU.mult, op1=ALU.add)
        P.cp(idx_i[:, :], idx_f)
        qall = qs_b[:, :, :].rearrange("p h q -> p (h q)")
        kflat = f32B[:, :, :].rearrange("p a b -> p (a b)")
        vflat = f32C[:, :, :].rearrange("p a b -> p (a b)")
        vpb4 = f32A[:, :, :].rearrange("p a b -> p (a b)")[:, 0:1024].bitcast(BF16).rearrange("p (e r d) -> p e r d", e=2, r=8)
        for n in range(c.NSB):
            for e_ in range(2):
                j = 2 * n + e_
                ix = idx_i[:, j:j + 1]
                ko = kflat[:, e_ * 1024:(e_ + 1) * 1024]
                vo = vflat[:, e_ * 1024:(e_ + 1) * 1024]
                P.add("pool", lambda e, o=ko, ix=ix: e.indirect_dma_start(
                    out=o, out_offset=None, in_=ck_h.ap(), in_offset=bass.IndirectOffsetOnAxis(ap=ix, axis=0)),
                    reads=[ix], writes=[ko], dma=True)
                P.add("pool", lambda e, o=vo, ix=ix: e.indirect_dma_start(
                    out=o, out_offset=None, in_=cv_h.ap(), in_offset=bass.IndirectOffsetOnAxis(ap=ix, axis=0)),
                    reads=[ix], writes=[vo], dma=True)
                P.cp(vpb4[:, e_, :, :], vo.rearrange("p (r d) -> p r d", r=8))
            for e_ in range(2):
                j = 2 * n + e_
                ko = kflat[:, e_ * 1024:(e_ + 1) * 1024]
                kt = KT[e_]
                for half in range(2):
                    pT = bank()
                    for rr in range(4):
                        r = half * 4 + rr
                        P.tr(pT[:, rr * 128:(rr + 1) * 128], ko[:, r * 128:(r + 1) * 128], ident_f[:, :])
                    P.cp(kt[:, half * 4:(half + 1) * 4, :], pT[:, :].rearrange("p (r k) -> p r k", r=4), eng="act")
                P.red(ksum[:, :, j], kt[:, :, :].rearrange("p r (h g) -> p h r g", h=8), axis=AX.XY)
                pS = bank()
                for r in range(8):
                    P.mm(pS[:, r * 32:(r + 1) * 32], kt[:, r, :], qall)
                t1 = tmp32()
                t3 = t1[:, 0:256].rearrange("p (r k) -> p r k", r=8)
                P.tt(t3, pS[:, 0:256].rearrange("p (r k) -> p r k", r=8),
                     sbias[:, j, :].unsqueeze(2).to_broadcast([128, 8, 32]), ALU.add)
                P.act(t1[:, 0:256], t1[:, 0:256], AF.Exp)
                P.tt(Pb[e_][:, :, :], t3, bmask[:, :].unsqueeze(1).to_broadcast([128, 8, 32]), ALU.mult)
            cnt = 0
            for e_ in range(2):
                for r in range(8):
                    P.mm(BK_O[:, 0:32], vpb4[:, e_, r, :], Pb[e_][:, r, :], start=(cnt == 0), stop=(cnt == 15))
                    cnt += 1
            cnt = 0
            for e_ in range(2):
                for r in range(8):
                    P.mm(BK_S[:, 0:32], ones_b[:, :], Pb[e_][:, r, :], start=(cnt == 0), stop=(cnt == 15))
                    cnt += 1
            P.cp(O_s[:, :, n, :], BK_O[:, 0:32].rearrange("p (h q) -> p h q", q=4))
            P.cp(L_s[:, :, n, :], BK_S[:, 0:32].rearrange("p (h q) -> p h q", q=4), eng="act")
        NSB = c.NSB
        P.red(kms[:, :, :], ksum[:, :, :].rearrange("p h (n e) -> p h n e", e=2))
        P.ts(kms[:, :, :], kms[:, :, :], 1.0 / 256, None, op0=ALU.mult)
        ncol = max(NSB, 8)
        for hh in range(8):
            pb = bank()
            P.mm(pb[0:4, 0:NSB], qs_f[:, hh, :], kms[:, hh, :])
            P.memset(sel[0:4, :, :], -1e9)
            sv = sel[0:4, :, :].rearrange("p a b -> p (a b)")
            P.cp(sv[:, 0:NSB], pb[0:4, 0:NSB])
            P.add("dve", lambda e, sv=sv: e.max(sml[0:4, 32:40], sv[:, 0:64]), reads=[sv[:, 0:64]], writes=[sml[0:4, 32:40]])
            m01 = tmp32()
            P.ts(m01[0:4, 0:NSB], sv[:, 0:NSB], sml[0:4, 34:35], None, op0=ALU.is_ge)
            pB = bank()
            for q in range(4):
                P.mm(pB[:, q * NSB:(q + 1) * NSB], eqc[0:4, q, :], m01[0:4, 0:NSB])
            pBv = pB[:, 0:4 * NSB].rearrange("p (q n) -> p q n", q=4)
            tm = tmp32()
            tmv = tm[:, 0:4 * NSB].rearrange("p (q n) -> p q n", q=4)
            P.tt(tmv, O_s[:, hh, 0:NSB, :].rearrange("p n q -> p q n"), pBv, ALU.mult)
            P.red(sml[:, 40:44], tmv)
            tm2 = tmp32()
            tmv2 = tm2[:, 0:4 * NSB].rearrange("p (q n) -> p q n", q=4)
            P.tt(tmv2, L_s[:, hh, 0:NSB, :].rearrange("p n q -> p q n"), pBv, ALU.mult)
            P.red(sml[:, 44:48], tmv2)
            pS = bank()
            P.mm(pS[0:4, 0:4], kn_b[:, hh, :], qs_b[:, hh, :])
            so = tmp32()
            P.tt(so[0:4, 0:4], pS[0:4, 0:4], sown[:, hh, :], ALU.add)
            P.act(sexp[0][0:4, 0:4], so[0:4, 0:4], AF.Exp)
            P.mm(BK_O[:, 0:4], vn_b[0:4, hh * 128:(hh + 1) * 128], sexp[0][0:4, 0:4])
            P.mm(BK_S[:, 0:4], ones_b[0:4, :], sexp[0][0:4, 0:4])
            P.tt(sml[:, 40:44], sml[:, 40:44], fq[:, hh, :], ALU.mult)
            P.tt(sml[:, 44:48], sml[:, 44:48], fq[:, hh, :], ALU.mult)
            P.tt(sml[:, 40:44], sml[:, 40:44], BK_O[:, 0:4], ALU.add)
            P.tt(sml[:, 44:48], sml[:, 44:48], BK_S[:, 0:4], ALU.add)
            P.add("dve", lambda e: e.reciprocal(sml[:, 44:48], sml[:, 44:48]), reads=[sml[:, 44:48]], writes=[sml[:, 44:48]])
            P.tt(oat_s[:, hh, :], sml[:, 40:44], sml[:, 44:48], ALU.mult)

    def finish():
        P.emit(es)
        es.close()
        return nc
    if STAGE < 2:
        return finish()
    P.dma(x1[0:c.MW, 0, :], xmini_h.ap())
    prologue([(0, c.MW, 0)], 0, [(0, 4, 1), (4, c.MW, 0)])
    inproj(c.MW, [(0, 4)], True, 0)
    for cc in range(8):
        P.ts(uhalo[:, cc, :], usamp[:, cc, 4:36], hflag[:, 0:1], None, op0=ALU.mult)
    if STAGE < 3:
        return finish()
    P.dma(cst[0:30, :], sconv_h.ap())
    for cc in range(8):
        pb = bank()
        P.tr(pb[:, 0:30], cst[0:30, cc * 128:(cc + 1) * 128], ident_f[0:30, 0:30])
        P.cp(ue_s[:, cc, 0:30], pb[:, 0:30])
        P.cp(ue_s[:, cc, 30:34], usamp[:, cc, 0:4])
    for cc in range(8):
        for t_ in range(4):
            tw = tmp32()
            P.tt(tw[:, 0:31], pvec[:, PV_WDW + cc * 31:PV_WDW + (cc + 1) * 31], ue_s[:, cc, t_:t_ + 31], ALU.mult)
            P.red(y_sm[:, cc, t_:t_ + 1], tw[:, 0:31])
        P.ts(y_sm[:, cc, :], y_sm[:, cc, :], pvec[:, PV_BDW + cc:PV_BDW + cc + 1], None, op0=ALU.add)
    transpose_out(lambda cc: ue_s[:, cc, 4:34], 30, conv_s_h)
    if STAGE < 4:
        return finish()
    attention_sample()
    if STAGE < 5:
        return finish()
    fm_rmsnorm_to([oat_s[:, hh, :] for hh in range(8)], 4, PV_GATTN, lambda i: cat[:, i, 0:4])
    conv_post(lambda cc: y_sm[:, cc, :], 4)
    fm_rmsnorm_to([y_sm[:, cc, :] for cc in range(8)], 4, PV_GCONV, lambda i: cat[:, 8 + i, 0:4])
    mix_out([(0, 4)], 4, 1)

    def out_s(si, M, st):
        P.dma(y_s_h.ap(), st[0:4, :])
    ffn([(0, 4)], [(0, 4, 1)], 4, 1, out_s)

    if STAGE < 6:
        return finish()
    for it in range(c.NOWN):
        for si in range(2):
            r0 = it * 256 + si * 128
            P.dma(x1[:, si, :], xown_h[r0:r0 + 128, :])
            P.dma(vb_t[:, si, :], vbias_h[r0:r0 + 128, :])
            P.dma(v01_t[:, si, :], v01_h[r0:r0 + 128, :])
        P.dma(rall[32:36, :, :], rc_h[it])
        subs = [(0, 128), (1, 128)]
        prologue([(0, 128, 0), (1, 128, 128)], 0, [(0, 128, 0)])
        inproj(256, subs, False, it)
        for cc in range(8):
            P.cp(f32A[:, cc, 0:32], uhalo[:, cc, :], eng="act")

        def conv_chunk(cc):
            y = f32B[:, cc, :]
            w0 = PV_WDW + cc * 31
            P.ts(y, f32A[:, cc, 2:258], pvec[:, w0:w0 + 1], pvec[:, PV_BDW + cc:PV_BDW + cc + 1], op0=ALU.mult, op1=ALU.add)
            for j in range(1, 31):
                P.stt(y, f32A[:, cc, 2 + j:258 + j], pvec[:, w0 + j:w0 + j + 1], y, ALU.mult, ALU.add)
        attention_main(conv_chunk)
        if it == c.NOWN - 1:
            transpose_out(lambda cc: f32A[:, cc, 258:288], 30, convp_h)
        for cc in range(8):
            P.cp(uhalo[:, cc, :], f32A[:, cc, 256:288], eng="act")
        fm_rmsnorm_to([f32C[:, hh, :] for hh in range(8)], 256, PV_GATTN, lambda i: cat[:, i, :])
        conv_post(lambda cc: f32B[:, cc, :], 256)
        fm_rmsnorm_to([f32B[:, cc, :] for cc in range(8)], 256, PV_GCONV, lambda i: cat[:, 8 + i, :])
        mix_out(subs, 256, 0)

        def out_m(si, M, st, it=it):
            r0 = it * 256 + si * 128
            P.dma(y_own_h.ap()[r0:r0 + 128, :], st[:, :])
        ffn(subs, [(0, 128, 0)], 256, 0, out_m)

    P.emit(es)
    es.close()
    return nc


def _bf(a):
    return np.asarray(a, dtype=np.float32).astype(ml_dtypes.bfloat16)


def host_consts(cfg, qstart):
    c = cfg
    m = (2.0 ** (-8.0 * (np.arange(8) + 1.0) / 8)).astype(np.float64)
    NKT = c.NKT
    lp = np.zeros((36, NKT * 128), np.float32)
    for kt in range(NKT):
        sl = slice(kt * 128, (kt + 1) * 128)
        if kt // 2 < 16:
            lp[kt // 2, sl] = 1.0
        lp[32, sl] = 1.0
        lp[33, sl] = np.arange(128)
        lp[34, sl] = 128.0 * kt
        lp[35, sl] = 1.0
    j = np.arange(256)
    caus = np.zeros((128, 2, 256), np.float32)
    for jt in range(2):
        kidx = 128 * jt + np.arange(128)
        caus[:, jt, :] = np.where(kidx[:, None] <= j[None, :], 0.0, NEGB)
    rc = np.zeros((c.NOWN, 4, 8, 256), np.float32)
    for it in range(c.NOWN):
        q0 = qstart + it * 256
        for h in range(8):
            rc[it, 0, h] = -m[h] * j
            rc[it, 1, h] = m[h]
            rc[it, 2, h] = m[h]
            rc[it, 3, h] = -m[h] * q0
    tq = qstart + np.arange(c.OWN)
    nfull = tq // 256
    v01 = (np.arange(16)[None, :] < nfull[:, None]).astype(np.float32)
    vbias = np.where(v01 > 0, 0.0, -1e9).astype(np.float32)
    past = c.NPG * 128
    sbias = np.zeros((128, c.NPG, 8), np.float32)
    bmask = np.zeros((128, 32), np.float32)
    for h in range(8):
        bmask[h * 16:(h + 1) * 16, h * 4:(h + 1) * 4] = 1.0
        for g in range(16):
            for jp in range(c.NPG):
                sbias[h * 16 + g, jp, :] = m[h] * (128 * jp + 8 * g + np.arange(8) - past)
    fq = np.zeros((128, 8, 4), np.float32)
    sown = np.zeros((4, 8, 4), np.float32)
    for h in range(8):
        for q in range(4):
            fq[:, h, q] = np.exp(-m[h] * q)
            for k in range(4):
                sown[k, h, q] = -m[h] * (q - k) if k <= q else NEGB
    eqc = np.zeros((4, 4, 128), np.float32)
    for q in range(4):
        eqc[q, q, :] = 1.0
    ioff = (np.arange(128)[:, None] + 128.0 * np.arange(8)[None, :]).astype(np.float32)
    return dict(lpast=_bf(lp), caus=_bf(caus), rc=_bf(rc), vbias=vbias, v01=v01, sbias=sbias, bmask=bmask, fq=fq,
                sown=sown, eqc=eqc, ioff=ioff, ident_f=np.eye(128, dtype=np.float32), ident_b=_bf(np.eye(128)))


def core_inputs(cfg, inp, b, qstart, sb_):
    c = cfg
    f = np.float32
    d = host_consts(c, qstart)
    col = lambda v: np.ascontiguousarray(np.asarray(v, f).reshape(-1, 128).T)
    pv = np.zeros((128, NPV), f)
    pv[:, PV_GPRE_M:PV_GPRE_M + 16] = col(inp["g_pre_mix"][0])
    pv[:, PV_GPRE_F:PV_GPRE_F + 16] = col(inp["g_pre_ffn"][0])
    pv[:, PV_GATTN:PV_GATTN + 8] = col(inp["g_attn_out"][0])
    pv[:, PV_GCONV:PV_GCONV + 8] = col(inp["g_conv_out"][0])
    pv[:, PV_GLN:PV_GLN + 8] = col(inp["g_conv_ln"][0])
    pv[:, PV_BLN:PV_BLN + 8] = col(inp["b_conv_ln"][0])
    pv[:, PV_BDW:PV_BDW + 8] = col(inp["b_dw"][0])
    wdw = np.asarray(inp["w_dw"], f)[0, :, 0, :]
    pv[:, PV_WDW:] = wdw.reshape(31, 8, 128).transpose(2, 1, 0).reshape(128, 8 * 31)
    d["pvec"] = pv
    cv = np.zeros((128, 16, 33), f)
    cv[:, :, 0] = col(inp["c_prompt"][b])
    cv[:, :, 32] = col(inp["c_sample"][sb_])
    d["cvec"] = cv
    d["w_ada"] = np.asarray(inp["w_ada"], f)[0]
    ba = np.zeros((33, 12288), f)
    ba[0] = inp["b_ada"][0]
    ba[32] = inp["b_ada"][0]
    d["b_ada33"] = ba
    gp = np.zeros((33, 2, 2048), f)
    for r in (0, 32):
        gp[r, 0] = inp["g_post_mix"][0]
        gp[r, 1] = inp["g_post_ffn"][0]
    d["gpost33"] = gp
    d["w_in"] = np.asarray(inp["w_in"], f)[0]
    d["w_out"] = np.asarray(inp["w_out"], f)[0]
    d["w_gu"] = np.asarray(inp["w_gate_up"], f)[0]
    d["w_down"] = np.asarray(inp["w_down"], f)[0]
    xp = np.asarray(inp["x_prompt"], f)
    d["xb"] = xp[b]
    d["xown"] = xp[b, qstart:qstart + c.OWN]
    xm = np.zeros((c.MW, 2048), f)
    xm[0:4] = np.asarray(inp["x_sample"], f)[sb_]
    if qstart > 0:
        xm[4:36] = xp[b, qstart - 32:qstart]
    d["xmini"] = xm
    d["haloflag"] = np.full((128, 1), 1.0 if qstart > 0 else 0.0, f)
    d["cache_k"] = np.asarray(inp["cache_k"], f)[0].reshape(-1, 1024)
    d["cache_v"] = np.asarray(inp["cache_v"], f)[0].reshape(-1, 1024)
    d["ptab"] = np.asarray(inp["page_table"], np.int32)[sb_:sb_ + 1]
    d["sconv"] = np.asarray(inp["state_conv"], f)[0, sb_]
    return d


_NC_CACHE = {}


def run_cores(cfg, inp, assign):
    key = (cfg.S, cfg.OWN, cfg.NPG, cfg.NPOOL)
    if key not in _NC_CACHE:
        _NC_CACHE[key] = build(cfg)
    nc = _NC_CACHE[key]
    maps = [core_inputs(cfg, inp, b, q, s) for (b, q, s) in assign]
    res = run_bass_kernel_spmd(nc, maps, core_ids=list(range(len(assign))))
    return res.results


def kernel(**inp):
    cfg = Cfg()
    assign = [(cid // 4, (cid % 4) * 1024, cid) for cid in range(8)]
    r = run_cores(cfg, inp, assign)
    f = np.float32
    y_p = np.zeros((2, 4096, 2048), f)
    k_p = np.zeros((1, 2, 32, 8, 128, 128), f)
    v_p = np.zeros((1, 2, 32, 8, 128, 128), f)
    cv_p = np.zeros((1, 2, 30, 1024), f)
    y_s = np.zeros((8, 4, 2048), f)
    k_s = np.zeros((1, 8, 8, 4, 128), f)
    v_s = np.zeros((1, 8, 8, 4, 128), f)
    cv_s = np.zeros((1, 8, 30, 1024), f)
    for cid, (b, q, s) in enumerate(assign):
        o = r[cid]
        y_p[b, q:q + 1024] = o["y_own"]
        k_p[0, b, q // 128:q // 128 + 8] = o["k_own"]
        v_p[0, b, q // 128:q // 128 + 8] = o["v_own"]
        if q == 3072:
            cv_p[0, b] = o["convp"]
        y_s[s] = o["y_s"]
        k_s[0, s] = o["k_s"]
        v_s[0, s] = o["v_s"]
        cv_s[0, s] = o["conv_s"]
    return (y_p, y_s, k_p, v_p, cv_p, k_s, v_s, cv_s)
```

```python
import numpy as np
import concourse.bass as bass
import concourse.mybir as mybir

F32 = mybir.dt.float32
BF16 = mybir.dt.bfloat16
I32 = mybir.dt.int32
AF = mybir.ActivationFunctionType
ALU = mybir.AluOpType
AX = mybir.AxisListType

GRAN = 64
NDSEM = 20


def _esize(dt):
    if dt == BF16:
        return 2
    return 4


class Prog:
    def __init__(self, nc):
        self.nc = nc
        self.ins = []
        self.lastw = {}
        self.readers = {}
        self.tbytes = {}
        self.psum = set()
        self.dma_n = {"sp": 0, "pool": 0}
        self.dma_last = {}

    def reg(self, t, nbytes, psum=False):
        self.tbytes[t.name] = nbytes
        if psum:
            self.psum.add(t.name)

    def keys(self, x):
        if isinstance(x, tuple):
            return [x]
        name = x.tensor.name
        pb = self.tbytes.get(name)
        if pb is None or name in self.psum:
            return [(name,)]
        es = _esize(x.dtype)
        off = (x.offset * es) % pb
        span = es
        for (st, cnt) in x.ap[1:]:
            span += abs(st) * (cnt - 1) * es
        g0 = off // GRAN
        g1 = (off + span - 1) // GRAN
        return [(name, g) for g in range(g0, g1 + 1)]

    def add(self, eng, fn, reads=(), writes=(), dma=False):
        idx = len(self.ins)
        deps = set()
        rk = [k for r in reads for k in self.keys(r)]
        wk = [k for w in writes for k in self.keys(w)]
        pk = [k for k in rk if k[0] in self.psum]
        if pk:
            rk = [k for k in rk if k[0] not in self.psum]
            wk = wk + pk
        for k in rk:
            d = self.lastw.get(k)
            if d is not None:
                deps.add(d)
        for k in wk:
            d = self.lastw.get(k)
            if d is not None:
                deps.add(d)
            for r in self.readers.get(k, ()):
                deps.add(r)
        for k in rk:
            self.readers.setdefault(k, []).append(idx)
        for k in wk:
            self.lastw[k] = idx
            self.readers[k] = []
        sem = None
        if dma:
            n = self.dma_n[eng]
            self.dma_n[eng] = n + 1
            sem = (eng, n % NDSEM)
            prev = self.dma_last.get(sem)
            if prev is not None:
                deps.add(prev)
            self.dma_last[sem] = idx
        deps.discard(idx)
        self.ins.append(dict(eng=eng, fn=fn, deps=deps, dma=dma, sig=bool(dma), sem=sem, val=0))
        return idx

    def emit(self, es):
        nc = self.nc
        ins = self.ins
        for i in ins:
            for d in i["deps"]:
                di = ins[d]
                if i["eng"] == "pe" and di["eng"] == "pe" and not di["dma"] and not i["dma"]:
                    continue
                di["sig"] = True
        cnt = {}
        for i in ins:
            if not i["sig"]:
                continue
            if i["dma"]:
                s = i["sem"]
                cnt[s] = cnt.get(s, 0) + 16
            else:
                s = (i["eng"], "c")
                i["sem"] = s
                cnt[s] = cnt.get(s, 0) + 1
            i["val"] = cnt[s]
        semobj = {}
        for s in cnt:
            semobj[s] = es.enter_context(nc.semaphore("s_%s_%s" % s))
        final = dict(cnt)

        def make(engname):
            def body(e):
                waited = {}
                for i in ins:
                    if i["eng"] != engname:
                        continue
                    need = {}
                    for d in sorted(i["deps"]):
                        di = ins[d]
                        if engname == "pe" and di["eng"] == "pe" and not di["dma"] and not i["dma"]:
                            continue
                        s = di["sem"]
                        v = di["val"]
                        if need.get(s, 0) < v:
                            need[s] = v
                    for s, v in need.items():
                        if waited.get(s, 0) < v:
                            e.wait_ge(semobj[s], v)
                            waited[s] = v
                    r = i["fn"](e)
                    if i["sig"]:
                        r.then_inc(semobj[i["sem"]], 16 if i["dma"] else 1)
                if engname == "sp":
                    for s, v in final.items():
                        if s[1] != "c" and waited.get(s, 0) < v:
                            e.wait_ge(semobj[s], v)
            return body

        block = es.enter_context(nc.Block())
        block.tensor(make("pe"))
        block.scalar(make("act"))
        block.vector(make("dve"))
        block.gpsimd(make("pool"))
        block.sync(make("sp"))

    def mm(self, out, lhsT, rhs, start=True, stop=True):
        self.add("pe", lambda e: e.matmul(out, lhsT, rhs, start=start, stop=stop),
                 reads=[lhsT, rhs], writes=[out])

    def tr(self, out, in_, ident):
        self.add("pe", lambda e: e.transpose(out, in_, ident), reads=[in_, ident], writes=[out])

    def act(self, out, in_, func, bias=None, scale=None, accum=None):
        kw = {}
        rd = [in_]
        if bias is not None:
            kw["bias"] = bias
            if not isinstance(bias, (int, float)):
                rd.append(bias)
        if scale is not None:
            kw["scale"] = scale
            if not isinstance(scale, (int, float)):
                rd.append(scale)
        wr = [out]
        if accum is not None:
            kw["accum_out"] = accum
            wr.append(accum)
            self.memset(accum, 0.0)
        self.add("act", lambda e: e.activation(out, in_, func, **kw), reads=rd, writes=wr)

    def tt(self, out, in0, in1, op, eng="dve"):
        self.add(eng, lambda e: e.tensor_tensor(out, in0, in1, op), reads=[in0, in1], writes=[out])

    def ts(self, out, in0, s1, s2=None, op0=ALU.mult, op1=None, eng="dve", accum=None):
        rd = [in0]
        if not isinstance(s1, (int, float)):
            rd.append(s1)
        if s2 is not None and not isinstance(s2, (int, float)):
            rd.append(s2)
        kw = {}
        if op1 is not None:
            kw["op1"] = op1
        wr = [out]
        if accum is not None:
            kw["accum_out"] = accum
            wr.append(accum)
        self.add(eng, lambda e: e.tensor_scalar(out, in0, s1, s2, op0, **kw), reads=rd, writes=wr)

    def stt(self, out, in0, s, in1, op0, op1, eng="dve"):
        rd = [in0, in1]
        if not isinstance(s, (int, float)):
            rd.append(s)
        self.add(eng, lambda e: e.scalar_tensor_tensor(out, in0, s, in1, op0, op1), reads=rd, writes=[out])

    def cp(self, out, in_, eng="dve"):
        if eng == "act":
            self.act(out, in_, AF.Copy)
        else:
            self.add(eng, lambda e: e.tensor_copy(out, in_), reads=[in_], writes=[out])

    def red(self, out, in_, op=ALU.add, eng="dve", axis=AX.X):
        self.add(eng, lambda e: e.tensor_reduce(out, in_, axis, op), reads=[in_], writes=[out])

    def memset(self, out, v, eng="dve"):
        self.add(eng, lambda e: e.memset(out, v), writes=[out])

    def dma(self, out, in_, q="sp", rk=None, wk=None, **kw):
        rd = [in_] if rk is None else list(rk)
        wr = [out] if wk is None else list(wk)
        self.add(q, lambda e: e.dma_start(out=out, in_=in_, **kw), reads=rd, writes=wr, dma=True)
from contextlib import ExitStack
import ml_dtypes
from concourse.bass_utils import run_bass_kernel_spmd

EPS = 1e-6
SCALE = 128.0 ** -0.5
NEGB = -30000.0


class Cfg:
    D = 2048
    KC = 16
    H = 8
    CC = 8
    FF = 5632
    FC = 44
    T = 256
    DS = 4
    HALO = 32
    MW = 36

    def __init__(self, S=4096, OWN=1024, NPG=128, NPOOL=1280):
        self.S = S
        self.OWN = OWN
        self.NPG = NPG
        self.NPOOL = NPOOL
        self.NKT = S // 128
        self.NBLK = S // 256
        self.NOWN = OWN // 256
        self.NT1 = S // 256
        self.NSB = NPG // 2


PV_GPRE_M, PV_GPRE_F, PV_GATTN, PV_GCONV, PV_GLN, PV_BLN, PV_BDW, PV_WDW = 0, 16, 32, 40, 48, 56, 64, 72
NPV = 72 + 8 * 31


def build(cfg):
    c = cfg
    nc = bass.Bass("TRN2", target_bir_lowering=False)
    P = Prog(nc)
    es = ExitStack()
    T, NKT, S = c.T, c.NKT, c.S

    def din(name, shape, dt=F32):
        return nc.dram_tensor(name, list(shape), dt, kind="ExternalInput")

    def dout(name, shape, dt=F32):
        return nc.dram_tensor(name, list(shape), dt, kind="ExternalOutput")

    ident_f_h = din("ident_f", [128, 128])
    ident_b_h = din("ident_b", [128, 128], BF16)
    cvec_h = din("cvec", [128, 16, 33])
    w_ada_h = din("w_ada", [2048, 12288])
    b_ada_h = din("b_ada33", [33, 12288])
    gpost_h = din("gpost33", [33, 2, 2048])
    pvec_h = din("pvec", [128, NPV])
    w_in_h = din("w_in", [2048, 5120])
    w_out_h = din("w_out", [2048, 2048])
    w_gu_h = din("w_gu", [2048, 11264])
    w_dn_h = din("w_down", [5632, 2048])
    xb_h = din("xb", [S, 2048])
    xown_h = din("xown", [c.OWN, 2048])
    xmini_h = din("xmini", [c.MW, 2048])
    hflag_h = din("haloflag", [128, 1])
    lpast_h = din("lpast", [36, NKT * 128], BF16)
    caus_h = din("caus", [128, 2, 256], BF16)
    rc_h = din("rc", [c.NOWN, 4, 8, 256], BF16)
    vbias_h = din("vbias", [c.OWN, 16])
    v01_h = din("v01", [c.OWN, 16])
    ck_h = din("cache_k", [c.NPOOL * 128, 1024])
    cv_h = din("cache_v", [c.NPOOL * 128, 1024])
    ptab_h = din("ptab", [1, c.NPG], I32)
    ioff_h = din("ioff", [128, 8])
    sconv_h = din("sconv", [30, 1024])
    sbias_h = din("sbias", [128, c.NPG, 8])
    bmask_h = din("bmask", [128, 32])
    fq_h = din("fq", [128, 8, 4])
    sown_h = din("sown", [4, 8, 4])
    eq_h = din("eqc", [4, 4, 128])

    y_own_h = dout("y_own", [c.OWN, 2048])
    k_own_h = dout("k_own", [c.OWN // 128, 8, 128, 128])
    v_own_h = dout("v_own", [c.OWN // 128, 8, 128, 128])
    convp_h = dout("convp", [30, 1024])
    y_s_h = dout("y_s", [4, 2048])
    k_s_h = dout("k_s", [8, 4, 128])
    v_s_h = dout("v_s", [8, 4, 128])
    conv_s_h = dout("conv_s", [30, 1024])

    kf_h = nc.dram_tensor("kf_scr", [8, 128, S], BF16)
    vt_h = nc.dram_tensor("vt_scr", [S, 1024], BF16)

    def sb(name, shape, dt=F32):
        t = es.enter_context(nc.sbuf_tensor(name, list(shape), dt))
        n = 1
        for s_ in shape[1:]:
            n *= s_
        P.reg(t, n * (2 if dt == BF16 else 4))
        return t

    banks = []
    for i in range(8):
        t = es.enter_context(nc.psum_tensor("pb%d" % i, [128, 512], F32))
        P.reg(t, 2048, psum=True)
        banks.append(t)
    rot = [0]

    def bank():
        b = banks[rot[0] % 4]
        rot[0] += 1
        return b
    BK_O, BK_S, BK_X, BK_Y = banks[4], banks[5], banks[6], banks[7]

    ident_f = sb("identf", [128, 128])
    ident_b = sb("identb", [128, 128], BF16)
    ones_f = sb("onesf", [128, 128])
    ones_b = sb("onesb", [128, 128], BF16)
    epsc = sb("epsc", [128, 1])
    pvec = sb("pvecs", [128, NPV])
    modcol = sb("modcol", [128, 192])
    abv = sb("abv", [128, 2, 2, 16])
    gtrow = sb("gtrow", [33, 2, 2048])
    lpast = sb("lpasts", [36, NKT * 128], BF16)
    caus = sb("causs", [128, 2, 256], BF16)
    rall = sb("rall", [36, 8, 256], BF16)
    kmsum = sb("kmsum", [128, 8, 16])
    hflag = sb("hflag", [128, 1])
    wsl = [sb("wsl%d" % i, [128, 16, 512], BF16) for i in range(3)]
    x1 = sb("x1", [128, 2, 2048])
    xn = sb("xn", [128, 1, 2048], BF16)
    hB = sb("hB", [128, 16, 256], BF16)
    bigA = sb("bigA", [128, 44, 256], BF16)
    f32A = sb("f32A", [128, 8, 288])
    f32B = sb("f32B", [128, 8, 256])
    f32C = sb("f32C", [128, 8, 256])
    scr16 = sb("scr16", [128, 4096])
    ptl = [sb("ptl%d" % i, [128, 256], BF16) for i in range(3)]
    t32 = [sb("t32_%d" % i, [128, 512]) for i in range(4)]
    sml = sb("sml", [128, 64])
    lnm = sb("lnm", [128, 256])
    lnv = sb("lnv", [128, 256])
    sel = sb("sel", [128, 4, 16])
    selb = sb("selb", [128, 16], BF16)
    vb_t = sb("vb_t", [128, 2, 16])
    v01_t = sb("v01_t", [128, 2, 16])
    uhalo = sb("uhalo", [128, 8, 32])
    usamp = sb("usamp", [128, 8, 36])
    qs_b = sb("qs_b", [128, 8, 4], BF16)
    qs_f = sb("qs_f", [128, 8, 4])
    kn_b = sb("kn_b", [128, 8, 4], BF16)
    vn_b = sb("vn_b", [4, 1024], BF16)
    kms = sb("kms", [128, 8, c.NSB])
    sbias = sb("sbias_s", [128, c.NPG, 8])
    bmask = sb("bmask_s", [128, 32])
    KT = [sb("KT%d" % i, [128, 8, 128], BF16) for i in range(2)]
    Pb = [sb("Pb%d" % i, [128, 8, 32], BF16) for i in range(2)]
    fq = sb("fq_s", [128, 8, 4])
    sown = sb("sown_s", [4, 8, 4])
    eqc = sb("eqc_s", [4, 4, 128])
    ptb_i = sb("ptb_i", [128, c.NPG], I32)
    ptb_f = sb("ptb_f", [128, c.NPG])
    ioff = sb("ioff_s", [128, 8])
    idx_i = sb("idx_i", [128, c.NPG], I32)
    sexp = [sb("sexp%d" % i, [128, 4], BF16) for i in range(2)]
    oat_s = sb("oat_s", [128, 8, 4])
    ue_s = sb("ue_s", [128, 8, 34])
    y_sm = sb("y_sm", [128, 8, 4])

    mrow = t32[0][0:33, :]
    brow = t32[1][0:33, :]
    grow = t32[2][0:33, :]
    idx_f = f32C[:, :, :].rearrange("p a b -> p (a b)")[:, 0:c.NPG]
    cst = scr16[0:30, 0:1024]
    _fa = f32A[:, :, :].rearrange("p a b -> p (a b)")
    cvs = _fa[:, 0:528].rearrange("p (k c) -> p k c", c=33)
    cvb = _fa[:, 1024:1288].bitcast(BF16).rearrange("p (k c) -> p k c", c=33)
    ksum = x1[:, 1, 0:8 * c.NPG].rearrange("p (h j) -> p h j", h=8)
    tctr = [0]

    def tmp32():
        t = t32[tctr[0] % 4]
        tctr[0] += 1
        return t
    pctr = [0]

    def ptile():
        t = ptl[pctr[0] % 3]
        pctr[0] += 1
        return t

    P.dma(ident_f[:, :], ident_f_h.ap())
    P.dma(ident_b[:, :], ident_b_h.ap())
    P.memset(ones_f[:, :], 1.0)
    P.memset(ones_b[:, :], 1.0)
    P.memset(epsc[:, :], EPS)
    P.memset(rall[:, :, :], 0.0)
    P.memset(kmsum[:, :, :], 0.0)
    P.dma(pvec[:, :], pvec_h.ap())
    P.dma(lpast[:, :], lpast_h.ap())
    P.dma(caus[:, :, :], caus_h.ap())
    P.dma(hflag[:, :], hflag_h.ap())
    P.dma(cvs[:, :, :], cvec_h.ap())
    P.dma(sbias[:, :, :], sbias_h.ap())
    P.dma(bmask[:, :], bmask_h.ap())
    P.dma(fq[:, :, :], fq_h.ap())
    P.dma(sown[:, :, :], sown_h.ap())
    P.dma(eqc[:, :, :], eq_h.ap())
    P.dma(ioff[:, :], ioff_h.ap())
    P.dma(ptb_i[:, :], ptab_h.ap().partition_broadcast(128))

    wctr = [0]

    def stream(src, kch, cols):
        slot = wsl[wctr[0] % 3]
        wctr[0] += 1
        v = src.rearrange("(kc p) c -> p kc c", p=128)
        for k0 in range(0, kch, 4):
            k1 = min(kch, k0 + 4)
            P.dma(slot[:, k0:k1, 0:cols], v[:, k0:k1, :], q="pool")
        return slot

    import os as _os
    KSUB = int(_os.environ.get("KSUB", "99"))
    if KSUB < 1:
        P.emit(es); es.close(); return nc
    P.act(cvb[:, :, :], cvs[:, :, :], AF.Silu)
    pmc = BK_X
    def mod_tile(t):
        wt = stream(w_ada_h[:, t * 512:(t + 1) * 512], 16, 512)
        pb = bank()
        for kc in range(16):
            P.mm(pb[0:33, :], cvb[:, kc, :], wt[:, kc, :], start=(kc == 0), stop=(kc == 15))
        P.dma(brow[:, :], b_ada_h[:, t * 512:(t + 1) * 512])
        P.tt(mrow[:, :], pb[0:33, :], brow[:, :], ALU.add)
        for ri, r in enumerate((0, 32)):
            for j in range(4):
                col = ri * 96 + t * 4 + j
                P.mm(pmc[:, col:col + 1], mrow[r:r + 1, j * 128:(j + 1) * 128], ones_f[r:r + 1, 0:1])
        which = {2: 0, 5: 1}.get(t // 4)
        if which is not None:
            cs = slice((t % 4) * 512, (t % 4 + 1) * 512)
            P.dma(grow[:, :], gpost_h[:, which, cs])
            P.tt(gtrow[:, which, cs], mrow[:, :], grow[:, :], ALU.mult)

    for t in range(8):
        mod_tile(t)
    for ri in range(2):
        o = ri * 96
        P.cp(modcol[:, o:o + 32], pmc[:, o:o + 32])
        P.stt(abv[:, ri, 0, :], modcol[:, o + 16:o + 32], 1.0, pvec[:, PV_GPRE_M:PV_GPRE_M + 16], ALU.add, ALU.mult)
    mod_rest = list(range(8, 24))

    def mod_finish():
        while mod_rest:
            mod_tile(mod_rest.pop(0))
        for ri in range(2):
            o = ri * 96
            P.cp(modcol[:, o + 32:o + 96], pmc[:, o + 32:o + 96])
            P.stt(abv[:, ri, 1, :], modcol[:, o + 64:o + 80], 1.0, pvec[:, PV_GPRE_F:PV_GPRE_F + 16], ALU.add, ALU.mult)

    def ab(ri, f, kc):
        o = ri * 96 + (48 if f else 0)
        return abv[:, ri, f, kc:kc + 1], modcol[:, o + kc:o + kc + 1]

    def rstd_from(out, in_, inv_n, M):
        P.act(out, in_, AF.Sqrt, bias=epsc[0:M, 0:1], scale=inv_n)
        P.add("dve", lambda e: e.reciprocal(out, out), reads=[out], writes=[out])

    def prologue(subs, f, colparams):
        for (si, M, coff) in subs:
            ssq = sml[0:M, 0:1]
            P.act(xn[0:M, 0, :], x1[0:M, si, :], AF.Square, accum=ssq)
            rs = sml[0:M, 1:2]
            rstd_from(rs, ssq, 1.0 / 2048, M)
            P.ts(xn[0:M, 0, :], x1[0:M, si, :], rs, None, op0=ALU.mult)
            for kc in range(16):
                pb = bank()
                pv = pb[:, :].bitcast(BF16)
                P.tr(pv[:, 0:M], xn[0:M, 0, kc * 128:(kc + 1) * 128], ident_b[0:M, 0:M])
                for (c0, c1, ri) in colparams:
                    a, b = ab(ri, f, kc)
                    if kc % 2 == 0:
                        P.act(hB[:, kc, coff + c0:coff + c1], pv[:, c0:c1], AF.Identity, bias=b, scale=a)
                    else:
                        P.ts(hB[:, kc, coff + c0:coff + c1], pv[:, c0:c1], a, b, op0=ALU.mult, op1=ALU.add)

    def linear_fm(wt, kch, rhs_kc, N, mch, cb):
        for m in range(mch):
            pb = bank()
            for kc in range(kch):
                P.mm(pb[:, 0:N], wt[:, kc, m * 128:(m + 1) * 128], rhs_kc(kc), start=(kc == 0), stop=(kc == kch - 1))
            cb(m, pb[:, 0:N])

    def fm_sumsq(chunks, N, pacc):
        n = len(chunks)
        for i, ch in enumerate(chunks):
            t = tmp32()
            P.act(t[:, 0:N], ch, AF.Square)
            P.mm(pacc[:, 0:N], ones_f[:, :], t[:, 0:N], start=(i == 0), stop=(i == n - 1))

    def fm_rmsnorm_to(chunks, N, gcol0, dst_of):
        fm_sumsq(chunks, N, BK_X)
        rt = tmp32()
        rstd_from(rt[:, 0:N], BK_X[:, 0:N], 1.0 / (128 * len(chunks)), 128)
        for i, ch in enumerate(chunks):
            P.stt(dst_of(i), ch, pvec[:, gcol0 + i:gcol0 + i + 1], rt[:, 0:N], ALU.mult, ALU.mult)

    def conv_post(ych, N):
        for i in range(8):
            P.mm(BK_X[:, 0:N], ones_f[:, :], ych(i), start=(i == 0), stop=(i == 7))
        fm_sumsq([ych(i) for i in range(8)], N, BK_Y)
        mean = lnm
        P.ts(mean[:, 0:N], BK_X[:, 0:N], 1.0 / 1024, None, op0=ALU.mult)
        msq = tmp32()
        P.tt(msq[:, 0:N], mean[:, 0:N], mean[:, 0:N], ALU.mult)
        var = lnv
        P.stt(var[:, 0:N], BK_Y[:, 0:N], 1.0 / 1024, msq[:, 0:N], ALU.mult, ALU.subtract)
        rstd_from(var[:, 0:N], var[:, 0:N], 1.0, 128)
        for i in range(8):
            y = ych(i)
            P.tt(y, y, mean[:, 0:N], ALU.subtract)
            P.tt(y, y, var[:, 0:N], ALU.mult)
            P.act(y, y, AF.Identity, bias=pvec[:, PV_BLN + i:PV_BLN + i + 1], scale=pvec[:, PV_GLN + i:PV_GLN + i + 1])
            sg = tmp32()
            P.act(sg[:, 0:N], y, AF.Sigmoid)
            P.tt(y, y, sg[:, 0:N], ALU.mult)

    def proj_post(w_h, kch_total, kgrp, lhs_kc, subs, ri, which, dst_cb):
        stages = [f32B[:, :, :].rearrange("p a b -> p (a b)"), f32C[:, :, :].rearrange("p a b -> p (a b)")]
        ngr = (kch_total + kgrp - 1) // kgrp
        r = 32 * ri
        for ct in range(4):
            pbs = [bank() for _ in subs]
            for g in range(ngr):
                k0 = g * kgrp
                kn = min(kgrp, kch_total - k0)
                wt = stream(w_h[k0 * 128:(k0 + kn) * 128, ct * 512:(ct + 1) * 512], kn, 512)
                for (si, M), pb in zip(subs, pbs):
                    for kk in range(kn):
                        kc = k0 + kk
                        P.mm(pb[0:M, :], lhs_kc(kc, si, M), wt[:, kk, :], start=(kc == 0), stop=(kc == kch_total - 1))
            for (si, M), pb in zip(subs, pbs):
                st = stages[si]
                P.cp(st[0:M, ct * 512:(ct + 1) * 512], pb[0:M, :])
                j = tmp32()
                P.act(j[0:M, :], pb[0:M, :], AF.Square, accum=sml[0:M, 8 + si * 4 + ct:9 + si * 4 + ct])
        for (si, M) in subs:
            st = stages[si]
            ss = sml[0:M, 16 + si:17 + si]
            P.red(ss, sml[0:M, 8 + si * 4:12 + si * 4])
            rstd_from(ss, ss, 1.0 / 2048, M)
            for ct in range(4):
                cs = slice(ct * 512, (ct + 1) * 512)
                pg = bank()
                P.mm(pg[0:M, :], ones_f[r:r + 1, 0:M], gtrow[r:r + 1, which, cs])
                tw = tmp32()
                P.stt(tw[0:M, :], st[0:M, cs], ss, pg[0:M, :], ALU.mult, ALU.mult)
                P.tt(st[0:M, cs], x1[0:M, si, cs], tw[0:M, :], ALU.add)
            dst_cb(si, M, st)

    def ffn(subs, colparams, N, ri, out_cb):
        prologue([(si, M, si * 128) for (si, M) in subs], 1, colparams)
        for g in range(11):
            wg = stream(w_gu_h[:, g * 512:(g + 1) * 512], 16, 512)
            wu = stream(w_gu_h[:, 5632 + g * 512:5632 + (g + 1) * 512], 16, 512)
            for m in range(4):
                pg = bank()
                for kc in range(16):
                    P.mm(pg[:, 0:N], wg[:, kc, m * 128:(m + 1) * 128], hB[:, kc, 0:N], start=(kc == 0), stop=(kc == 15))
                P.act(f32A[:, m, 0:N], pg[:, 0:N], AF.Silu)
            for m in range(4):
                pu = bank()
                for kc in range(16):
                    P.mm(pu[:, 0:N], wu[:, kc, m * 128:(m + 1) * 128], hB[:, kc, 0:N], start=(kc == 0), stop=(kc == 15))
                P.tt(bigA[:, g * 4 + m, 0:N], f32A[:, m, 0:N], pu[:, 0:N], ALU.mult)
        proj_post(w_dn_h, 44, 11, lambda kc, si, M: bigA[:, kc, si * 128:si * 128 + M], subs, ri, 1, out_cb)

    def mix_out(subs, N, ri):
        def dst(si, M, st):
            P.cp(x1[0:M, si, :], st[0:M, :])
        proj_post(w_out_h, 16, 16, lambda kc, si, M: bigA[:, 16 + kc, si * 128:si * 128 + M], subs, ri, 0, dst)

    def transpose_out(src_cc, ncols, dst_h):
        for cc in range(8):
            pb = bank()
            P.tr(pb[0:ncols, 0:128], src_cc(cc), ident_f[:, :])
            P.cp(cst[0:ncols, cc * 128:(cc + 1) * 128], pb[0:ncols, 0:128])
        P.dma(dst_h.ap(), cst[0:ncols, :])

    import os as _os
    STAGE = int(_os.environ.get("KSTAGE", "99"))
    kst = bigA[:, 0:8, :]
    vst = bigA[:, 8:16, :].rearrange("p (s a) b -> p s (a b)", s=2)
    KP1 = int(_os.environ.get("KP1", "99"))
    KP1SUB = int(_os.environ.get("KP1SUB", "99"))
    for ti in range(min(KP1, c.NT1 - 1) if STAGE >= 1 else 0):
        for si in range(2):
            P.dma(x1[:, si, :], xb_h[ti * 256 + si * 128:ti * 256 + (si + 1) * 128, :])
        prologue([(0, 128, 0), (1, 128, 128)], 0, [(0, 128, 0)])
        if KP1SUB < 1:
            continue
        for half in range(2):
            wt = stream(w_in_h[:, 1024 + half * 512:1024 + (half + 1) * 512], 16, 512)

            KD = int(_os.environ.get("KDBG", "7"))

            def cbk(m, pb, half=half):
                hh = half * 4 + m
                if KD & 2:
                    P.cp(kst[:, hh, :], pb, eng="act")
                if KD & 4:
                    P.red(kmsum[:, hh, ti:ti + 1], pb)
            if KD & 1:
                linear_fm(wt, 16, lambda kc: hB[:, kc, :], 256, 4, cbk)
        if KP1SUB < 2:
            continue
        P.dma(kf_h.ap()[:, :, ti * 256:(ti + 1) * 256].rearrange("h p t -> p h t"), kst, wk=[("kf", ti)])
        for half in range(2):
            wt = stream(w_in_h[:, 2048 + half * 512:2048 + (half + 1) * 512], 16, 512)
            for si in range(2):
                pb = bank()
                for kc in range(16):
                    P.mm(pb[:, :], hB[:, kc, si * 128:(si + 1) * 128], wt[:, kc, :], start=(kc == 0), stop=(kc == 15))
                P.cp(vst[:, si, half * 512:(half + 1) * 512], pb[:, :], eng=("act" if si else "dve"))
        P.dma(vt_h.ap()[ti * 256:(ti + 1) * 256, :].rearrange("(s p) c -> p s c", p=128), vst, wk=[("vt", ti)])
        if mod_rest:
            mod_tile(mod_rest.pop(0))
    mod_finish()
    kmT = sb("kmT", [128, 8, 16])
    P.ts(kmT[:, :, :], kmsum[:, :, :], 1.0 / 256, None, op0=ALU.mult)
    kf_keys = [("kf", i) for i in range(c.NT1 - 1)]
    vt_keys = [("vt", i) for i in range(c.NT1 - 1)]

    qf = bigA[:, 0:8, :]
    kfm = bigA[:, 8:16, :]
    cat = bigA[:, 16:32, :]
    vown = bigA[:, 32:40, :].rearrange("p (s a) b -> p s (a b)", s=2)
    kvst = [f32B[:, :, :].rearrange("p a b -> p (a b)"), f32C[:, :, :].rearrange("p a b -> p (a b)")]

    def inproj(N, subs, mini, tile_i):
        rhs = lambda kc: hB[:, kc, 0:N]
        for ct in range(4):
            wt = stream(w_in_h[:, 1024 + ct * 512:1024 + (ct + 1) * 512], 16, 512)
            for (si, M) in subs:
                pb = bank()
                for kc in range(16):
                    P.mm(pb[0:M, :], hB[:, kc, si * 128:si * 128 + M], wt[:, kc, :], start=(kc == 0), stop=(kc == 15))
                P.cp(kvst[si][0:M, ct * 512:(ct + 1) * 512], pb[0:M, :], eng=("act" if ct % 2 else "dve"))
        for (si, M) in subs:
            if mini:
                P.dma(k_s_h.ap().rearrange("h t d -> t h d"), kvst[0][0:4, 0:1024].rearrange("t (h d) -> t h d", h=8))
                P.dma(v_s_h.ap().rearrange("h t d -> t h d"), kvst[0][0:4, 1024:2048].rearrange("t (h d) -> t h d", h=8))
                P.cp(vn_b[0:4, :], kvst[0][0:4, 1024:2048])
            else:
                pg = tile_i * 2 + si
                P.dma(k_own_h.ap()[pg].rearrange("h t d -> t h d"), kvst[si][:, 0:1024].rearrange("t (h d) -> t h d", h=8))
                P.dma(v_own_h.ap()[pg].rearrange("h t d -> t h d"), kvst[si][:, 1024:2048].rearrange("t (h d) -> t h d", h=8))
                P.cp(vown[:, si, :], kvst[si][:, 1024:2048])
        for half in range(2):
            wt = stream(w_in_h[:, half * 512:(half + 1) * 512], 16, 512)

            def cbq(m, pb, half=half):
                hh = half * 4 + m
                P.act(qf[:, hh, 0:N], pb, AF.Copy, scale=SCALE)
                if mini:
                    P.ts(qs_f[:, hh, :], pb[:, 0:4], SCALE, None, op0=ALU.mult)
                    P.cp(qs_b[:, hh, :], qf[:, hh, 0:4])
                else:
                    q32 = tmp32()
                    P.ts(q32[:, 0:N], pb, SCALE, None, op0=ALU.mult)
                    select_main(hh, q32)
            linear_fm(wt, 16, rhs, N, 4, cbq)
        for half in range(2):
            wt = stream(w_in_h[:, 1024 + half * 512:1024 + (half + 1) * 512], 16, 512)

            def cbk(m, pb, half=half):
                hh = half * 4 + m
                P.cp(kfm[:, hh, 0:N], pb, eng="act")
                if mini:
                    P.cp(kn_b[:, hh, :], kfm[:, hh, 0:4])
            linear_fm(wt, 16, rhs, N, 4, cbk)
        for half in range(2):
            wt = stream(w_in_h[:, 3072 + half * 512:3072 + (half + 1) * 512], 16, 512)

            def cba(m, pb, half=half):
                P.cp(f32B[:, half * 4 + m, 0:N], pb, eng="act")
            linear_fm(wt, 16, rhs, N, 4, cba)
        for half in range(2):
            wt = stream(w_in_h[:, 4096 + half * 512:4096 + (half + 1) * 512], 16, 512)

            def cbg(m, pb, half=half):
                cc = half * 4 + m
                sg = tmp32()
                P.act(sg[:, 0:N], pb, AF.Sigmoid)
                if mini:
                    P.tt(usamp[:, cc, 0:N], f32B[:, cc, 0:N], sg[:, 0:N], ALU.mult)
                else:
                    P.tt(f32A[:, cc, 32:32 + N], f32B[:, cc, 0:N], sg[:, 0:N], ALU.mult)
            linear_fm(wt, 16, rhs, N, 4, cbg)

    def select_main(hh, q32):
        for si in range(2):
            pb = bank()
            P.mm(pb[:, 0:16], q32[:, si * 128:(si + 1) * 128], kmT[:, hh, :])
            s0 = sel[:, 0, :]
            P.tt(s0, pb[:, 0:16], vb_t[:, si, :], ALU.add)
            P.add("dve", lambda e: e.max(sel[:, 1, 0:8], sel[:, 0, :]), reads=[s0], writes=[sel[:, 1, 0:8]])
            P.ts(sel[:, 2, :], s0, sel[:, 1, 2:3], None, op0=ALU.is_ge)
            P.tt(sel[:, 2, :], sel[:, 2, :], v01_t[:, si, :], ALU.mult)
            P.ts(selb[:, :], sel[:, 2, :], -1.0, -NEGB, op0=ALU.add, op1=ALU.mult)
            pt = bank()
            pv = pt[:, :].bitcast(BF16)
            P.tr(pv[0:16, 0:128], selb[:, :], ident_b[:, :])
            P.cp(rall[0:16, hh, si * 128:(si + 1) * 128], pv[0:16, 0:128], eng="act")

    kA = scr16[:, 0:2048].bitcast(BF16)
    vA = scr16[:, 2048:4096].bitcast(BF16).rearrange("p (k d) -> p k d", d=128)

    def attention_main(per_head=None, npast=None):
        NP = NKT if npast is None else min(NKT, npast)
        for hh in range(8):
            P.dma(kA[:, 0:NP * 128], kf_h.ap()[hh][:, 0:NP * 128], rk=kf_keys)
            P.dma(vA[:, 0:NP, :], vt_h.ap()[0:NP * 128, hh * 128:(hh + 1) * 128].rearrange("(k p) d -> p k d", p=128), rk=vt_keys)
            tot = NP + 2

            def emit_s(i):
                pS = bank()
                if i < NP:
                    P.mm(pS[:, 0:256], kA[:, i * 128:(i + 1) * 128], qf[:, hh, :], start=True, stop=False)
                    P.mm(pS[:, 0:256], lpast[0:36, i * 128:(i + 1) * 128], rall[0:36, hh, :], start=False, stop=True)
                else:
                    jt = i - NP
                    P.mm(pS[:, 0:256], kfm[:, hh, jt * 128:(jt + 1) * 128], qf[:, hh, :], start=True, stop=False)
                    P.mm(pS[:, 0:256], lpast[32:35, jt * 128:(jt + 1) * 128], rall[32:35, hh, :], start=False, stop=False)
                    P.mm(pS[:, 0:256], ident_b[:, :], caus[:, jt, :], start=False, stop=True)
                return pS
            LOOK = 2
            sb_ = {}
            for i in range(min(LOOK, tot)):
                sb_[i] = emit_s(i)
            for i in range(tot):
                pS = sb_.pop(i)
                if i < NP:
                    vt_ = vA[:, i, :]
                else:
                    jt = i - NP
                    vt_ = vown[:, jt, hh * 128:(hh + 1) * 128]
                pt = ptile()
                P.act(pt[:, :], pS[:, 0:256], AF.Exp)
                P.mm(BK_O[:, 0:256], vt_, pt[:, :], start=(i == 0), stop=(i == tot - 1))
                P.mm(BK_S[:, 0:256], ones_b[:, :], pt[:, :], start=(i == 0), stop=(i == tot - 1))
                if i + LOOK < tot:
                    sb_[i + LOOK] = emit_s(i + LOOK)
            if per_head is not None:
                per_head(hh)
            rs = tmp32()
            P.add("dve", lambda e, rs=rs: e.reciprocal(rs[:, 0:256], BK_S[:, 0:256]), reads=[BK_S[:, 0:256]], writes=[rs[:, 0:256]])
            P.tt(f32C[:, hh, :], BK_O[:, 0:256], rs[:, 0:256], ALU.mult)

    O_s = scr16[:, 0:2048].rearrange("p (h n q) -> p h n q", h=8, q=4)
    L_s = scr16[:, 2048:4096].rearrange("p (h n q) -> p h n q", h=8, q=4)
    kpg = [f32B[:, :, :].rearrange("p a b -> p (a b)")[:, 0:1024].rearrange("p (h d) -> p h d", h=8),
           f32B[:, :, :].rearrange("p a b -> p (a b)")[:, 1024:2048].rearrange("p (h d) -> p h d", h=8)]
    vpg = [f32C[:, :, :].rearrange("p a b -> p (a b)")[:, 0:1024].rearrange("p (h d) -> p h d", h=8),
           f32C[:, :, :].rearrange("p a b -> p (a b)")[:, 1024:2048].rearrange("p (h d) -> p h d", h=8)]
    vpb = f32A[:, :, :].rearrange("p a b -> p (a b)")[:, 0:1024].bitcast(BF16).rearrange("p (e h d) -> p e h d", e=2, h=8)

    def attention_sample():
        P.cp(ptb_f[:, :], ptb_i[:, :])
        P.ts(idx_f, ptb_f[:, :], 128.0, ioff[:, 0:1], op0=ALU.mult, op1=ALU.add)
        P.cp(idx_i[:, :], idx_f)
        qall = qs_b[:, :, :].rearrange("p h q -> p (h q)")
        kflat = f32B[:, :, :].rearrange("p a b -> p (a b)")
        vflat = f32C[:, :, :].rearrange("p a b -> p (a b)")
        vpb4 = f32A[:, :, :].rearrange("p a b -> p (a b)")[:, 0:1024].bitcast(BF16).rearrange("p (e r d) -> p e r d", e=2, r=8)
        for n in range(c.NSB):
            for e_ in range(2):
                j = 2 * n + e_
                ix = idx_i[:, j:j + 1]
                ko = kflat[:, e_ * 1024:(e_ + 1) * 1024]
                vo = vflat[:, e_ * 1024:(e_ + 1) * 1024]
                P.add("pool", lambda e, o=ko, ix=ix: e.indirect_dma_start(
                    out=o, out_offset=None, in_=ck_h.ap(), in_offset=bass.IndirectOffsetOnAxis(ap=ix, axis=0)),
                    reads=[ix], writes=[ko], dma=True)
                P.add("pool", lambda e, o=vo, ix=ix: e.indirect_dma_start(
                    out=o, out_offset=None, in_=cv_h.ap(), in_offset=bass.IndirectOffsetOnAxis(ap=ix, axis=0)),
                    reads=[ix], writes=[vo], dma=True)
                P.cp(vpb4[:, e_, :, :], vo.rearrange("p (r d) -> p r d", r=8))
            for e_ in range(2):
                j = 2 * n + e_
                ko = kflat[:, e_ * 1024:(e_ + 1) * 1024]
                kt = KT[e_]
                for half in range(2):
                    pT = bank()
                    for rr in range(4):
                        r = half * 4 + rr
                        P.tr(pT[:, rr * 128:(rr + 1) * 128], ko[:, r * 128:(r + 1) * 128], ident_f[:, :])
                    P.cp(kt[:, half * 4:(half + 1) * 4, :], pT[:, :].rearrange("p (r k) -> p r k", r=4), eng="act")
                P.red(ksum[:, :, j], kt[:, :, :].rearrange("p r (h g) -> p h r g", h=8), axis=AX.XY)
                pS = bank()
                for r in range(8):
                    P.mm(pS[:, r * 32:(r + 1) * 32], kt[:, r, :], qall)
                t1 = tmp32()
                t3 = t1[:, 0:256].rearrange("p (r k) -> p r k", r=8)
                P.tt(t3, pS[:, 0:256].rearrange("p (r k) -> p r k", r=8),
                     sbias[:, j, :].unsqueeze(2).to_broadcast([128, 8, 32]), ALU.add)
                P.act(t1[:, 0:256], t1[:, 0:256], AF.Exp)
                P.tt(Pb[e_][:, :, :], t3, bmask[:, :].unsqueeze(1).to_broadcast([128, 8, 32]), ALU.mult)
            cnt = 0
            for e_ in range(2):
                for r in range(8):
                    P.mm(BK_O[:, 0:32], vpb4[:, e_, r, :], Pb[e_][:, r, :], start=(cnt == 0), stop=(cnt == 15))
                    cnt += 1
            cnt = 0
            for e_ in range(2):
                for r in range(8):
                    P.mm(BK_S[:, 0:32], ones_b[:, :], Pb[e_][:, r, :], start=(cnt == 0), stop=(cnt == 15))
                    cnt += 1
            P.cp(O_s[:, :, n, :], BK_O[:, 0:32].rearrange("p (h q) -> p h q", q=4))
            P.cp(L_s[:, :, n, :], BK_S[:, 0:32].rearrange("p (h q) -> p h q", q=4), eng="act")
        NSB = c.NSB
        P.red(kms[:, :, :], ksum[:, :, :].rearrange("p h (n e) -> p h n e", e=2))
        P.ts(kms[:, :, :], kms[:, :, :], 1.0 / 256, None, op0=ALU.mult)
        ncol = max(NSB, 8)
        for hh in range(8):
            pb = bank()
            P.mm(pb[0:4, 0:NSB], qs_f[:, hh, :], kms[:, hh, :])
            P.memset(sel[0:4, :, :], -1e9)
            sv = sel[0:4, :, :].rearrange("p a b -> p (a b)")
            P.cp(sv[:, 0:NSB], pb[0:4, 0:NSB])
            P.add("dve", lambda e, sv=sv: e.max(sml[0:4, 32:40], sv[:, 0:64]), reads=[sv[:, 0:64]], writes=[sml[0:4, 32:40]])
            m01 = tmp32()
            P.ts(m01[0:4, 0:NSB], sv[:, 0:NSB], sml[0:4, 34:35], None, op0=ALU.is_ge)
            pB = bank()
            for q in range(4):
                P.mm(pB[:, q * NSB:(q + 1) * NSB], eqc[0:4, q, :], m01[0:4, 0:NSB])
            pBv = pB[:, 0:4 * NSB].rearrange("p (q n) -> p q n", q=4)
            tm = tmp32()
            tmv = tm[:, 0:4 * NSB].rearrange("p (q n) -> p q n", q=4)
            P.tt(tmv, O_s[:, hh, 0:NSB, :].rearrange("p n q -> p q n"), pBv, ALU.mult)
            P.red(sml[:, 40:44], tmv)
            tm2 = tmp32()
            tmv2 = tm2[:, 0:4 * NSB].rearrange("p (q n) -> p q n", q=4)
            P.tt(tmv2, L_s[:, hh, 0:NSB, :].rearrange("p n q -> p q n"), pBv, ALU.mult)
            P.red(sml[:, 44:48], tmv2)
            pS = bank()
            P.mm(pS[0:4, 0:4], kn_b[:, hh, :], qs_b[:, hh, :])
            so = tmp32()
            P.tt(so[0:4, 0:4], pS[0:4, 0:4], sown[:, hh, :], ALU.add)
            P.act(sexp[0][0:4, 0:4], so[0:4, 0:4], AF.Exp)
            P.mm(BK_O[:, 0:4], vn_b[0:4, hh * 128:(hh + 1) * 128], sexp[0][0:4, 0:4])
            P.mm(BK_S[:, 0:4], ones_b[0:4, :], sexp[0][0:4, 0:4])
            P.tt(sml[:, 40:44], sml[:, 40:44], fq[:, hh, :], ALU.mult)
            P.tt(sml[:, 44:48], sml[:, 44:48], fq[:, hh, :], ALU.mult)
            P.tt(sml[:, 40:44], sml[:, 40:44], BK_O[:, 0:4], ALU.add)
            P.tt(sml[:, 44:48], sml[:, 44:48], BK_S[:, 0:4], ALU.add)
            P.add("dve", lambda e: e.reciprocal(sml[:, 44:48], sml[:, 44:48]), reads=[sml[:, 44:48]], writes=[sml[:, 44:48]])
            P.tt(oat_s[:, hh, :], sml[:, 40:44], sml[:, 44:48], ALU.mult)

    def finish():
        P.emit(es)
        es.close()
        return nc
    if STAGE < 2:
        return finish()
    P.dma(x1[0:c.MW, 0, :], xmini_h.ap())
    prologue([(0, c.MW, 0)], 0, [(0, 4, 1), (4, c.MW, 0)])
    inproj(c.MW, [(0, 4)], True, 0)
    for cc in range(8):
        P.ts(uhalo[:, cc, :], usamp[:, cc, 4:36], hflag[:, 0:1], None, op0=ALU.mult)
    if STAGE < 3:
        return finish()
    P.dma(cst[0:30, :], sconv_h.ap())
    for cc in range(8):
        pb = bank()
        P.tr(pb[:, 0:30], cst[0:30, cc * 128:(cc + 1) * 128], ident_f[0:30, 0:30])
        P.cp(ue_s[:, cc, 0:30], pb[:, 0:30])
        P.cp(ue_s[:, cc, 30:34], usamp[:, cc, 0:4])
    for cc in range(8):
        for t_ in range(4):
            tw = tmp32()
            P.tt(tw[:, 0:31], pvec[:, PV_WDW + cc * 31:PV_WDW + (cc + 1) * 31], ue_s[:, cc, t_:t_ + 31], ALU.mult)
            P.red(y_sm[:, cc, t_:t_ + 1], tw[:, 0:31])
        P.ts(y_sm[:, cc, :], y_sm[:, cc, :], pvec[:, PV_BDW + cc:PV_BDW + cc + 1], None, op0=ALU.add)
    transpose_out(lambda cc: ue_s[:, cc, 4:34], 30, conv_s_h)
    if STAGE < 4:
        return finish()
    attention_sample()
    if STAGE < 5:
        return finish()
    fm_rmsnorm_to([oat_s[:, hh, :] for hh in range(8)], 4, PV_GATTN, lambda i: cat[:, i, 0:4])
    conv_post(lambda cc: y_sm[:, cc, :], 4)
    fm_rmsnorm_to([y_sm[:, cc, :] for cc in range(8)], 4, PV_GCONV, lambda i: cat[:, 8 + i, 0:4])
    mix_out([(0, 4)], 4, 1)

    def out_s(si, M, st):
        P.dma(y_s_h.ap(), st[0:4, :])
    ffn([(0, 4)], [(0, 4, 1)], 4, 1, out_s)

    if STAGE < 6:
        return finish()
    for it in range(c.NOWN):
        for si in range(2):
            r0 = it * 256 + si * 128
            P.dma(x1[:, si, :], xown_h[r0:r0 + 128, :])
            P.dma(vb_t[:, si, :], vbias_h[r0:r0 + 128, :])
            P.dma(v01_t[:, si, :], v01_h[r0:r0 + 128, :])
        P.dma(rall[32:36, :, :], rc_h[it])
        subs = [(0, 128), (1, 128)]
        prologue([(0, 128, 0), (1, 128, 128)], 0, [(0, 128, 0)])
        inproj(256, subs, False, it)
        for cc in range(8):
            P.cp(f32A[:, cc, 0:32], uhalo[:, cc, :], eng="act")

        def conv_chunk(cc):
            y = f32B[:, cc, :]
            w0 = PV_WDW + cc * 31
            P.ts(y, f32A[:, cc, 2:258], pvec[:, w0:w0 + 1], pvec[:, PV_BDW + cc:PV_BDW + cc + 1], op0=ALU.mult, op1=ALU.add)
            for j in range(1, 31):
                P.stt(y, f32A[:, cc, 2 + j:258 + j], pvec[:, w0 + j:w0 + j + 1], y, ALU.mult, ALU.add)
        attention_main(conv_chunk, npast=2 * ((S - c.OWN) // 256 + it))
        if it == c.NOWN - 1:
            transpose_out(lambda cc: f32A[:, cc, 258:288], 30, convp_h)
        for cc in range(8):
            P.cp(uhalo[:, cc, :], f32A[:, cc, 256:288], eng="act")
        fm_rmsnorm_to([f32C[:, hh, :] for hh in range(8)], 256, PV_GATTN, lambda i: cat[:, i, :])
        conv_post(lambda cc: f32B[:, cc, :], 256)
        fm_rmsnorm_to([f32B[:, cc, :] for cc in range(8)], 256, PV_GCONV, lambda i: cat[:, 8 + i, :])
        mix_out(subs, 256, 0)

        def out_m(si, M, st, it=it):
            r0 = it * 256 + si * 128
            P.dma(y_own_h.ap()[r0:r0 + 128, :], st[:, :])
        ffn(subs, [(0, 128, 0)], 256, 0, out_m)

    P.emit(es)
    es.close()
    return nc


def _bf(a):
    return np.asarray(a, dtype=np.float32).astype(ml_dtypes.bfloat16)


def host_consts(cfg, qstart):
    c = cfg
    m = (2.0 ** (-8.0 * (np.arange(8) + 1.0) / 8)).astype(np.float64)
    NKT = c.NKT
    lp = np.zeros((36, NKT * 128), np.float32)
    for kt in range(NKT):
        sl = slice(kt * 128, (kt + 1) * 128)
        if kt // 2 < 16:
            lp[kt // 2, sl] = 1.0
        lp[32, sl] = 1.0
        lp[33, sl] = np.arange(128)
        lp[34, sl] = 128.0 * kt
        lp[35, sl] = 1.0
    j = np.arange(256)
    caus = np.zeros((128, 2, 256), np.float32)
    for jt in range(2):
        kidx = 128 * jt + np.arange(128)
        caus[:, jt, :] = np.where(kidx[:, None] <= j[None, :], 0.0, NEGB)
    rc = np.zeros((c.NOWN, 4, 8, 256), np.float32)
    for it in range(c.NOWN):
        q0 = qstart + it * 256
        for h in range(8):
            rc[it, 0, h] = -m[h] * j
            rc[it, 1, h] = m[h]
            rc[it, 2, h] = m[h]
            rc[it, 3, h] = -m[h] * q0
    tq = qstart + np.arange(c.OWN)
    nfull = tq // 256
    v01 = (np.arange(16)[None, :] < nfull[:, None]).astype(np.float32)
    vbias = np.where(v01 > 0, 0.0, -1e9).astype(np.float32)
    past = c.NPG * 128
    sbias = np.zeros((128, c.NPG, 8), np.float32)
    bmask = np.zeros((128, 32), np.float32)
    for h in range(8):
        bmask[h * 16:(h + 1) * 16, h * 4:(h + 1) * 4] = 1.0
        for g in range(16):
            for jp in range(c.NPG):
                sbias[h * 16 + g, jp, :] = m[h] * (128 * jp + 8 * g + np.arange(8) - past)
    fq = np.zeros((128, 8, 4), np.float32)
    sown = np.zeros((4, 8, 4), np.float32)
    for h in range(8):
        for q in range(4):
            fq[:, h, q] = np.exp(-m[h] * q)
            for k in range(4):
                sown[k, h, q] = -m[h] * (q - k) if k <= q else NEGB
    eqc = np.zeros((4, 4, 128), np.float32)
    for q in range(4):
        eqc[q, q, :] = 1.0
    ioff = (np.arange(128)[:, None] + 128.0 * np.arange(8)[None, :]).astype(np.float32)
    return dict(lpast=_bf(lp), caus=_bf(caus), rc=_bf(rc), vbias=vbias, v01=v01, sbias=sbias, bmask=bmask, fq=fq,
                sown=sown, eqc=eqc, ioff=ioff, ident_f=np.eye(128, dtype=np.float32), ident_b=_bf(np.eye(128)))


def core_inputs(cfg, inp, b, qstart, sb_):
    c = cfg
    f = np.float32
    d = host_consts(c, qstart)
    col = lambda v: np.ascontiguousarray(np.asarray(v, f).reshape(-1, 128).T)
    pv = np.zeros((128, NPV), f)
    pv[:, PV_GPRE_M:PV_GPRE_M + 16] = col(inp["g_pre_mix"][0])
    pv[:, PV_GPRE_F:PV_GPRE_F + 16] = col(inp["g_pre_ffn"][0])
    pv[:, PV_GATTN:PV_GATTN + 8] = col(inp["g_attn_out"][0])
    pv[:, PV_GCONV:PV_GCONV + 8] = col(inp["g_conv_out"][0])
    pv[:, PV_GLN:PV_GLN + 8] = col(inp["g_conv_ln"][0])
    pv[:, PV_BLN:PV_BLN + 8] = col(inp["b_conv_ln"][0])
    pv[:, PV_BDW:PV_BDW + 8] = col(inp["b_dw"][0])
    wdw = np.asarray(inp["w_dw"], f)[0, :, 0, :]
    pv[:, PV_WDW:] = wdw.reshape(31, 8, 128).transpose(2, 1, 0).reshape(128, 8 * 31)
    d["pvec"] = pv
    cv = np.zeros((128, 16, 33), f)
    cv[:, :, 0] = col(inp["c_prompt"][b])
    cv[:, :, 32] = col(inp["c_sample"][sb_])
    d["cvec"] = cv
    d["w_ada"] = np.asarray(inp["w_ada"], f)[0]
    ba = np.zeros((33, 12288), f)
    ba[0] = inp["b_ada"][0]
    ba[32] = inp["b_ada"][0]
    d["b_ada33"] = ba
    gp = np.zeros((33, 2, 2048), f)
    for r in (0, 32):
        gp[r, 0] = inp["g_post_mix"][0]
        gp[r, 1] = inp["g_post_ffn"][0]
    d["gpost33"] = gp
    d["w_in"] = np.asarray(inp["w_in"], f)[0]
    d["w_out"] = np.asarray(inp["w_out"], f)[0]
    d["w_gu"] = np.asarray(inp["w_gate_up"], f)[0]
    d["w_down"] = np.asarray(inp["w_down"], f)[0]
    xp = np.asarray(inp["x_prompt"], f)
    d["xb"] = xp[b]
    d["xown"] = xp[b, qstart:qstart + c.OWN]
    xm = np.zeros((c.MW, 2048), f)
    xm[0:4] = np.asarray(inp["x_sample"], f)[sb_]
    if qstart > 0:
        xm[4:36] = xp[b, qstart - 32:qstart]
    d["xmini"] = xm
    d["haloflag"] = np.full((128, 1), 1.0 if qstart > 0 else 0.0, f)
    d["cache_k"] = np.asarray(inp["cache_k"], f)[0].reshape(-1, 1024)
    d["cache_v"] = np.asarray(inp["cache_v"], f)[0].reshape(-1, 1024)
    d["ptab"] = np.asarray(inp["page_table"], np.int32)[sb_:sb_ + 1]
    d["sconv"] = np.asarray(inp["state_conv"], f)[0, sb_]
    return d


_NC_CACHE = {}


def run_cores(cfg, inp, assign):
    key = (cfg.S, cfg.OWN, cfg.NPG, cfg.NPOOL)
    if key not in _NC_CACHE:
        _NC_CACHE[key] = build(cfg)
    nc = _NC_CACHE[key]
    maps = [core_inputs(cfg, inp, b, q, s) for (b, q, s) in assign]
    res = run_bass_kernel_spmd(nc, maps, core_ids=list(range(len(assign))))
    return res.results


def kernel(**inp):
    cfg = Cfg()
    assign = [(cid // 4, (cid % 4) * 1024, cid) for cid in range(8)]
    r = run_cores(cfg, inp, assign)
    f = np.float32
    y_p = np.zeros((2, 4096, 2048), f)
    k_p = np.zeros((1, 2, 32, 8, 128, 128), f)
    v_p = np.zeros((1, 2, 32, 8, 128, 128), f)
    cv_p = np.zeros((1, 2, 30, 1024), f)
    y_s = np.zeros((8, 4, 2048), f)
    k_s = np.zeros((1, 8, 8, 4, 128), f)
    v_s = np.zeros((1, 8, 8, 4, 128), f)
    cv_s = np.zeros((1, 8, 30, 1024), f)
    for cid, (b, q, s) in enumerate(assign):
        o = r[cid]
        y_p[b, q:q + 1024] = o["y_own"]
        k_p[0, b, q // 128:q // 128 + 8] = o["k_own"]
        v_p[0, b, q // 128:q // 128 + 8] = o["v_own"]
        if q == 3072:
            cv_p[0, b] = o["convp"]
        y_s[s] = o["y_s"]
        k_s[0, s] = o["k_s"]
        v_s[0, s] = o["v_s"]
        cv_s[0, s] = o["conv_s"]
    return (y_p, y_s, k_p, v_p, cv_p, k_s, v_s, cv_s)
```

```python
import numpy as np
import concourse.bass as bass
import concourse.mybir as mybir

F32 = mybir.dt.float32
BF16 = mybir.dt.bfloat16
I32 = mybir.dt.int32
AF = mybir.ActivationFunctionType
ALU = mybir.AluOpType
AX = mybir.AxisListType

GRAN = 64
NDSEM = 20


def _esize(dt):
    if dt == BF16:
        return 2
    return 4


class Prog:
    def __init__(self, nc):
        self.nc = nc
        self.ins = []
        self.lastw = {}
        self.readers = {}
        self.tbytes = {}
        self.psum = set()
        self.dma_n = {"sp": 0, "pool": 0}
        self.dma_last = {}

    def reg(self, t, nbytes, psum=False):
        self.tbytes[t.name] = nbytes
        if psum:
            self.psum.add(t.name)

    def keys(self, x):
        if isinstance(x, tuple):
            return [x]
        name = x.tensor.name
        pb = self.tbytes.get(name)
        if pb is None or name in self.psum:
            return [(name,)]
        es = _esize(x.dtype)
        off = (x.offset * es) % pb
        span = es
        for (st, cnt) in x.ap[1:]:
            span += abs(st) * (cnt - 1) * es
        g0 = off // GRAN
        g1 = (off + span - 1) // GRAN
        return [(name, g) for g in range(g0, g1 + 1)]

    def add(self, eng, fn, reads=(), writes=(), dma=False):
        idx = len(self.ins)
        deps = set()
        rk = [k for r in reads for k in self.keys(r)]
        wk = [k for w in writes for k in self.keys(w)]
        pk = [k for k in rk if k[0] in self.psum]
        if pk:
            rk = [k for k in rk if k[0] not in self.psum]
            wk = wk + pk
        for k in rk:
            d = self.lastw.get(k)
            if d is not None:
                deps.add(d)
        for k in wk:
            d = self.lastw.get(k)
            if d is not None:
                deps.add(d)
            for r in self.readers.get(k, ()):
                deps.add(r)
        for k in rk:
            self.readers.setdefault(k, []).append(idx)
        for k in wk:
            self.lastw[k] = idx
            self.readers[k] = []
        sem = None
        if dma:
            n = self.dma_n[eng]
            self.dma_n[eng] = n + 1
            sem = (eng, n % NDSEM)
            prev = self.dma_last.get(sem)
            if prev is not None:
                deps.add(prev)
            self.dma_last[sem] = idx
        deps.discard(idx)
        self.ins.append(dict(eng=eng, fn=fn, deps=deps, dma=dma, sig=bool(dma), sem=sem, val=0))
        return idx

    def emit(self, es):
        nc = self.nc
        ins = self.ins
        for i in ins:
            for d in i["deps"]:
                di = ins[d]
                if i["eng"] == "pe" and di["eng"] == "pe" and not di["dma"] and not i["dma"]:
                    continue
                di["sig"] = True
        cnt = {}
        for i in ins:
            if not i["sig"]:
                continue
            if i["dma"]:
                s = i["sem"]
                cnt[s] = cnt.get(s, 0) + 16
            else:
                s = (i["eng"], "c")
                i["sem"] = s
                cnt[s] = cnt.get(s, 0) + 1
            i["val"] = cnt[s]
        semobj = {}
        for s in cnt:
            semobj[s] = es.enter_context(nc.semaphore("s_%s_%s" % s))
        final = dict(cnt)

        def make(engname):
            def body(e):
                waited = {}
                for i in ins:
                    if i["eng"] != engname:
                        continue
                    need = {}
                    for d in sorted(i["deps"]):
                        di = ins[d]
                        if engname == "pe" and di["eng"] == "pe" and not di["dma"] and not i["dma"]:
                            continue
                        s = di["sem"]
                        v = di["val"]
                        if need.get(s, 0) < v:
                            need[s] = v
                    for s, v in need.items():
                        if waited.get(s, 0) < v:
                            e.wait_ge(semobj[s], v)
                            waited[s] = v
                    r = i["fn"](e)
                    if i["sig"]:
                        r.then_inc(semobj[i["sem"]], 16 if i["dma"] else 1)
                if engname == "sp":
                    for s, v in final.items():
                        if s[1] != "c" and waited.get(s, 0) < v:
                            e.wait_ge(semobj[s], v)
            return body

        block = es.enter_context(nc.Block())
        block.tensor(make("pe"))
        block.scalar(make("act"))
        block.vector(make("dve"))
        block.gpsimd(make("pool"))
        block.sync(make("sp"))

    def mm(self, out, lhsT, rhs, start=True, stop=True):
        self.add("pe", lambda e: e.matmul(out, lhsT, rhs, start=start, stop=stop),
                 reads=[lhsT, rhs], writes=[out])

    def tr(self, out, in_, ident):
        self.add("pe", lambda e: e.transpose(out, in_, ident), reads=[in_, ident], writes=[out])

    def act(self, out, in_, func, bias=None, scale=None, accum=None):
        kw = {}
        rd = [in_]
        if bias is not None:
            kw["bias"] = bias
            if not isinstance(bias, (int, float)):
                rd.append(bias)
        if scale is not None:
            kw["scale"] = scale
            if not isinstance(scale, (int, float)):
                rd.append(scale)
        wr = [out]
        if accum is not None:
            kw["accum_out"] = accum
            wr.append(accum)
            self.memset(accum, 0.0)
        self.add("act", lambda e: e.activation(out, in_, func, **kw), reads=rd, writes=wr)

    def tt(self, out, in0, in1, op, eng="dve"):
        self.add(eng, lambda e: e.tensor_tensor(out, in0, in1, op), reads=[in0, in1], writes=[out])

    def ts(self, out, in0, s1, s2=None, op0=ALU.mult, op1=None, eng="dve", accum=None):
        rd = [in0]
        if not isinstance(s1, (int, float)):
            rd.append(s1)
        if s2 is not None and not isinstance(s2, (int, float)):
            rd.append(s2)
        kw = {}
        if op1 is not None:
            kw["op1"] = op1
        wr = [out]
        if accum is not None:
            kw["accum_out"] = accum
            wr.append(accum)
        self.add(eng, lambda e: e.tensor_scalar(out, in0, s1, s2, op0, **kw), reads=rd, writes=wr)

    def stt(self, out, in0, s, in1, op0, op1, eng="dve"):
        rd = [in0, in1]
        if not isinstance(s, (int, float)):
            rd.append(s)
        self.add(eng, lambda e: e.scalar_tensor_tensor(out, in0, s, in1, op0, op1), reads=rd, writes=[out])

    def cp(self, out, in_, eng="dve"):
        if eng == "act":
            self.act(out, in_, AF.Copy)
        else:
            self.add(eng, lambda e: e.tensor_copy(out, in_), reads=[in_], writes=[out])

    def red(self, out, in_, op=ALU.add, eng="dve", axis=AX.X):
        self.add(eng, lambda e: e.tensor_reduce(out, in_, axis, op), reads=[in_], writes=[out])

    def memset(self, out, v, eng="dve"):
        self.add(eng, lambda e: e.memset(out, v), writes=[out])

    def dma(self, out, in_, q="sp", rk=None, wk=None, **kw):
        rd = [in_] if rk is None else list(rk)
        wr = [out] if wk is None else list(wk)
        self.add(q, lambda e: e.dma_start(out=out, in_=in_, **kw), reads=rd, writes=wr, dma=True)
from contextlib import ExitStack
import ml_dtypes
from concourse.bass_utils import run_bass_kernel_spmd

EPS = 1e-6
SCALE = 128.0 ** -0.5
NEGB = -30000.0


class Cfg:
    D = 2048
    KC = 16
    H = 8
    CC = 8
    FF = 5632
    FC = 44
    T = 256
    DS = 4
    HALO = 32
    MW = 36

    def __init__(self, S=4096, OWN=1024, NPG=128, NPOOL=1280):
        self.S = S
        self.OWN = OWN
        self.NPG = NPG
        self.NPOOL = NPOOL
        self.NKT = S // 128
        self.NBLK = S // 256
        self.NOWN = OWN // 256
        self.NT1 = S // 256
        self.NSB = NPG // 2


PV_GPRE_M, PV_GPRE_F, PV_GATTN, PV_GCONV, PV_GLN, PV_BLN, PV_BDW, PV_WDW = 0, 16, 32, 40, 48, 56, 64, 72
NPV = 72 + 8 * 31


def build(cfg):
    c = cfg
    nc = bass.Bass("TRN2", target_bir_lowering=False)
    P = Prog(nc)
    es = ExitStack()
    T, NKT, S = c.T, c.NKT, c.S

    def din(name, shape, dt=F32):
        return nc.dram_tensor(name, list(shape), dt, kind="ExternalInput")

    def dout(name, shape, dt=F32):
        return nc.dram_tensor(name, list(shape), dt, kind="ExternalOutput")

    ident_f_h = din("ident_f", [128, 128])
    ident_b_h = din("ident_b", [128, 128], BF16)
    cvec_h = din("cvec", [128, 16, 33])
    w_ada_h = din("w_ada", [2048, 12288])
    b_ada_h = din("b_ada33", [33, 12288])
    gpost_h = din("gpost33", [33, 2, 2048])
    pvec_h = din("pvec", [128, NPV])
    w_in_h = din("w_in", [2048, 5120])
    w_out_h = din("w_out", [2048, 2048])
    w_gu_h = din("w_gu", [2048, 11264])
    w_dn_h = din("w_down", [5632, 2048])
    xb_h = din("xb", [S, 2048])
    xown_h = din("xown", [c.OWN, 2048])
    xmini_h = din("xmini", [c.MW, 2048])
    hflag_h = din("haloflag", [128, 1])
    lpast_h = din("lpast", [36, NKT * 128], BF16)
    caus_h = din("caus", [128, 2, 256], BF16)
    rc_h = din("rc", [c.NOWN, 4, 8, 256], BF16)
    vbias_h = din("vbias", [c.OWN, 16])
    v01_h = din("v01", [c.OWN, 16])
    ck_h = din("cache_k", [c.NPOOL * 128, 1024])
    cv_h = din("cache_v", [c.NPOOL * 128, 1024])
    ptab_h = din("ptab", [1, c.NPG], I32)
    ioff_h = din("ioff", [128, 8])
    sconv_h = din("sconv", [30, 1024])
    sbias_h = din("sbias", [128, c.NPG, 8])
    bmask_h = din("bmask", [128, 32])
    fq_h = din("fq", [128, 8, 4])
    sown_h = din("sown", [4, 8, 4])
    eq_h = din("eqc", [4, 4, 128])

    y_own_h = dout("y_own", [c.OWN, 2048])
    k_own_h = dout("k_own", [c.OWN // 128, 8, 128, 128])
    v_own_h = dout("v_own", [c.OWN // 128, 8, 128, 128])
    convp_h = dout("convp", [30, 1024])
    y_s_h = dout("y_s", [4, 2048])
    k_s_h = dout("k_s", [8, 4, 128])
    v_s_h = dout("v_s", [8, 4, 128])
    conv_s_h = dout("conv_s", [30, 1024])

    kf_h = nc.dram_tensor("kf_scr", [8, 128, S], BF16)
    vt_h = nc.dram_tensor("vt_scr", [S, 1024], BF16)

    def sb(name, shape, dt=F32):
        t = es.enter_context(nc.sbuf_tensor(name, list(shape), dt))
        n = 1
        for s_ in shape[1:]:
            n *= s_
        P.reg(t, n * (2 if dt == BF16 else 4))
        return t

    banks = []
    for i in range(8):
        t = es.enter_context(nc.psum_tensor("pb%d" % i, [128, 512], F32))
        P.reg(t, 2048, psum=True)
        banks.append(t)
    rot = [0]

    def bank():
        b = banks[rot[0] % 4]
        rot[0] += 1
        return b
    BK_O, BK_S, BK_X, BK_Y = banks[4], banks[5], banks[6], banks[7]

    ident_f = sb("identf", [128, 128])
    ident_b = sb("identb", [128, 128], BF16)
    ones_f = sb("onesf", [128, 128])
    ones_b = sb("onesb", [128, 128], BF16)
    epsc = sb("epsc", [128, 1])
    pvec = sb("pvecs", [128, NPV])
    modcol = sb("modcol", [128, 192])
    abv = sb("abv", [128, 2, 2, 16])
    gtrow = sb("gtrow", [33, 2, 2048])
    lpast = sb("lpasts", [36, NKT * 128], BF16)
    caus = sb("causs", [128, 2, 256], BF16)
    rall = sb("rall", [36, 8, 256], BF16)
    kmsum = sb("kmsum", [128, 8, 16])
    hflag = sb("hflag", [128, 1])
    wsl = [sb("wsl%d" % i, [128, 16, 512], BF16) for i in range(3)]
    x1 = sb("x1", [128, 2, 2048])
    xn = sb("xn", [128, 1, 2048], BF16)
    hB = sb("hB", [128, 16, 256], BF16)
    bigA = sb("bigA", [128, 44, 256], BF16)
    f32A = sb("f32A", [128, 8, 288])
    f32B = sb("f32B", [128, 8, 256])
    f32C = sb("f32C", [128, 8, 256])
    scr16 = sb("scr16", [128, 4096])
    ptl = [sb("ptl%d" % i, [128, 256], BF16) for i in range(3)]
    t32 = [sb("t32_%d" % i, [128, 512]) for i in range(4)]
    sml = sb("sml", [128, 64])
    lnm = sb("lnm", [128, 256])
    lnv = sb("lnv", [128, 256])
    sel = sb("sel", [128, 4, 16])
    selb = sb("selb", [128, 16], BF16)
    sel2 = [sel, sb("sel_b", [128, 4, 16])]
    selb2 = [sb("selb2_%d" % i, [128, 16], BF16) for i in range(4)]
    vb_t = sb("vb_t", [128, 2, 16])
    v01_t = sb("v01_t", [128, 2, 16])
    uhalo = sb("uhalo", [128, 8, 32])
    usamp = sb("usamp", [128, 8, 36])
    qs_b = sb("qs_b", [128, 8, 4], BF16)
    qs_f = sb("qs_f", [128, 8, 4])
    kn_b = sb("kn_b", [128, 8, 4], BF16)
    vn_b = sb("vn_b", [4, 1024], BF16)
    kms = sb("kms", [128, 8, c.NSB])
    sbias = sb("sbias_s", [128, c.NPG, 8])
    bmask = sb("bmask_s", [128, 32])
    KT = [sb("KT%d" % i, [128, 8, 128], BF16) for i in range(2)]
    Pb = [sb("Pb%d" % i, [128, 8, 32], BF16) for i in range(2)]
    fq = sb("fq_s", [128, 8, 4])
    sown = sb("sown_s", [4, 8, 4])
    eqc = sb("eqc_s", [4, 4, 128])
    ptb_i = sb("ptb_i", [128, c.NPG], I32)
    ptb_f = sb("ptb_f", [128, c.NPG])
    ioff = sb("ioff_s", [128, 8])
    idx_i = sb("idx_i", [128, c.NPG], I32)
    sexp = [sb("sexp%d" % i, [128, 4], BF16) for i in range(2)]
    oat_s = sb("oat_s", [128, 8, 4])
    ue_s = sb("ue_s", [128, 8, 34])
    y_sm = sb("y_sm", [128, 8, 4])

    mrow = t32[0][0:33, :]
    brow = t32[1][0:33, :]
    grow = t32[2][0:33, :]
    idx_f = f32C[:, :, :].rearrange("p a b -> p (a b)")[:, 0:c.NPG]
    cst = scr16[0:30, 0:1024]
    _fa = f32A[:, :, :].rearrange("p a b -> p (a b)")
    cvs = _fa[:, 0:528].rearrange("p (k c) -> p k c", c=33)
    cvb = _fa[:, 1024:1288].bitcast(BF16).rearrange("p (k c) -> p k c", c=33)
    ksum = x1[:, 1, 0:8 * c.NPG].rearrange("p (h j) -> p h j", h=8)
    tctr = [0]

    def tmp32():
        t = t32[tctr[0] % 4]
        tctr[0] += 1
        return t
    pctr = [0]

    def ptile():
        t = ptl[pctr[0] % 3]
        pctr[0] += 1
        return t

    P.dma(ident_f[:, :], ident_f_h.ap())
    P.dma(ident_b[:, :], ident_b_h.ap())
    P.memset(ones_f[:, :], 1.0)
    P.memset(ones_b[:, :], 1.0)
    P.memset(epsc[:, :], EPS)
    P.memset(rall[:, :, :], 0.0)
    P.memset(kmsum[:, :, :], 0.0)
    P.dma(pvec[:, :], pvec_h.ap())
    P.dma(lpast[:, :], lpast_h.ap())
    P.dma(caus[:, :, :], caus_h.ap())
    P.dma(hflag[:, :], hflag_h.ap())
    P.dma(cvs[:, :, :], cvec_h.ap())
    P.dma(sbias[:, :, :], sbias_h.ap())
    P.dma(bmask[:, :], bmask_h.ap())
    P.dma(fq[:, :, :], fq_h.ap())
    P.dma(sown[:, :, :], sown_h.ap())
    P.dma(eqc[:, :, :], eq_h.ap())
    P.dma(ioff[:, :], ioff_h.ap())
    P.dma(ptb_i[:, :], ptab_h.ap().partition_broadcast(128))

    wctr = [0]

    def stream(src, kch, cols):
        slot = wsl[wctr[0] % 3]
        wctr[0] += 1
        v = src.rearrange("(kc p) c -> p kc c", p=128)
        for k0 in range(0, kch, 4):
            k1 = min(kch, k0 + 4)
            P.dma(slot[:, k0:k1, 0:cols], v[:, k0:k1, :], q="pool")
        return slot

    import os as _os
    KSUB = int(_os.environ.get("KSUB", "99"))
    if KSUB < 1:
        P.emit(es); es.close(); return nc
    P.act(cvb[:, :, :], cvs[:, :, :], AF.Silu)
    pmc = BK_X
    def mod_tile(t):
        wt = stream(w_ada_h[:, t * 512:(t + 1) * 512], 16, 512)
        pb = bank()
        for kc in range(16):
            P.mm(pb[0:33, :], cvb[:, kc, :], wt[:, kc, :], start=(kc == 0), stop=(kc == 15))
        P.dma(brow[:, :], b_ada_h[:, t * 512:(t + 1) * 512])
        P.tt(mrow[:, :], pb[0:33, :], brow[:, :], ALU.add)
        for ri, r in enumerate((0, 32)):
            for j in range(4):
                col = ri * 96 + t * 4 + j
                P.mm(pmc[:, col:col + 1], mrow[r:r + 1, j * 128:(j + 1) * 128], ones_f[r:r + 1, 0:1])
        which = {2: 0, 5: 1}.get(t // 4)
        if which is not None:
            cs = slice((t % 4) * 512, (t % 4 + 1) * 512)
            P.dma(grow[:, :], gpost_h[:, which, cs])
            P.tt(gtrow[:, which, cs], mrow[:, :], grow[:, :], ALU.mult)

    for t in range(8):
        mod_tile(t)
    for ri in range(2):
        o = ri * 96
        P.cp(modcol[:, o:o + 32], pmc[:, o:o + 32])
        P.stt(abv[:, ri, 0, :], modcol[:, o + 16:o + 32], 1.0, pvec[:, PV_GPRE_M:PV_GPRE_M + 16], ALU.add, ALU.mult)
    mod_rest = list(range(8, 24))

    def mod_finish():
        while mod_rest:
            mod_tile(mod_rest.pop(0))
        for ri in range(2):
            o = ri * 96
            P.cp(modcol[:, o + 32:o + 96], pmc[:, o + 32:o + 96])
            P.stt(abv[:, ri, 1, :], modcol[:, o + 64:o + 80], 1.0, pvec[:, PV_GPRE_F:PV_GPRE_F + 16], ALU.add, ALU.mult)

    def ab(ri, f, kc):
        o = ri * 96 + (48 if f else 0)
        return abv[:, ri, f, kc:kc + 1], modcol[:, o + kc:o + kc + 1]

    def rstd_from(out, in_, inv_n, M):
        P.act(out, in_, AF.Sqrt, bias=epsc[0:M, 0:1], scale=inv_n)
        P.add("dve", lambda e: e.reciprocal(out, out), reads=[out], writes=[out])

    def prologue(subs, f, colparams):
        for (si, M, coff) in subs:
            ssq = sml[0:M, 0:1]
            P.act(xn[0:M, 0, :], x1[0:M, si, :], AF.Square, accum=ssq)
            rs = sml[0:M, 1:2]
            rstd_from(rs, ssq, 1.0 / 2048, M)
            P.ts(xn[0:M, 0, :], x1[0:M, si, :], rs, None, op0=ALU.mult)
            for kc in range(16):
                pb = bank()
                pv = pb[:, :].bitcast(BF16)
                P.tr(pv[:, 0:M], xn[0:M, 0, kc * 128:(kc + 1) * 128], ident_b[0:M, 0:M])
                for (c0, c1, ri) in colparams:
                    a, b = ab(ri, f, kc)
                    if kc % 2 == 0:
                        P.act(hB[:, kc, coff + c0:coff + c1], pv[:, c0:c1], AF.Identity, bias=b, scale=a)
                    else:
                        P.ts(hB[:, kc, coff + c0:coff + c1], pv[:, c0:c1], a, b, op0=ALU.mult, op1=ALU.add)

    def linear_fm(wt, kch, rhs_kc, N, mch, cb):
        for m in range(mch):
            pb = bank()
            for kc in range(kch):
                P.mm(pb[:, 0:N], wt[:, kc, m * 128:(m + 1) * 128], rhs_kc(kc), start=(kc == 0), stop=(kc == kch - 1))
            cb(m, pb[:, 0:N])

    def fm_sumsq(chunks, N, pacc):
        n = len(chunks)
        for i, ch in enumerate(chunks):
            t = tmp32()
            P.act(t[:, 0:N], ch, AF.Square)
            P.mm(pacc[:, 0:N], ones_f[:, :], t[:, 0:N], start=(i == 0), stop=(i == n - 1))

    def fm_rmsnorm_to(chunks, N, gcol0, dst_of):
        fm_sumsq(chunks, N, BK_X)
        rt = tmp32()
        rstd_from(rt[:, 0:N], BK_X[:, 0:N], 1.0 / (128 * len(chunks)), 128)
        for i, ch in enumerate(chunks):
            P.stt(dst_of(i), ch, pvec[:, gcol0 + i:gcol0 + i + 1], rt[:, 0:N], ALU.mult, ALU.mult)

    def conv_post(ych, N):
        for i in range(8):
            P.mm(BK_X[:, 0:N], ones_f[:, :], ych(i), start=(i == 0), stop=(i == 7))
        fm_sumsq([ych(i) for i in range(8)], N, BK_Y)
        mean = lnm
        P.ts(mean[:, 0:N], BK_X[:, 0:N], 1.0 / 1024, None, op0=ALU.mult)
        msq = tmp32()
        P.tt(msq[:, 0:N], mean[:, 0:N], mean[:, 0:N], ALU.mult)
        var = lnv
        P.stt(var[:, 0:N], BK_Y[:, 0:N], 1.0 / 1024, msq[:, 0:N], ALU.mult, ALU.subtract)
        rstd_from(var[:, 0:N], var[:, 0:N], 1.0, 128)
        for i in range(8):
            y = ych(i)
            P.tt(y, y, mean[:, 0:N], ALU.subtract)
            P.tt(y, y, var[:, 0:N], ALU.mult)
            P.act(y, y, AF.Identity, bias=pvec[:, PV_BLN + i:PV_BLN + i + 1], scale=pvec[:, PV_GLN + i:PV_GLN + i + 1])
            sg = tmp32()
            P.act(sg[:, 0:N], y, AF.Sigmoid)
            P.tt(y, y, sg[:, 0:N], ALU.mult)

    def proj_post(w_h, kch_total, kgrp, lhs_kc, subs, ri, which, dst_cb):
        stages = [f32B[:, :, :].rearrange("p a b -> p (a b)"), f32C[:, :, :].rearrange("p a b -> p (a b)")]
        ngr = (kch_total + kgrp - 1) // kgrp
        r = 32 * ri
        for ct in range(4):
            pbs = [bank() for _ in subs]
            for g in range(ngr):
                k0 = g * kgrp
                kn = min(kgrp, kch_total - k0)
                wt = stream(w_h[k0 * 128:(k0 + kn) * 128, ct * 512:(ct + 1) * 512], kn, 512)
                for (si, M), pb in zip(subs, pbs):
                    for kk in range(kn):
                        kc = k0 + kk
                        P.mm(pb[0:M, :], lhs_kc(kc, si, M), wt[:, kk, :], start=(kc == 0), stop=(kc == kch_total - 1))
            for (si, M), pb in zip(subs, pbs):
                st = stages[si]
                P.cp(st[0:M, ct * 512:(ct + 1) * 512], pb[0:M, :])
                j = tmp32()
                P.act(j[0:M, :], pb[0:M, :], AF.Square, accum=sml[0:M, 8 + si * 4 + ct:9 + si * 4 + ct])
        for (si, M) in subs:
            st = stages[si]
            ss = sml[0:M, 16 + si:17 + si]
            P.red(ss, sml[0:M, 8 + si * 4:12 + si * 4])
            rstd_from(ss, ss, 1.0 / 2048, M)
            for ct in range(4):
                cs = slice(ct * 512, (ct + 1) * 512)
                pg = bank()
                P.mm(pg[0:M, :], ones_f[r:r + 1, 0:M], gtrow[r:r + 1, which, cs])
                tw = tmp32()
                P.stt(tw[0:M, :], st[0:M, cs], ss, pg[0:M, :], ALU.mult, ALU.mult)
                P.tt(st[0:M, cs], x1[0:M, si, cs], tw[0:M, :], ALU.add)
            dst_cb(si, M, st)

    def ffn(subs, colparams, N, ri, out_cb):
        prologue([(si, M, si * 128) for (si, M) in subs], 1, colparams)
        for g in range(11):
            wg = stream(w_gu_h[:, g * 512:(g + 1) * 512], 16, 512)
            wu = stream(w_gu_h[:, 5632 + g * 512:5632 + (g + 1) * 512], 16, 512)
            for m in range(4):
                pg = bank()
                for kc in range(16):
                    P.mm(pg[:, 0:N], wg[:, kc, m * 128:(m + 1) * 128], hB[:, kc, 0:N], start=(kc == 0), stop=(kc == 15))
                P.act(f32A[:, m, 0:N], pg[:, 0:N], AF.Silu)
            for m in range(4):
                pu = bank()
                for kc in range(16):
                    P.mm(pu[:, 0:N], wu[:, kc, m * 128:(m + 1) * 128], hB[:, kc, 0:N], start=(kc == 0), stop=(kc == 15))
                P.tt(bigA[:, g * 4 + m, 0:N], f32A[:, m, 0:N], pu[:, 0:N], ALU.mult)
        proj_post(w_dn_h, 44, 11, lambda kc, si, M: bigA[:, kc, si * 128:si * 128 + M], subs, ri, 1, out_cb)

    def mix_out(subs, N, ri):
        def dst(si, M, st):
            P.cp(x1[0:M, si, :], st[0:M, :])
        proj_post(w_out_h, 16, 16, lambda kc, si, M: bigA[:, 16 + kc, si * 128:si * 128 + M], subs, ri, 0, dst)

    def transpose_out(src_cc, ncols, dst_h):
        for cc in range(8):
            pb = bank()
            P.tr(pb[0:ncols, 0:128], src_cc(cc), ident_f[:, :])
            P.cp(cst[0:ncols, cc * 128:(cc + 1) * 128], pb[0:ncols, 0:128])
        P.dma(dst_h.ap(), cst[0:ncols, :])

    import os as _os
    STAGE = int(_os.environ.get("KSTAGE", "99"))
    kst = bigA[:, 0:8, :]
    vst = bigA[:, 8:16, :].rearrange("p (s a) b -> p s (a b)", s=2)
    KP1 = int(_os.environ.get("KP1", "99"))
    KP1SUB = int(_os.environ.get("KP1SUB", "99"))
    for ti in range(min(KP1, c.NT1 - 1) if STAGE >= 1 else 0):
        for si in range(2):
            P.dma(x1[:, si, :], xb_h[ti * 256 + si * 128:ti * 256 + (si + 1) * 128, :])
        prologue([(0, 128, 0), (1, 128, 128)], 0, [(0, 128, 0)])
        if KP1SUB < 1:
            continue
        for half in range(2):
            wt = stream(w_in_h[:, 1024 + half * 512:1024 + (half + 1) * 512], 16, 512)

            KD = int(_os.environ.get("KDBG", "7"))

            def cbk(m, pb, half=half):
                hh = half * 4 + m
                if KD & 2:
                    P.cp(kst[:, hh, :], pb, eng="act")
                if KD & 4:
                    P.red(kmsum[:, hh, ti:ti + 1], pb)
            if KD & 1:
                linear_fm(wt, 16, lambda kc: hB[:, kc, :], 256, 4, cbk)
        if KP1SUB < 2:
            continue
        P.dma(kf_h.ap()[:, :, ti * 256:(ti + 1) * 256].rearrange("h p t -> p h t"), kst, wk=[("kf", ti)])
        for half in range(2):
            wt = stream(w_in_h[:, 2048 + half * 512:2048 + (half + 1) * 512], 16, 512)
            for si in range(2):
                pb = bank()
                for kc in range(16):
                    P.mm(pb[:, :], hB[:, kc, si * 128:(si + 1) * 128], wt[:, kc, :], start=(kc == 0), stop=(kc == 15))
                P.cp(vst[:, si, half * 512:(half + 1) * 512], pb[:, :], eng=("act" if si else "dve"))
        P.dma(vt_h.ap()[ti * 256:(ti + 1) * 256, :].rearrange("(s p) c -> p s c", p=128), vst, wk=[("vt", ti)])
        if mod_rest:
            mod_tile(mod_rest.pop(0))
    mod_finish()
    kmT = sb("kmT", [128, 8, 16])
    P.ts(kmT[:, :, :], kmsum[:, :, :], 1.0 / 256, None, op0=ALU.mult)
    kf_keys = [("kf", i) for i in range(c.NT1 - 1)]
    vt_keys = [("vt", i) for i in range(c.NT1 - 1)]

    qf = bigA[:, 0:8, :]
    kfm = bigA[:, 8:16, :]
    cat = bigA[:, 16:32, :]
    vown = bigA[:, 32:40, :].rearrange("p (s a) b -> p s (a b)", s=2)
    kvst = [f32B[:, :, :].rearrange("p a b -> p (a b)"), f32C[:, :, :].rearrange("p a b -> p (a b)")]

    def inproj(N, subs, mini, tile_i):
        rhs = lambda kc: hB[:, kc, 0:N]
        for ct in range(4):
            wt = stream(w_in_h[:, 1024 + ct * 512:1024 + (ct + 1) * 512], 16, 512)
            for (si, M) in subs:
                pb = bank()
                for kc in range(16):
                    P.mm(pb[0:M, :], hB[:, kc, si * 128:si * 128 + M], wt[:, kc, :], start=(kc == 0), stop=(kc == 15))
                P.cp(kvst[si][0:M, ct * 512:(ct + 1) * 512], pb[0:M, :], eng=("act" if ct % 2 else "dve"))
        for (si, M) in subs:
            if mini:
                P.dma(k_s_h.ap().rearrange("h t d -> t h d"), kvst[0][0:4, 0:1024].rearrange("t (h d) -> t h d", h=8))
                P.dma(v_s_h.ap().rearrange("h t d -> t h d"), kvst[0][0:4, 1024:2048].rearrange("t (h d) -> t h d", h=8))
                P.cp(vn_b[0:4, :], kvst[0][0:4, 1024:2048])
            else:
                pg = tile_i * 2 + si
                P.dma(k_own_h.ap()[pg].rearrange("h t d -> t h d"), kvst[si][:, 0:1024].rearrange("t (h d) -> t h d", h=8))
                P.dma(v_own_h.ap()[pg].rearrange("h t d -> t h d"), kvst[si][:, 1024:2048].rearrange("t (h d) -> t h d", h=8))
                P.cp(vown[:, si, :], kvst[si][:, 1024:2048])
        for half in range(2):
            wt = stream(w_in_h[:, half * 512:(half + 1) * 512], 16, 512)

            def cbq(m, pb, half=half):
                hh = half * 4 + m
                P.act(qf[:, hh, 0:N], pb, AF.Copy, scale=SCALE)
                if mini:
                    P.ts(qs_f[:, hh, :], pb[:, 0:4], SCALE, None, op0=ALU.mult)
                    P.cp(qs_b[:, hh, :], qf[:, hh, 0:4])
                else:
                    q32 = tmp32()
                    P.ts(q32[:, 0:N], pb, SCALE, None, op0=ALU.mult)
                    select_main(hh, q32)
            linear_fm(wt, 16, rhs, N, 4, cbq)
        select_flush()
        for half in range(2):
            wt = stream(w_in_h[:, 1024 + half * 512:1024 + (half + 1) * 512], 16, 512)

            def cbk(m, pb, half=half):
                hh = half * 4 + m
                P.cp(kfm[:, hh, 0:N], pb, eng="act")
                if mini:
                    P.cp(kn_b[:, hh, :], kfm[:, hh, 0:4])
            linear_fm(wt, 16, rhs, N, 4, cbk)
        for half in range(2):
            wt = stream(w_in_h[:, 3072 + half * 512:3072 + (half + 1) * 512], 16, 512)

            def cba(m, pb, half=half):
                P.cp(f32B[:, half * 4 + m, 0:N], pb, eng="act")
            linear_fm(wt, 16, rhs, N, 4, cba)
        for half in range(2):
            wt = stream(w_in_h[:, 4096 + half * 512:4096 + (half + 1) * 512], 16, 512)

            def cbg(m, pb, half=half):
                cc = half * 4 + m
                sg = tmp32()
                P.act(sg[:, 0:N], pb, AF.Sigmoid)
                if mini:
                    P.tt(usamp[:, cc, 0:N], f32B[:, cc, 0:N], sg[:, 0:N], ALU.mult)
                else:
                    P.tt(f32A[:, cc, 32:32 + N], f32B[:, cc, 0:N], sg[:, 0:N], ALU.mult)
            linear_fm(wt, 16, rhs, N, 4, cbg)

    sel_pending = []

    def select_flush():
        while sel_pending:
            hh, si, sbf = sel_pending.pop(0)
            pt = bank()
            pv = pt[:, :].bitcast(BF16)
            P.tr(pv[0:16, 0:128], sbf[:, :], ident_b[:, :])
            P.cp(rall[0:16, hh, si * 128:(si + 1) * 128], pv[0:16, 0:128], eng="act")

    def select_main(hh, q32):
        pbs = []
        for si in range(2):
            pb = bank()
            P.mm(pb[:, 0:16], q32[:, si * 128:(si + 1) * 128], kmT[:, hh, :])
            pbs.append(pb)
        select_flush()
        for si in range(2):
            pb = pbs[si]
            s0 = sel2[si][:, 0, :]
            P.tt(s0, pb[:, 0:16], vb_t[:, si, :], ALU.add)
            P.add("dve", lambda e, si=si: e.max(sel2[si][:, 1, 0:8], sel2[si][:, 0, :]), reads=[s0], writes=[sel2[si][:, 1, 0:8]])
            P.ts(sel2[si][:, 2, :], s0, sel2[si][:, 1, 2:3], None, op0=ALU.is_ge)
            P.tt(sel2[si][:, 2, :], sel2[si][:, 2, :], v01_t[:, si, :], ALU.mult)
            sbf = selb2[(hh * 2 + si) % 4]
            P.ts(sbf[:, :], sel2[si][:, 2, :], -1.0, -NEGB, op0=ALU.add, op1=ALU.mult)
            sel_pending.append((hh, si, sbf))

    kA = scr16[:, 0:2048].bitcast(BF16)
    vA = scr16[:, 2048:4096].bitcast(BF16).rearrange("p (k d) -> p k d", d=128)

    def attention_main(per_head=None, npast=None):
        NP = NKT if npast is None else min(NKT, npast)
        for hh in range(8):
            P.dma(kA[:, 0:NP * 128], kf_h.ap()[hh][:, 0:NP * 128], rk=kf_keys)
            P.dma(vA[:, 0:NP, :], vt_h.ap()[0:NP * 128, hh * 128:(hh + 1) * 128].rearrange("(k p) d -> p k d", p=128), rk=vt_keys)
            tot = NP + 2

            def emit_s(i):
                pS = bank()
                if i < NP:
                    P.mm(pS[:, 0:256], kA[:, i * 128:(i + 1) * 128], qf[:, hh, :], start=True, stop=False)
                    P.mm(pS[:, 0:256], lpast[0:36, i * 128:(i + 1) * 128], rall[0:36, hh, :], start=False, stop=True)
                else:
                    jt = i - NP
                    P.mm(pS[:, 0:256], kfm[:, hh, jt * 128:(jt + 1) * 128], qf[:, hh, :], start=True, stop=False)
                    P.mm(pS[:, 0:256], lpast[32:35, jt * 128:(jt + 1) * 128], rall[32:35, hh, :], start=False, stop=False)
                    P.mm(pS[:, 0:256], ident_b[:, :], caus[:, jt, :], start=False, stop=True)
                return pS
            LOOK = 2
            sb_ = {}
            for i in range(min(LOOK, tot)):
                sb_[i] = emit_s(i)
            for i in range(tot):
                pS = sb_.pop(i)
                if i < NP:
                    vt_ = vA[:, i, :]
                else:
                    jt = i - NP
                    vt_ = vown[:, jt, hh * 128:(hh + 1) * 128]
                pt = ptile()
                P.act(pt[:, :], pS[:, 0:256], AF.Exp)
                P.mm(BK_O[:, 0:256], vt_, pt[:, :], start=(i == 0), stop=(i == tot - 1))
                P.mm(BK_S[:, 0:256], ones_b[:, :], pt[:, :], start=(i == 0), stop=(i == tot - 1))
                if i + LOOK < tot:
                    sb_[i + LOOK] = emit_s(i + LOOK)
            if per_head is not None:
                per_head(hh)
            rs = tmp32()
            P.add("dve", lambda e, rs=rs: e.reciprocal(rs[:, 0:256], BK_S[:, 0:256]), reads=[BK_S[:, 0:256]], writes=[rs[:, 0:256]])
            P.tt(f32C[:, hh, :], BK_O[:, 0:256], rs[:, 0:256], ALU.mult)

    O_s = scr16[:, 0:2048].rearrange("p (h n q) -> p h n q", h=8, q=4)
    L_s = scr16[:, 2048:4096].rearrange("p (h n q) -> p h n q", h=8, q=4)
    kpg = [f32B[:, :, :].rearrange("p a b -> p (a b)")[:, 0:1024].rearrange("p (h d) -> p h d", h=8),
           f32B[:, :, :].rearrange("p a b -> p (a b)")[:, 1024:2048].rearrange("p (h d) -> p h d", h=8)]
    vpg = [f32C[:, :, :].rearrange("p a b -> p (a b)")[:, 0:1024].rearrange("p (h d) -> p h d", h=8),
           f32C[:, :, :].rearrange("p a b -> p (a b)")[:, 1024:2048].rearrange("p (h d) -> p h d", h=8)]
    vpb = f32A[:, :, :].rearrange("p a b -> p (a b)")[:, 0:1024].bitcast(BF16).rearrange("p (e h d) -> p e h d", e=2, h=8)

    def attention_sample():
        P.cp(ptb_f[:, :], ptb_i[:, :])
        P.ts(idx_f, ptb_f[:, :], 128.0, ioff[:, 0:1], op0=ALU.mult, op1=ALU.add)
        P.cp(idx_i[:, :], idx_f)
        qall = qs_b[:, :, :].rearrange("p h q -> p (h q)")
        kflat = f32B[:, :, :].rearrange("p a b -> p (a b)")
        vflat = f32C[:, :, :].rearrange("p a b -> p (a b)")
        vpb4 = f32A[:, :, :].rearrange("p a b -> p (a b)")[:, 0:1024].bitcast(BF16).rearrange("p (e r d) -> p e r d", e=2, r=8)
        for n in range(c.NSB):
            for e_ in range(2):
                j = 2 * n + e_
                ix = idx_i[:, j:j + 1]
                ko = kflat[:, e_ * 1024:(e_ + 1) * 1024]
                vo = vflat[:, e_ * 1024:(e_ + 1) * 1024]
                P.add("pool", lambda e, o=ko, ix=ix: e.indirect_dma_start(
                    out=o, out_offset=None, in_=ck_h.ap(), in_offset=bass.IndirectOffsetOnAxis(ap=ix, axis=0)),
                    reads=[ix], writes=[ko], dma=True)
                P.add("pool", lambda e, o=vo, ix=ix: e.indirect_dma_start(
                    out=o, out_offset=None, in_=cv_h.ap(), in_offset=bass.IndirectOffsetOnAxis(ap=ix, axis=0)),
                    reads=[ix], writes=[vo], dma=True)
                P.cp(vpb4[:, e_, :, :], vo.rearrange("p (r d) -> p r d", r=8))
            for e_ in range(2):
                j = 2 * n + e_
                ko = kflat[:, e_ * 1024:(e_ + 1) * 1024]
                kt = KT[e_]
                for half in range(2):
                    pT = bank()
                    for rr in range(4):
                        r = half * 4 + rr
                        P.tr(pT[:, rr * 128:(rr + 1) * 128], ko[:, r * 128:(r + 1) * 128], ident_f[:, :])
                    P.cp(kt[:, half * 4:(half + 1) * 4, :], pT[:, :].rearrange("p (r k) -> p r k", r=4), eng="act")
                P.red(ksum[:, :, j], kt[:, :, :].rearrange("p r (h g) -> p h r g", h=8), axis=AX.XY)
                pS = bank()
                for r in range(8):
                    P.mm(pS[:, r * 32:(r + 1) * 32], kt[:, r, :], qall)
                t1 = tmp32()
                t3 = t1[:, 0:256].rearrange("p (r k) -> p r k", r=8)
                P.tt(t3, pS[:, 0:256].rearrange("p (r k) -> p r k", r=8),
                     sbias[:, j, :].unsqueeze(2).to_broadcast([128, 8, 32]), ALU.add)
                P.act(t1[:, 0:256], t1[:, 0:256], AF.Exp)
                P.tt(Pb[e_][:, :, :], t3, bmask[:, :].unsqueeze(1).to_broadcast([128, 8, 32]), ALU.mult)
            cnt = 0
            for e_ in range(2):
                for r in range(8):
                    P.mm(BK_O[:, 0:32], vpb4[:, e_, r, :], Pb[e_][:, r, :], start=(cnt == 0), stop=(cnt == 15))
                    cnt += 1
            cnt = 0
            for e_ in range(2):
                for r in range(8):
                    P.mm(BK_S[:, 0:32], ones_b[:, :], Pb[e_][:, r, :], start=(cnt == 0), stop=(cnt == 15))
                    cnt += 1
            P.cp(O_s[:, :, n, :], BK_O[:, 0:32].rearrange("p (h q) -> p h q", q=4))
            P.cp(L_s[:, :, n, :], BK_S[:, 0:32].rearrange("p (h q) -> p h q", q=4), eng="act")
        NSB = c.NSB
        P.red(kms[:, :, :], ksum[:, :, :].rearrange("p h (n e) -> p h n e", e=2))
        P.ts(kms[:, :, :], kms[:, :, :], 1.0 / 256, None, op0=ALU.mult)
        ncol = max(NSB, 8)
        for hh in range(8):
            pb = bank()
            P.mm(pb[0:4, 0:NSB], qs_f[:, hh, :], kms[:, hh, :])
            P.memset(sel[0:4, :, :], -1e9)
            sv = sel[0:4, :, :].rearrange("p a b -> p (a b)")
            P.cp(sv[:, 0:NSB], pb[0:4, 0:NSB])
            P.add("dve", lambda e, sv=sv: e.max(sml[0:4, 32:40], sv[:, 0:64]), reads=[sv[:, 0:64]], writes=[sml[0:4, 32:40]])
            m01 = tmp32()
            P.ts(m01[0:4, 0:NSB], sv[:, 0:NSB], sml[0:4, 34:35], None, op0=ALU.is_ge)
            pB = bank()
            for q in range(4):
                P.mm(pB[:, q * NSB:(q + 1) * NSB], eqc[0:4, q, :], m01[0:4, 0:NSB])
            pBv = pB[:, 0:4 * NSB].rearrange("p (q n) -> p q n", q=4)
            tm = tmp32()
            tmv = tm[:, 0:4 * NSB].rearrange("p (q n) -> p q n", q=4)
            P.tt(tmv, O_s[:, hh, 0:NSB, :].rearrange("p n q -> p q n"), pBv, ALU.mult)
            P.red(sml[:, 40:44], tmv)
            tm2 = tmp32()
            tmv2 = tm2[:, 0:4 * NSB].rearrange("p (q n) -> p q n", q=4)
            P.tt(tmv2, L_s[:, hh, 0:NSB, :].rearrange("p n q -> p q n"), pBv, ALU.mult)
            P.red(sml[:, 44:48], tmv2)
            pS = bank()
            P.mm(pS[0:4, 0:4], kn_b[:, hh, :], qs_b[:, hh, :])
            so = tmp32()
            P.tt(so[0:4, 0:4], pS[0:4, 0:4], sown[:, hh, :], ALU.add)
            P.act(sexp[0][0:4, 0:4], so[0:4, 0:4], AF.Exp)
            P.mm(BK_O[:, 0:4], vn_b[0:4, hh * 128:(hh + 1) * 128], sexp[0][0:4, 0:4])
            P.mm(BK_S[:, 0:4], ones_b[0:4, :], sexp[0][0:4, 0:4])
            P.tt(sml[:, 40:44], sml[:, 40:44], fq[:, hh, :], ALU.mult)
            P.tt(sml[:, 44:48], sml[:, 44:48], fq[:, hh, :], ALU.mult)
            P.tt(sml[:, 40:44], sml[:, 40:44], BK_O[:, 0:4], ALU.add)
            P.tt(sml[:, 44:48], sml[:, 44:48], BK_S[:, 0:4], ALU.add)
            P.add("dve", lambda e: e.reciprocal(sml[:, 44:48], sml[:, 44:48]), reads=[sml[:, 44:48]], writes=[sml[:, 44:48]])
            P.tt(oat_s[:, hh, :], sml[:, 40:44], sml[:, 44:48], ALU.mult)

    def finish():
        P.emit(es)
        es.close()
        return nc
    if STAGE < 2:
        return finish()
    P.dma(x1[0:c.MW, 0, :], xmini_h.ap())
    prologue([(0, c.MW, 0)], 0, [(0, 4, 1), (4, c.MW, 0)])
    inproj(c.MW, [(0, 4)], True, 0)
    for cc in range(8):
        P.ts(uhalo[:, cc, :], usamp[:, cc, 4:36], hflag[:, 0:1], None, op0=ALU.mult)
    if STAGE < 3:
        return finish()
    P.dma(cst[0:30, :], sconv_h.ap())
    for cc in range(8):
        pb = bank()
        P.tr(pb[:, 0:30], cst[0:30, cc * 128:(cc + 1) * 128], ident_f[0:30, 0:30])
        P.cp(ue_s[:, cc, 0:30], pb[:, 0:30])
        P.cp(ue_s[:, cc, 30:34], usamp[:, cc, 0:4])
    for cc in range(8):
        for t_ in range(4):
            tw = tmp32()
            P.tt(tw[:, 0:31], pvec[:, PV_WDW + cc * 31:PV_WDW + (cc + 1) * 31], ue_s[:, cc, t_:t_ + 31], ALU.mult)
            P.red(y_sm[:, cc, t_:t_ + 1], tw[:, 0:31])
        P.ts(y_sm[:, cc, :], y_sm[:, cc, :], pvec[:, PV_BDW + cc:PV_BDW + cc + 1], None, op0=ALU.add)
    transpose_out(lambda cc: ue_s[:, cc, 4:34], 30, conv_s_h)
    if STAGE < 4:
        return finish()
    attention_sample()
    if STAGE < 5:
        return finish()
    fm_rmsnorm_to([oat_s[:, hh, :] for hh in range(8)], 4, PV_GATTN, lambda i: cat[:, i, 0:4])
    conv_post(lambda cc: y_sm[:, cc, :], 4)
    fm_rmsnorm_to([y_sm[:, cc, :] for cc in range(8)], 4, PV_GCONV, lambda i: cat[:, 8 + i, 0:4])
    mix_out([(0, 4)], 4, 1)

    def out_s(si, M, st):
        P.dma(y_s_h.ap(), st[0:4, :])
    ffn([(0, 4)], [(0, 4, 1)], 4, 1, out_s)

    if STAGE < 6:
        return finish()
    for it in range(c.NOWN):
        for si in range(2):
            r0 = it * 256 + si * 128
            P.dma(x1[:, si, :], xown_h[r0:r0 + 128, :])
            P.dma(vb_t[:, si, :], vbias_h[r0:r0 + 128, :])
            P.dma(v01_t[:, si, :], v01_h[r0:r0 + 128, :])
        P.dma(rall[32:36, :, :], rc_h[it])
        subs = [(0, 128), (1, 128)]
        prologue([(0, 128, 0), (1, 128, 128)], 0, [(0, 128, 0)])
        inproj(256, subs, False, it)
        for cc in range(8):
            P.cp(f32A[:, cc, 0:32], uhalo[:, cc, :], eng="act")

        def conv_chunk(cc):
            y = f32B[:, cc, :]
            w0 = PV_WDW + cc * 31
            P.ts(y, f32A[:, cc, 2:258], pvec[:, w0:w0 + 1], pvec[:, PV_BDW + cc:PV_BDW + cc + 1], op0=ALU.mult, op1=ALU.add)
            for j in range(1, 31):
                P.stt(y, f32A[:, cc, 2 + j:258 + j], pvec[:, w0 + j:w0 + j + 1], y, ALU.mult, ALU.add)
        def head_hook(hh):
            if hh < 4:
                conv_chunk(2 * hh)
                conv_chunk(2 * hh + 1)
            elif hh == 4:
                conv_post(lambda cc: f32B[:, cc, :], 256)
            elif hh == 5:
                fm_rmsnorm_to([f32B[:, cc, :] for cc in range(8)], 256, PV_GCONV, lambda i: cat[:, 8 + i, :])
        attention_main(head_hook, npast=2 * ((S - c.OWN) // 256 + it))
        if it == c.NOWN - 1:
            transpose_out(lambda cc: f32A[:, cc, 258:288], 30, convp_h)
        for cc in range(8):
            P.cp(uhalo[:, cc, :], f32A[:, cc, 256:288], eng="act")
        fm_rmsnorm_to([f32C[:, hh, :] for hh in range(8)], 256, PV_GATTN, lambda i: cat[:, i, :])
        mix_out(subs, 256, 0)

        def out_m(si, M, st, it=it):
            r0 = it * 256 + si * 128
            P.dma(y_own_h.ap()[r0:r0 + 128, :], st[:, :])
        ffn(subs, [(0, 128, 0)], 256, 0, out_m)

    P.emit(es)
    es.close()
    return nc


def _bf(a):
    return np.asarray(a, dtype=np.float32).astype(ml_dtypes.bfloat16)


def host_consts(cfg, qstart):
    c = cfg
    m = (2.0 ** (-8.0 * (np.arange(8) + 1.0) / 8)).astype(np.float64)
    NKT = c.NKT
    lp = np.zeros((36, NKT * 128), np.float32)
    for kt in range(NKT):
        sl = slice(kt * 128, (kt + 1) * 128)
        if kt // 2 < 16:
            lp[kt // 2, sl] = 1.0
        lp[32, sl] = 1.0
        lp[33, sl] = np.arange(128)
        lp[34, sl] = 128.0 * kt
        lp[35, sl] = 1.0
    j = np.arange(256)
    caus = np.zeros((128, 2, 256), np.float32)
    for jt in range(2):
        kidx = 128 * jt + np.arange(128)
        caus[:, jt, :] = np.where(kidx[:, None] <= j[None, :], 0.0, NEGB)
    rc = np.zeros((c.NOWN, 4, 8, 256), np.float32)
    for it in range(c.NOWN):
        q0 = qstart + it * 256
        for h in range(8):
            rc[it, 0, h] = -m[h] * j
            rc[it, 1, h] = m[h]
            rc[it, 2, h] = m[h]
            rc[it, 3, h] = -m[h] * q0
    tq = qstart + np.arange(c.OWN)
    nfull = tq // 256
    v01 = (np.arange(16)[None, :] < nfull[:, None]).astype(np.float32)
    vbias = np.where(v01 > 0, 0.0, -1e9).astype(np.float32)
    past = c.NPG * 128
    sbias = np.zeros((128, c.NPG, 8), np.float32)
    bmask = np.zeros((128, 32), np.float32)
    for h in range(8):
        bmask[h * 16:(h + 1) * 16, h * 4:(h + 1) * 4] = 1.0
        for g in range(16):
            for jp in range(c.NPG):
                sbias[h * 16 + g, jp, :] = m[h] * (128 * jp + 8 * g + np.arange(8) - past)
    fq = np.zeros((128, 8, 4), np.float32)
    sown = np.zeros((4, 8, 4), np.float32)
    for h in range(8):
        for q in range(4):
            fq[:, h, q] = np.exp(-m[h] * q)
            for k in range(4):
                sown[k, h, q] = -m[h] * (q - k) if k <= q else NEGB
    eqc = np.zeros((4, 4, 128), np.float32)
    for q in range(4):
        eqc[q, q, :] = 1.0
    ioff = (np.arange(128)[:, None] + 128.0 * np.arange(8)[None, :]).astype(np.float32)
    return dict(lpast=_bf(lp), caus=_bf(caus), rc=_bf(rc), vbias=vbias, v01=v01, sbias=sbias, bmask=bmask, fq=fq,
                sown=sown, eqc=eqc, ioff=ioff, ident_f=np.eye(128, dtype=np.float32), ident_b=_bf(np.eye(128)))


def core_inputs(cfg, inp, b, qstart, sb_):
    c = cfg
    f = np.float32
    d = host_consts(c, qstart)
    col = lambda v: np.ascontiguousarray(np.asarray(v, f).reshape(-1, 128).T)
    pv = np.zeros((128, NPV), f)
    pv[:, PV_GPRE_M:PV_GPRE_M + 16] = col(inp["g_pre_mix"][0])
    pv[:, PV_GPRE_F:PV_GPRE_F + 16] = col(inp["g_pre_ffn"][0])
    pv[:, PV_GATTN:PV_GATTN + 8] = col(inp["g_attn_out"][0])
    pv[:, PV_GCONV:PV_GCONV + 8] = col(inp["g_conv_out"][0])
    pv[:, PV_GLN:PV_GLN + 8] = col(inp["g_conv_ln"][0])
    pv[:, PV_BLN:PV_BLN + 8] = col(inp["b_conv_ln"][0])
    pv[:, PV_BDW:PV_BDW + 8] = col(inp["b_dw"][0])
    wdw = np.asarray(inp["w_dw"], f)[0, :, 0, :]
    pv[:, PV_WDW:] = wdw.reshape(31, 8, 128).transpose(2, 1, 0).reshape(128, 8 * 31)
    d["pvec"] = pv
    cv = np.zeros((128, 16, 33), f)
    cv[:, :, 0] = col(inp["c_prompt"][b])
    cv[:, :, 32] = col(inp["c_sample"][sb_])
    d["cvec"] = cv
    d["w_ada"] = np.asarray(inp["w_ada"], f)[0]
    ba = np.zeros((33, 12288), f)
    ba[0] = inp["b_ada"][0]
    ba[32] = inp["b_ada"][0]
    d["b_ada33"] = ba
    gp = np.zeros((33, 2, 2048), f)
    for r in (0, 32):
        gp[r, 0] = inp["g_post_mix"][0]
        gp[r, 1] = inp["g_post_ffn"][0]
    d["gpost33"] = gp
    d["w_in"] = np.asarray(inp["w_in"], f)[0]
    d["w_out"] = np.asarray(inp["w_out"], f)[0]
    d["w_gu"] = np.asarray(inp["w_gate_up"], f)[0]
    d["w_down"] = np.asarray(inp["w_down"], f)[0]
    xp = np.asarray(inp["x_prompt"], f)
    d["xb"] = xp[b]
    d["xown"] = xp[b, qstart:qstart + c.OWN]
    xm = np.zeros((c.MW, 2048), f)
    xm[0:4] = np.asarray(inp["x_sample"], f)[sb_]
    if qstart > 0:
        xm[4:36] = xp[b, qstart - 32:qstart]
    d["xmini"] = xm
    d["haloflag"] = np.full((128, 1), 1.0 if qstart > 0 else 0.0, f)
    d["cache_k"] = np.asarray(inp["cache_k"], f)[0].reshape(-1, 1024)
    d["cache_v"] = np.asarray(inp["cache_v"], f)[0].reshape(-1, 1024)
    d["ptab"] = np.asarray(inp["page_table"], np.int32)[sb_:sb_ + 1]
    d["sconv"] = np.asarray(inp["state_conv"], f)[0, sb_]
    return d


_NC_CACHE = {}


def run_cores(cfg, inp, assign):
    key = (cfg.S, cfg.OWN, cfg.NPG, cfg.NPOOL)
    if key not in _NC_CACHE:
        _NC_CACHE[key] = build(cfg)
    nc = _NC_CACHE[key]
    maps = [core_inputs(cfg, inp, b, q, s) for (b, q, s) in assign]
    res = run_bass_kernel_spmd(nc, maps, core_ids=list(range(len(assign))))
    return res.results


def kernel(**inp):
    cfg = Cfg()
    assign = [(cid // 4, (cid % 4) * 1024, cid) for cid in range(8)]
    r = run_cores(cfg, inp, assign)
    f = np.float32
    y_p = np.zeros((2, 4096, 2048), f)
    k_p = np.zeros((1, 2, 32, 8, 128, 128), f)
    v_p = np.zeros((1, 2, 32, 8, 128, 128), f)
    cv_p = np.zeros((1, 2, 30, 1024), f)
    y_s = np.zeros((8, 4, 2048), f)
    k_s = np.zeros((1, 8, 8, 4, 128), f)
    v_s = np.zeros((1, 8, 8, 4, 128), f)
    cv_s = np.zeros((1, 8, 30, 1024), f)
    for cid, (b, q, s) in enumerate(assign):
        o = r[cid]
        y_p[b, q:q + 1024] = o["y_own"]
        k_p[0, b, q // 128:q // 128 + 8] = o["k_own"]
        v_p[0, b, q // 128:q // 128 + 8] = o["v_own"]
        if q == 3072:
            cv_p[0, b] = o["convp"]
        y_s[s] = o["y_s"]
        k_s[0, s] = o["k_s"]
        v_s[0, s] = o["v_s"]
        cv_s[0, s] = o["conv_s"]
    return (y_p, y_s, k_p, v_p, cv_p, k_s, v_s, cv_s)
```
